# Optimizing a Trainium2 kernel written in Bass

```python
import jax, jax.numpy as jnp
from jax import lax
import numpy as np

D_MODEL = 1024
BATCH = 16
SEQ = 2048
DEPTH = 4

PE_DIM = 256
D_MIX = D_MODEL
N_MIXERS = 4
GROUP_W = D_MIX // N_MIXERS
HEAD_DIM = 64
GROUP_HEADS = GROUP_W // HEAD_DIM
CONV_K = 4
MLSTM_CHUNK = 64
RG_C = 8.0
RWKV_DECAY_RANK = 32
RWKV_ICL_RANK = 32
RWKV_GATE_RANK = 64
RWKV_GN_EPS = 64e-5
SSD_STATE = 64
SSD_GROUPS = 2
SSD_CHUNK = 128
SSD_XBC = GROUP_W + 2 * SSD_GROUPS * SSD_STATE
M_COLS = 4 * GROUP_W + 2 * GROUP_HEADS
G_COLS = 2 * GROUP_W
R_COLS = 3 * GROUP_W + RWKV_DECAY_RANK + RWKV_ICL_RANK + RWKV_GATE_RANK
S_COLS = GROUP_W + SSD_XBC + GROUP_HEADS
N_IN = M_COLS + G_COLS + R_COLS + S_COLS
D_FF = 256 * ((8 * D_MODEL // 3 + 255) // 256)
N_EXPERTS = 8
TOP_K = 2
EXPERT_FF = D_FF
MOE_BLOCK = 256
N_DENSE = (DEPTH + 1) // 2
N_MOE = DEPTH // 2
ALPHA = (2 * DEPTH) ** 0.25
BETA = (8 * DEPTH) ** -0.25
LN_EPS = 1e-5
RMS_EPS = 1e-6

kernel_name = 'hymba_style_hybrid_mlstm_rglru_rwkv7_ssd_moe'


def _layer_norm(x, g, b):
    xf = x.astype(jnp.float32)
    xc = xf - jnp.mean(xf, -1, keepdims=True)
    var = jnp.mean(xc * xc, -1, keepdims=True)
    return (xc * lax.rsqrt(var + LN_EPS)).astype(x.dtype) * g + b


def _rms(x, w):
    xf = x.astype(jnp.float32)
    y = xf * lax.rsqrt(jnp.mean(xf * xf, -1, keepdims=True) + RMS_EPS)
    return y.astype(x.dtype) * w


def _causal_dwconv(u, w, b):
    k, c = w.shape
    out = lax.conv_general_dilated(u, w[:, None, :].astype(u.dtype), window_strides=(1,),
                                   padding=[(k - 1, 0)], dimension_numbers=('NWC', 'WIO', 'NWC'),
                                   feature_group_count=c)
    return out + b


def _linrec_combine(left, right):
    a_l, b_l = left
    a_r, b_r = right
    return a_l * a_r, a_r * b_l + b_r


def mlstm_group(cols, i_bias, f_bias, norm_w):
    bsz, seq, _ = cols.shape
    L, H, dh = MLSTM_CHUNK, GROUP_HEADS, HEAD_DIM
    nc = seq // L
    W = GROUP_W
    q, k, v, o, i_pre, f_pre = jnp.split(cols, [W, 2 * W, 3 * W, 4 * W, 4 * W + H], axis=-1)

    def heads(t):
        return t.reshape(bsz, nc, L, H, dh).transpose(0, 3, 1, 2, 4)

    def gates(t):
        return t.astype(jnp.float32).reshape(bsz, nc, L, H).transpose(0, 3, 1, 2)

    qh, kh, vh = heads(q), heads(k) * (dh ** -0.5), heads(v)
    log_i = gates(i_pre + i_bias)
    log_f = jax.nn.log_sigmoid(gates(f_pre + f_bias))
    b = jnp.cumsum(log_f, axis=-1)
    g = b[..., -1]
    a = g[..., None] - b + log_i

    def chunk_step(carry, xs):
        C, n, m = carry
        k_c, v_c, a_c, g_c = xs
        m_new = jnp.maximum(g_c + m, a_c.max(-1))
        decay = jnp.exp(g_c + m - m_new)
        w = jnp.exp(a_c - m_new[..., None])
        C_new = decay[..., None, None] * C + jnp.einsum('bhl,bhlv,bhlk->bhvk', w, v_c, k_c)
        n_new = decay[..., None] * n + jnp.einsum('bhl,bhlk->bhk', w, k_c)
        return (C_new, n_new, m_new), (C, n, m)

    init = (jnp.zeros((bsz, H, dh, dh), jnp.float32), jnp.zeros((bsz, H, dh), jnp.float32),
            jnp.zeros((bsz, H), jnp.float32))
    xs = (kh.transpose(2, 0, 1, 3, 4), vh.transpose(2, 0, 1, 3, 4), a.transpose(2, 0, 1, 3), g.transpose(2, 0, 1))
    _, (C_prev, n_prev, m_prev) = lax.scan(chunk_step, init, xs)
    C_prev = C_prev.transpose(1, 2, 0, 3, 4)
    n_prev = n_prev.transpose(1, 2, 0, 3)
    m_prev = m_prev.transpose(1, 2, 0)

    causal = jnp.tril(jnp.ones((L, L), bool))
    log_D = jnp.where(causal, b[..., :, None] - b[..., None, :] + log_i[..., None, :], -jnp.inf)
    inter_log = b + m_prev[..., None]
    m_t = jnp.maximum(inter_log, log_D.max(-1))
    s = jnp.einsum('bhcld,bhcsd->bhcls', qh, kh) * jnp.exp(log_D - m_t[..., None])
    inter_w = jnp.exp(inter_log - m_t)
    num = jnp.einsum('bhcls,bhcsd->bhcld', s, vh) + inter_w[..., None] * jnp.einsum('bhcvk,bhclk->bhclv', C_prev, qh)
    den = s.sum(-1) + inter_w * jnp.einsum('bhck,bhclk->bhcl', n_prev, qh)
    h = num / jnp.maximum(jnp.abs(den), jnp.exp(-m_t))[..., None]
    h = h.transpose(0, 2, 3, 1, 4).reshape(bsz, seq, H, dh)
    h = _rms(h.astype(cols.dtype), norm_w.reshape(H, dh)).reshape(bsz, seq, W)
    return h * jax.nn.sigmoid(o)


def rglru_group(cols, conv_w, conv_b, w_a, b_a, w_x, b_x, lam):
    bsz, seq, _ = cols.shape
    xb, gate = jnp.split(cols, [GROUP_W], axis=-1)
    xc = _causal_dwconv(xb, conv_w, conv_b)
    xh = xc.reshape(bsz, seq, GROUP_HEADS, HEAD_DIM)
    r = jax.nn.sigmoid(jnp.einsum('bshi,hij->bshj', xh, w_a).reshape(bsz, seq, GROUP_W) + b_a)
    ig = jax.nn.sigmoid(jnp.einsum('bshi,hij->bshj', xh, w_x).reshape(bsz, seq, GROUP_W) + b_x)
    log_a = (-RG_C * jax.nn.softplus(-lam) * r).astype(jnp.float32)
    a = jnp.exp(log_a)
    u = jnp.sqrt(-jnp.expm1(2.0 * log_a)) * (ig * xc)
    _, h = lax.associative_scan(_linrec_combine, (a, u), axis=1)
    return h.astype(cols.dtype) * jax.nn.gelu(gate)


def rwkv7_group(cols, mu, w0, w2, a0, a2, g2, k_k, k_a, r_k, ln_w, ln_b):
    bsz, seq, _ = cols.shape
    H, N, W = GROUP_HEADS, HEAD_DIM, GROUP_W
    prev = jnp.pad(cols[:, :-1], ((0, 0), (1, 0), (0, 0)))
    cols = cols + (prev - cols) * mu
    r, k, v, wd, ad, gd = jnp.split(
        cols, [W, 2 * W, 3 * W, 3 * W + RWKV_DECAY_RANK, 3 * W + RWKV_DECAY_RANK + RWKV_ICL_RANK], axis=-1)
    w_log = -jax.nn.softplus(-(w0 + jnp.tanh(wd) @ w2)) - 0.5
    decay = jnp.exp(-jnp.exp(w_log.astype(jnp.float32)))
    a = jax.nn.sigmoid(a0 + ad @ a2)
    g = jax.nn.sigmoid(gd) @ g2

    def hd(t):
        return t.reshape(bsz, seq, H, N)

    kk = hd(k * k_k).astype(jnp.float32)
    kk = kk * lax.rsqrt(jnp.maximum(jnp.sum(kk * kk, -1, keepdims=True), 1e-24))
    k = k * (1.0 + (a - 1.0) * k_a)
    rh, kh, vh = hd(r), hd(k), hd(v)

    def step(S, xs):
        r_t, d_t, k_t, v_t, kk_t, a_t = xs
        sa = jnp.einsum('bhvk,bhk->bhv', S, kk_t)
        S = (S * d_t[:, :, None, :] - sa[..., None] * (kk_t * a_t)[:, :, None, :]
             + v_t[..., None] * k_t[:, :, None, :])
        return S, jnp.einsum('bhvk,bhk->bhv', S, r_t)

    def tm(t):
        return t.astype(jnp.float32).transpose(1, 0, 2, 3)

    S0 = jnp.zeros((bsz, H, N, N), jnp.float32)
    _, y = lax.scan(step, S0, (tm(rh), tm(hd(decay)), tm(kh), tm(vh), tm(kk), tm(hd(a))))
    y = y.transpose(1, 0, 2, 3)
    yc = y - jnp.mean(y, -1, keepdims=True)
    yn = yc * lax.rsqrt(jnp.mean(yc * yc, -1, keepdims=True) + RWKV_GN_EPS)
    yn = yn.reshape(bsz, seq, W).astype(cols.dtype) * ln_w + ln_b
    bonus = jnp.sum(rh * kh * r_k, -1, keepdims=True) * vh
    return (yn + bonus.reshape(bsz, seq, W)) * g


def ssd_group(cols, conv_w, conv_b, dt_bias, a_log, d_skip, norm_w):
    bsz, seq, _ = cols.shape
    H, P, G, N, L = GROUP_HEADS, HEAD_DIM, SSD_GROUPS, SSD_STATE, SSD_CHUNK
    nc = seq // L
    z, xbc, dt = jnp.split(cols, [GROUP_W, GROUP_W + SSD_XBC], axis=-1)
    xbc = jax.nn.silu(_causal_dwconv(xbc, conv_w, conv_b))
    xs, bm, cm = jnp.split(xbc, [GROUP_W, GROUP_W + G * N], axis=-1)
    x = xs.reshape(bsz, nc, L, H, P)
    rep = H // G
    bm = jnp.repeat(bm.reshape(bsz, nc, L, G, N), rep, axis=3)
    cm = jnp.repeat(cm.reshape(bsz, nc, L, G, N), rep, axis=3)
    dt = jax.nn.softplus((dt + dt_bias).astype(jnp.float32)).reshape(bsz, nc, L, H)
    a_dt = (-jnp.exp(a_log.astype(jnp.float32)) * dt).transpose(0, 3, 1, 2)
    a_cs = jnp.cumsum(a_dt, axis=-1)
    causal = jnp.tril(jnp.ones((L, L), bool))
    decay_in = jnp.exp(jnp.where(causal, a_cs[..., :, None] - a_cs[..., None, :], -jnp.inf))
    xdt = x * dt[..., None]
    scores = jnp.einsum('bclhn,bcshn->bhcls', cm, bm) * decay_in
    y_diag = jnp.einsum('bhcls,bcshp->bclhp', scores, xdt)
    decay_states = jnp.exp(a_cs[..., -1:] - a_cs)
    states = jnp.einsum('bclhn,bhcl,bclhp->bchpn', bm, decay_states, xdt)
    tot_cs = jnp.cumsum(jnp.pad(a_cs[..., -1], ((0, 0), (0, 0), (1, 0))), axis=-1)
    chunk_causal = jnp.tril(jnp.ones((nc + 1, nc + 1), bool))
    decay_chunk = jnp.exp(jnp.where(chunk_causal, tot_cs[..., :, None] - tot_cs[..., None, :], -jnp.inf))
    states = jnp.pad(states, ((0, 0), (1, 0), (0, 0), (0, 0), (0, 0)))
    states_in = jnp.einsum('bhzc,bchpn->bzhpn', decay_chunk, states)[:, :-1]
    y_off = jnp.einsum('bclhn,bchpn,bhcl->bclhp', cm, states_in, jnp.exp(a_cs))
    y = y_diag + y_off + x * d_skip[:, None]
    y = y.reshape(bsz, seq, GROUP_W).astype(cols.dtype) * jax.nn.silu(z)
    y = _rms(y.reshape(bsz, seq, G, GROUP_W // G), norm_w.reshape(G, GROUP_W // G))
    return y.reshape(bsz, seq, GROUP_W)


def _swiglu(x, w1, w3, w2):
    return (jax.nn.silu(x @ w1) * (x @ w3)) @ w2


def _moe_swiglu(x, w_router, w1, w3, w2):
    bsz, seq, d = x.shape
    T = bsz * seq
    xf = x.reshape(T, d)
    logits = (xf @ w_router).astype(jnp.float32)
    top_logit, top_e = lax.top_k(logits, TOP_K)
    gate = jax.nn.softmax(top_logit, axis=-1).astype(x.dtype)
    flat_e = top_e.reshape(-1)
    flat_tok = jnp.repeat(jnp.arange(T, dtype=jnp.int32), TOP_K)
    order = jnp.argsort(flat_e)
    se, st, sg = flat_e[order], flat_tok[order], gate.reshape(-1)[order]
    counts = jnp.bincount(flat_e, length=N_EXPERTS)
    padded = (counts + MOE_BLOCK - 1) // MOE_BLOCK * MOE_BLOCK
    pad_end = jnp.cumsum(padded)
    start = jnp.cumsum(counts) - counts
    dest = (pad_end - padded)[se] + jnp.arange(T * TOP_K) - start[se]
    n_blocks = -(-(T * TOP_K) // MOE_BLOCK) + N_EXPERTS
    rows = n_blocks * MOE_BLOCK
    slot_tok = jnp.zeros((rows,), jnp.int32).at[dest].set(st)
    slot_gate = jnp.zeros((rows,), x.dtype).at[dest].set(sg)
    block_e = jnp.minimum(jnp.searchsorted(pad_end, jnp.arange(n_blocks) * MOE_BLOCK, side='right'), N_EXPERTS - 1)
    xb = xf[slot_tok].reshape(n_blocks, MOE_BLOCK, d)

    def expert_block(args):
        xblk, e = args
        return (jax.nn.silu(xblk @ w1[e]) * (xblk @ w3[e])) @ w2[e]

    yb = lax.map(expert_block, (xb, block_e))
    y = jnp.zeros((T, d), x.dtype).at[slot_tok].add(yb.reshape(rows, d) * slot_gate[:, None])
    return y.reshape(bsz, seq, d)


def setup_inputs(seed: int = 0) -> dict:
    key = jax.random.key(seed)
    ks = iter(jax.random.split(key, 64))

    def nrm(shape, scale):
        return jax.random.normal(next(ks), shape, jnp.float32) * scale

    def unif(shape, lo, hi):
        return jax.random.uniform(next(ks), shape, jnp.float32, lo, hi)

    Lyr, W, H, P = DEPTH, GROUP_W, GROUP_HEADS, HEAD_DIM
    x = nrm((BATCH, SEQ, D_MODEL), 1.0)
    p = nrm((DEPTH, BATCH, SEQ, PE_DIM), 1.0)
    w_in = nrm((Lyr, D_MODEL, N_IN), D_MODEL ** -0.5)
    m_i_bias = nrm((Lyr, H), 0.1)
    m_f_bias = jnp.linspace(3.0, 6.0, H)[None, :] + nrm((Lyr, H), 0.1)
    m_norm_w = 1.0 + nrm((Lyr, W), 0.02)
    g_conv_w = nrm((Lyr, CONV_K, W), CONV_K ** -0.5)
    g_conv_b = nrm((Lyr, W), 0.01)
    g_w_a = nrm((Lyr, H, P, P), P ** -0.5)
    g_b_a = nrm((Lyr, W), 0.01)
    g_w_x = nrm((Lyr, H, P, P), P ** -0.5)
    g_b_x = nrm((Lyr, W), 0.01)
    s_pow = unif((Lyr, W), 0.9, 0.999) ** (1.0 / RG_C)
    g_lambda = jnp.log(s_pow) - jnp.log1p(-s_pow)
    r_mu = unif((Lyr, R_COLS), 0.0, 1.0)
    r_w0 = (-6.0 + 5.0 * (jnp.arange(W, dtype=jnp.float32) / (W - 1)) ** 0.85)[None, :] + nrm((Lyr, W), 0.1)
    r_w2 = nrm((Lyr, RWKV_DECAY_RANK, W), 0.1 * RWKV_DECAY_RANK ** -0.5)
    r_a0 = nrm((Lyr, W), 0.1)
    r_a2 = nrm((Lyr, RWKV_ICL_RANK, W), RWKV_ICL_RANK ** -0.5)
    r_g2 = nrm((Lyr, RWKV_GATE_RANK, W), RWKV_GATE_RANK ** -0.5)
    r_k_k = 0.85 + nrm((Lyr, W), 0.02)
    r_k_a = 1.0 + nrm((Lyr, W), 0.02)
    r_r_k = -0.04 + nrm((Lyr, H, P), 0.01)
    r_ln_w = 1.0 + nrm((Lyr, W), 0.02)
    r_ln_b = nrm((Lyr, W), 0.01)
    s_conv_w = nrm((Lyr, CONV_K, SSD_XBC), CONV_K ** -0.5)
    s_conv_b = nrm((Lyr, SSD_XBC), 0.01)
    dt0 = jnp.exp(unif((Lyr, H), float(np.log(1e-3)), float(np.log(1e-1))))
    s_dt_bias = dt0 + jnp.log(-jnp.expm1(-dt0))
    s_a_log = jnp.log(unif((Lyr, H), 1.0, 16.0))
    s_d = 1.0 + nrm((Lyr, H), 0.1)
    s_norm_w = 1.0 + nrm((Lyr, W), 0.02)
    w_out = nrm((Lyr, D_MIX, D_MODEL), D_MIX ** -0.5 * BETA)
    ln1_g = 1.0 + nrm((Lyr, D_MODEL), 0.02)
    ln1_b = nrm((Lyr, D_MODEL), 0.01)
    ln2_g = 1.0 + nrm((Lyr, D_MODEL), 0.02)
    ln2_b = nrm((Lyr, D_MODEL), 0.01)
    f_w1 = nrm((N_DENSE, D_MODEL, D_FF), D_MODEL ** -0.5)
    f_w3 = nrm((N_DENSE, D_MODEL, D_FF), D_MODEL ** -0.5)
    f_w2 = nrm((N_DENSE, D_FF, D_MODEL), D_FF ** -0.5 * BETA)
    e_router = nrm((N_MOE, D_MODEL, N_EXPERTS), D_MODEL ** -0.5)
    e_w1 = nrm((N_MOE, N_EXPERTS, D_MODEL, EXPERT_FF), D_MODEL ** -0.5)
    e_w3 = nrm((N_MOE, N_EXPERTS, D_MODEL, EXPERT_FF), D_MODEL ** -0.5)
    e_w2 = nrm((N_MOE, N_EXPERTS, EXPERT_FF, D_MODEL), EXPERT_FF ** -0.5 * BETA)
    pe_proj = nrm((Lyr, PE_DIM, D_MODEL), PE_DIM ** -0.5 * BETA)
    pe_gate_w = nrm((Lyr, D_MODEL, D_MODEL), D_MODEL ** -0.5)
    pe_gate_b = nrm((Lyr, D_MODEL), 0.01)
    return {'x': x, 'p': p, 'w_in': w_in, 'm_i_bias': m_i_bias, 'm_f_bias': m_f_bias, 'm_norm_w': m_norm_w,
            'g_conv_w': g_conv_w, 'g_conv_b': g_conv_b, 'g_w_a': g_w_a, 'g_b_a': g_b_a, 'g_w_x': g_w_x,
            'g_b_x': g_b_x, 'g_lambda': g_lambda, 'r_mu': r_mu, 'r_w0': r_w0, 'r_w2': r_w2, 'r_a0': r_a0,
            'r_a2': r_a2, 'r_g2': r_g2, 'r_k_k': r_k_k, 'r_k_a': r_k_a, 'r_r_k': r_r_k, 'r_ln_w': r_ln_w,
            'r_ln_b': r_ln_b, 's_conv_w': s_conv_w, 's_conv_b': s_conv_b, 's_dt_bias': s_dt_bias,
            's_a_log': s_a_log, 's_d': s_d, 's_norm_w': s_norm_w, 'w_out': w_out, 'ln1_g': ln1_g,
            'ln1_b': ln1_b, 'ln2_g': ln2_g, 'ln2_b': ln2_b, 'f_w1': f_w1, 'f_w3': f_w3, 'f_w2': f_w2,
            'e_router': e_router, 'e_w1': e_w1, 'e_w3': e_w3, 'e_w2': e_w2, 'pe_proj': pe_proj,
            'pe_gate_w': pe_gate_w, 'pe_gate_b': pe_gate_b}


def reference(x, p, w_in, m_i_bias, m_f_bias, m_norm_w, g_conv_w, g_conv_b, g_w_a, g_b_a, g_w_x, g_b_x,
              g_lambda, r_mu, r_w0, r_w2, r_a0, r_a2, r_g2, r_k_k, r_k_a, r_r_k, r_ln_w, r_ln_b,
              s_conv_w, s_conv_b, s_dt_bias, s_a_log, s_d, s_norm_w, w_out, ln1_g, ln1_b, ln2_g, ln2_b,
              f_w1, f_w3, f_w2, e_router, e_w1, e_w3, e_w2, pe_proj, pe_gate_w, pe_gate_b):
    splits = [M_COLS, M_COLS + G_COLS, M_COLS + G_COLS + R_COLS]
    for i in range(DEPTH):
        proj = x @ w_in[i]
        m_cols, g_cols, r_cols, s_cols = jnp.split(proj, splits, axis=-1)
        mix = jnp.concatenate([
            mlstm_group(m_cols, m_i_bias[i], m_f_bias[i], m_norm_w[i]),
            rglru_group(g_cols, g_conv_w[i], g_conv_b[i], g_w_a[i], g_b_a[i], g_w_x[i], g_b_x[i], g_lambda[i]),
            rwkv7_group(r_cols, r_mu[i], r_w0[i], r_w2[i], r_a0[i], r_a2[i], r_g2[i], r_k_k[i], r_k_a[i],
                        r_r_k[i], r_ln_w[i], r_ln_b[i]),
            ssd_group(s_cols, s_conv_w[i], s_conv_b[i], s_dt_bias[i], s_a_log[i], s_d[i], s_norm_w[i]),
        ], axis=-1)
        x = _layer_norm(ALPHA * x + mix @ w_out[i], ln1_g[i], ln1_b[i])
        if i % 2 == 0:
            ff = _swiglu(x, f_w1[i // 2], f_w3[i // 2], f_w2[i // 2])
        else:
            ff = _moe_swiglu(x, e_router[i // 2], e_w1[i // 2], e_w3[i // 2], e_w2[i // 2])
        pe = (p[i] @ pe_proj[i]) * jax.nn.sigmoid(x @ pe_gate_w[i] + pe_gate_b[i])
        x = _layer_norm(ALPHA * x + ff + pe, ln2_g[i], ln2_b[i])
    return x
```

```python
import numpy as np
from contextlib import ExitStack
import concourse.bass as bass
import concourse.mybir as mybir
from concourse.bass_utils import run_bass_kernel_spmd

F32 = mybir.dt.float32
BF16 = mybir.dt.bfloat16
AF = mybir.ActivationFunctionType
ALU = mybir.AluOpType
AX = mybir.AxisListType

ENGS = ["tensor", "vector", "scalar", "gpsimd", "sync"]
NPOOL = 16


class Sched:
    def __init__(self, nc, stack):
        self.nc = nc
        self.stack = stack
        self.prog = {e: [] for e in ENGS}
        self.sem = {}
        for e in ["tensor", "vector", "scalar", "gpsimd"]:
            self.sem[("c", e)] = stack.enter_context(nc.semaphore("c_" + e))
        for q in ["sync", "gpsimd"]:
            for i in range(NPOOL):
                self.sem[("d", q, i)] = stack.enter_context(nc.semaphore(f"d_{q}_{i}"))
        self.cnt = {e: 0 for e in ENGS}
        self.dman = {"sync": 0, "gpsimd": 0}
        self.known = {e: {} for e in ENGS}
        self.lastw = {}
        self.readers = {}
        self.nins = 0
        self.gran = {}
        self.allev = {}

    def keys(self, a):
        if isinstance(a, (str, tuple)):
            return [a]
        name = a.tensor.name
        g = self.gran.get(name)
        if g is None:
            return [name]
        ps = 1
        for d in list(a.tensor.shape)[1:]:
            ps *= int(d)
        off = int(a.offset) % ps
        ext = 1
        for (stp, cnt) in list(a.ap)[1:]:
            ext += (int(cnt) - 1) * abs(int(stp))
        return [(name, i) for i in range(off // g, (off + ext - 1) // g + 1)]

    def split(self, t, gran):
        self.gran[t.name] = gran

    def _ks(self, lst):
        out = []
        for a in lst:
            out.extend(self.keys(a))
        return out

    def _deps(self, eng, reads, writes, skip_self=False):
        need = {}
        for k in reads:
            ev = self.lastw.get(k)
            if ev is not None:
                need[ev[0]] = max(need.get(ev[0], 0), ev[1])
        for k in writes:
            ev = self.lastw.get(k)
            if ev is not None:
                need[ev[0]] = max(need.get(ev[0], 0), ev[1])
            for s, v in self.readers.get(k, {}).items():
                need[s] = max(need.get(s, 0), v)
        for s, v in need.items():
            if skip_self and s == ("c", eng):
                continue
            if self.known[eng].get(s, 0) < v:
                self.prog[eng].append(("wait", s, v))
                self.known[eng][s] = v

    def _commit(self, ev, reads, writes):
        self.allev[ev[0]] = max(self.allev.get(ev[0], 0), ev[1])
        for k in writes:
            self.lastw[k] = ev
            self.readers[k] = {}
        for k in reads:
            d = self.readers.setdefault(k, {})
            d[ev[0]] = max(d.get(ev[0], 0), ev[1])

    def op(self, eng, fn, reads=(), writes=(), skip_self=False):
        writes = list(writes) + [a for a in reads if not isinstance(a, (str, tuple)) and "PSum" in type(a.tensor).__name__]
        reads = self._ks(reads)
        writes = self._ks(writes)
        self._deps(eng, reads, writes, skip_self)
        self.cnt[eng] += 1
        ev = (("c", eng), self.cnt[eng])
        self.prog[eng].append(("op", fn, ev))
        self._commit(ev, reads, writes)
        self.nins += 1

    def dma(self, q, out, in_, reads=None, writes=None, **kw):
        reads = self._ks(reads if reads is not None else [in_])
        writes = self._ks(writes if writes is not None else [out])
        n = self.dman[q]
        slot = n % NPOOL
        tgt = 16 * (n // NPOOL + 1)
        s = ("d", q, slot)
        if n >= NPOOL and self.known[q].get(s, 0) < tgt - 16:
            self.prog[q].append(("wait", s, tgt - 16))
            self.known[q][s] = tgt - 16
        self._deps(q, reads, writes)
        self.dman[q] += 1
        ev = (s, tgt)
        self.prog[q].append(("dma", (out, in_, kw), ev))
        self._commit(ev, reads, writes)
        self.nins += 1
        return ev

    def wait_all(self, eng="sync"):
        for s, v in self.allev.items():
            if self.known[eng].get(s, 0) < v:
                self.prog[eng].append(("wait", s, v))
                self.known[eng][s] = v

    def barrier(self):
        for e in ENGS:
            self.wait_all(e)
        self.lastw = {}
        self.readers = {}

    def emit(self):
        nc = self.nc
        sem = self.sem
        prog = self.prog
        with nc.Block() as block:
            def run(engname):
                def body(eng):
                    for it in prog[engname]:
                        if it[0] == "wait":
                            eng.wait_ge(sem[it[1]], it[2])
                        elif it[0] == "op":
                            ins = it[1](eng)
                            ins.then_inc(sem[it[2][0]], 1)
                        else:
                            out, in_, kw = it[1]
                            eng.dma_start(out=out, in_=in_, **kw).then_inc(sem[it[2][0]], 16)
                return body
            block.tensor(run("tensor"))
            block.vector(run("vector"))
            block.scalar(run("scalar"))
            block.gpsimd(run("gpsimd"))
            block.sync(run("sync"))
        self.prog = {e: [] for e in ENGS}

    def mm(self, out, lhsT, rhs, start=True, stop=True, extra_reads=(), sync_self=False):
        self.op("tensor", lambda e: e.matmul(out, lhsT, rhs, start=start, stop=stop),
                reads=[lhsT, rhs, *extra_reads], writes=[out], skip_self=not sync_self)

    def transpose(self, out, in_, ident):
        self.op("tensor", lambda e: e.transpose(out, in_, ident),
                reads=[in_, ident], writes=[out], skip_self=True)

    def act(self, out, in_, func, bias=0.0, scale=1.0, accum_out=None, eng="scalar"):
        reads = [in_]
        if not isinstance(bias, (int, float)):
            reads.append(bias)
        if not isinstance(scale, (int, float)):
            reads.append(scale)
        writes = [out]
        kw = {}
        if accum_out is not None:
            writes.append(accum_out)
            kw["accum_out"] = accum_out
        self.op("scalar", lambda e: e.activation(out, in_, func, bias=bias, scale=scale, **kw),
                reads=reads, writes=writes)

    def tt(self, out, in0, in1, op, eng="vector"):
        self.op(eng, lambda e: e.tensor_tensor(out, in0, in1, op), reads=[in0, in1], writes=[out])

    def ts(self, out, in0, s1, s2=None, op0=ALU.mult, op1=None, eng="vector", accum_out=None):
        reads = [in0]
        if not isinstance(s1, (int, float)):
            reads.append(s1)
        if s2 is not None and not isinstance(s2, (int, float)):
            reads.append(s2)
        writes = [out]
        kw = {}
        if op1 is not None:
            kw["op1"] = op1
        if accum_out is not None:
            kw["accum_out"] = accum_out
            writes.append(accum_out)
        self.op(eng, lambda e: e.tensor_scalar(out, in0, s1, s2, op0, **kw), reads=reads, writes=writes)

    def stt(self, out, in0, scalar, in1, op0, op1, eng="vector"):
        reads = [in0, in1]
        if not isinstance(scalar, (int, float)):
            reads.append(scalar)
        self.op(eng, lambda e: e.scalar_tensor_tensor(out, in0, scalar, in1, op0, op1), reads=reads, writes=[out])

    def copy(self, out, in_, eng="vector"):
        self.op(eng, lambda e: e.tensor_copy(out, in_), reads=[in_], writes=[out])

    def memset(self, ap, val, eng="vector"):
        self.op(eng, lambda e: e.memset(ap, val), reads=[], writes=[ap])

    def scan(self, out, d0, d1, init, op0=ALU.mult, op1=ALU.add):
        reads = [d0, d1]
        if not isinstance(init, (int, float)):
            reads.append(init)
        self.op("vector", lambda e: e.tensor_tensor_scan(out, d0, d1, init, op0, op1), reads=reads, writes=[out])

    def reduce(self, out, in_, op=ALU.add, axis=AX.X, eng="vector"):
        self.op(eng, lambda e: e.tensor_reduce(out, in_, axis, op), reads=[in_], writes=[out])

    def recip(self, out, in_):
        self.op("vector", lambda e: e.reciprocal(out, in_), reads=[in_], writes=[out])


DM = 1024
NIN = 3212
DFF = 2816
ALPHA = 8.0 ** 0.25
LN_EPS = 1e-5
OFF_M, OFF_G, OFF_R, OFF_S = 0, 1032, 1544, 2440

WNAMES = [("w_in", [4, 1024, 3212]), ("m_i_bias", [4, 4]), ("m_f_bias", [4, 4]), ("m_norm_w", [4, 256]),
          ("g_conv_w", [4, 4, 256]), ("g_conv_b", [4, 256]), ("g_w_a", [4, 4, 64, 64]), ("g_b_a", [4, 256]),
          ("g_w_x", [4, 4, 64, 64]), ("g_b_x", [4, 256]), ("g_lambda", [4, 256]), ("r_mu", [4, 896]),
          ("r_w0", [4, 256]), ("r_w2", [4, 32, 256]), ("r_a0", [4, 256]), ("r_a2", [4, 32, 256]),
          ("r_g2", [4, 64, 256]), ("r_k_k", [4, 256]), ("r_k_a", [4, 256]), ("r_r_k", [4, 4, 64]),
          ("r_ln_w", [4, 256]), ("r_ln_b", [4, 256]), ("s_conv_w", [4, 4, 512]), ("s_conv_b", [4, 512]),
          ("s_dt_bias", [4, 4]), ("s_a_log", [4, 4]), ("s_d", [4, 4]), ("s_norm_w", [4, 256]),
          ("w_out", [4, 1024, 1024]), ("ln1_g", [4, 1024]), ("ln1_b", [4, 1024]), ("ln2_g", [4, 1024]),
          ("ln2_b", [4, 1024]), ("f_w1", [2, 1024, 2816]), ("f_w3", [2, 1024, 2816]), ("f_w2", [2, 2816, 1024]),
          ("e_router", [2, 1024, 8]), ("e_w1", [2, 8, 1024, 2816]), ("e_w3", [2, 8, 1024, 2816]),
          ("e_w2", [2, 8, 2816, 1024]), ("pe_proj", [4, 256, 1024]), ("pe_gate_w", [4, 1024, 1024]),
          ("pe_gate_b", [4, 1024])]


def host_consts():
    j = np.arange(128)
    c = {}
    c["c_ident"] = np.eye(128, dtype=np.float32)
    c["c_triu"] = (j[:, None] <= j[None, :]).astype(np.float32)
    c["c_strl"] = (j[:, None] > j[None, :]).astype(np.float32)
    sel = np.zeros((8, 8, 128), np.float32)
    for e in range(8):
        sel[e, e, :] = 1.0
    c["c_sel"] = sel
    return c


class Ctx:
    pass


def build(depth=4, seq=2048, debug=False, mixers=("m", "g", "r", "s")):
    T = 2 * seq
    NT = T // 512
    NTS = seq // 512
    nc = bass.Bass("TRN2", target_bir_lowering=False)
    D = {}
    D["x"] = nc.dram_tensor("x", [T, DM], F32, kind="ExternalInput").ap()
    D["p"] = nc.dram_tensor("p", [4, T, 256], F32, kind="ExternalInput").ap()
    for n, sh in WNAMES:
        D[n] = nc.dram_tensor(n, sh, F32, kind="ExternalInput").ap()
    for n, a in host_consts().items():
        D[n] = nc.dram_tensor(n, list(a.shape), F32, kind="ExternalInput").ap()
    out = nc.dram_tensor("out", [T, DM], F32, kind="ExternalOutput").ap()
    sk = "ExternalOutput" if debug else "Internal"
    XT = nc.dram_tensor("XT", [DM, T], F32, kind=sk).ap()
    PRJ = nc.dram_tensor("PRJ", [3328, T], F32, kind=sk).ap()
    MIX = nc.dram_tensor("MIX", [DM, T], BF16, kind=sk).ap()
    X1T = nc.dram_tensor("X1T", [DM, T], F32, kind=sk).ap()
    XTv = XT.rearrange("(c p) t -> p c t", p=128)
    X1Tv = X1T.rearrange("(c p) t -> p c t", p=128)
    MIXv = MIX.rearrange("(c p) t -> p c t", p=128)

    with ExitStack() as top, nc.allow_non_contiguous_dma(reason="small param / strided tile loads"):
        S = Sched(nc, top)

        def ld(q, dst, src):
            S.dma(q, dst, src, reads=[], writes=[dst])

        def stq(q, dst, src):
            S.dma(q, dst, src, reads=[src], writes=[])

        def colvec(dst, vec, n=128):
            C = dst.shape[1]
            for c in range(C):
                ld("sync", dst[:, c:c + 1], vec[c * n:(c + 1) * n].rearrange("(p o) -> p o", o=1))

        UID = [0]

        class Ph:
            def __init__(self):
                self.st = ExitStack()
                UID[0] += 1
                self.uid = UID[0]

            def __enter__(self):
                self.st.__enter__()
                return self

            def __exit__(self, *a):
                S.barrier()
                S.emit()
                return self.st.__exit__(*a)

            def sb(self, name, shape, dt=F32):
                return self.st.enter_context(nc.sbuf_tensor(f"{name}_{self.uid}", shape, dt))

            def ps(self, name, shape, dt=F32):
                return self.st.enter_context(nc.psum_tensor(f"{name}_{self.uid}", shape, dt))

            def consts(self, names):
                r = {}
                for n in names:
                    sh = list(D[n].shape)
                    t = self.sb("k_" + n, sh if len(sh) == 2 else [sh[0], sh[1], sh[2]])
                    ld("sync", t[:], D[n])
                    r[n] = t
                return r

        def layer_norm(ph, z, ones, g, b, pm, pq, sq, mean, rstd):
            S.act(sq[:], z[:], AF.Square)
            for c in range(8):
                S.mm(pm[:], ones[:], z[:, c, :], start=(c == 0), stop=(c == 7))
            for c in range(8):
                S.mm(pq[:], ones[:], sq[:, c, :], start=(c == 0), stop=(c == 7))
            S.act(mean[:], pm[:], AF.Copy)
            S.tt(rstd[:], mean[:], mean[:], ALU.mult)
            S.tt(rstd[:], pq[:], rstd[:], ALU.subtract)
            S.act(rstd[:], rstd[:], AF.Ln, bias=LN_EPS)
            S.act(rstd[:], rstd[:], AF.Exp, scale=-0.5)
            S.tt(z[:], z[:], mean[:].unsqueeze(1).broadcast_to([128, 8, 512]), ALU.subtract)
            S.tt(z[:], z[:], rstd[:].unsqueeze(1).broadcast_to([128, 8, 512]), ALU.mult)
            for c in range(8):
                S.ts(z[:, c, :], z[:, c, :], g[:, c:c + 1], b[:, c:c + 1], op0=ALU.mult, op1=ALU.add,
                     eng="gpsimd")

        def phase_in():
            with Ph() as ph:
                ident = ph.consts(["c_ident"])["c_ident"]
                xin = [ph.sb(f"xin{i}", [128, 4, DM]) for i in range(2)]
                xo = [ph.sb(f"xo{i}", [128, 8, 512]) for i in range(2)]
                pts = [ph.ps(f"pt{i}", [128, 512]) for i in range(4)]
                for t in range(NT):
                    xi = xin[t % 2]
                    ld("sync", xi[:], D["x"][t * 512:(t + 1) * 512, :].rearrange("(j p) f -> p j f", p=128))
                    o = xo[t % 2]
                    for c in range(8):
                        pt = pts[c % 4]
                        for j in range(4):
                            S.transpose(pt[:, j * 128:(j + 1) * 128], xi[:, j, c * 128:(c + 1) * 128], ident[:])
                        if c % 2 == 0:
                            S.copy(o[:, c, :], pt[:])
                        else:
                            S.act(o[:, c, :], pt[:], AF.Copy)
                    stq("sync", XTv[:, :, t * 512:(t + 1) * 512], o[:])

        def phase_proj(l):
            fuse_g = "g" in mixers
            with Ph() as ph:
                win = ph.sb("win", [128, 8, NIN], BF16)
                wv = D["w_in"][l].rearrange("(k p) n -> p k n", p=128)
                for c0 in range(0, NIN, 1024):
                    c1_ = min(NIN, c0 + 1024)
                    ld("gpsimd", win[:, :, c0:c1_], wv[:, :, c0:c1_])
                xb = [ph.sb(f"xb{i}", [128, 8, 512], BF16) for i in range(2)]
                stg = [ph.sb(f"stg{i}", [128, 512]) for i in range(6)]
                pps = [ph.ps(f"pp{i}", [128, 512]) for i in range(6)]
                chunks = [(i * 128, 128) for i in range(8)] + [(1024, 8)] + [(OFF_G + i * 128, 128) for i in range(4)] \
                    + [(OFF_R + i * 128, 128) for i in range(7)] + [(OFF_S + i * 128, 128) for i in range(6)] + [(3208, 4)]
                if fuse_g:
                    cw = ph.sb("cw", [128, 2, 4])
                    for j in range(4):
                        for c in range(2):
                            ld("sync", cw[:, c, j:j + 1], D["g_conv_w"][l, j, c * 128:(c + 1) * 128].rearrange("(p o) -> p o", o=1))
                    cb = ph.sb("cb", [128, 2]); colvec(cb, D["g_conv_b"][l])
                    ba = ph.sb("ba", [128, 2]); colvec(ba, D["g_b_a"][l])
                    bx = ph.sb("bx", [128, 2]); colvec(bx, D["g_b_x"][l])
                    lam = ph.sb("lam", [128, 2]); colvec(lam, D["g_lambda"][l])
                    c1 = ph.sb("c1", [128, 2])
                    S.act(c1[:], lam[:], AF.Exp, scale=-1.0)
                    S.act(c1[:], c1[:], AF.Ln, bias=1.0)
                    S.ts(c1[:], c1[:], -8.0, op0=ALU.mult)
                    bda = ph.sb("bda", [128, 2, 128]); bdx = ph.sb("bdx", [128, 2, 128])
                    S.memset(bda[:], 0.0); S.memset(bdx[:], 0.0)
                    for c in range(2):
                        for hh in range(2):
                            ld("sync", bda[hh * 64:(hh + 1) * 64, c, hh * 64:(hh + 1) * 64], D["g_w_a"][l, 2 * c + hh])
                            ld("sync", bdx[hh * 64:(hh + 1) * 64, c, hh * 64:(hh + 1) * 64], D["g_w_x"][l, 2 * c + hh])
                    xbh = [ph.sb(f"xbh{i}", [128, 2, 515]) for i in range(2)]
                    gtt = [ph.sb(f"gtt{i}", [128, 2, 512]) for i in range(2)]
                    for t_ in xbh:
                        S.split(t_, 515)
                    for t_ in gtt:
                        S.split(t_, 512)
                    Wk = {nm: [ph.sb(f"rg{nm}{c}", [128, 512]) for c in range(2)] for nm in ("xc", "A", "U", "H")}
                    obg = [ph.sb(f"rgob{c}", [128, 512], BF16) for c in range(2)]
                    hprev = [ph.sb(f"hprev{c}", [128, 1]) for c in range(2)]
                    gps = [ph.ps(f"gp{i}", [128, 512]) for i in range(2)]

                    def rg_tile_gen(t, c):
                        p = t % 2
                        first = (t % NTS == 0)
                        tok0 = t * 512
                        if first:
                            S.memset(xbh[p][:, c, 0:3], 0.0)
                        else:
                            S.copy(xbh[p][:, c, 0:3], xbh[1 - p][:, c, 512:515], eng="gpsimd")
                        yield
                        if True:
                            xh = xbh[p]; g_ = gtt[p][:, c, :]
                            xc, A, U, Hh = Wk["xc"][c][:], Wk["A"][c][:], Wk["U"][c][:], Wk["H"][c][:]
                            S.ts(xc, xh[:, c, 3:515], cw[:, c, 3:4], cb[:, c:c + 1], op0=ALU.mult, op1=ALU.add)
                            yield
                            for j in range(3):
                                S.stt(xc, xh[:, c, j:j + 512], cw[:, c, j:j + 1], xc, ALU.mult, ALU.add)
                                yield
                            S.mm(gps[0][:], bda[:, c, :], xc)
                            S.mm(gps[1][:], bdx[:, c, :], xc)
                            S.act(A, gps[0][:], AF.Sigmoid, bias=ba[:, c:c + 1])
                            S.act(U, gps[1][:], AF.Sigmoid, bias=bx[:, c:c + 1])
                            yield
                            S.act(A, A, AF.Exp, scale=c1[:, c:c + 1])
                            S.tt(U, U, xc, ALU.mult, eng="gpsimd")
                            yield
                            S.tt(Hh, A, A, ALU.mult, eng="gpsimd")
                            yield
                            S.ts(Hh, Hh, -1.0, 1.0, op0=ALU.mult, op1=ALU.add, eng="gpsimd")
                            yield
                            S.ts(Hh, Hh, 1e-30, None, op0=ALU.max, eng="gpsimd")
                            yield
                            S.act(Hh, Hh, AF.Sqrt)
                            yield
                            S.tt(U, U, Hh, ALU.mult, eng="gpsimd")
                            yield
                            S.scan(Hh, A, U, 0.0 if first else hprev[c][:, 0:1])
                            yield
                            S.copy(hprev[c][:, 0:1], Wk["H"][c][:, 511:512], eng="gpsimd")
                            S.tt(A, g_, g_, ALU.mult, eng="gpsimd")
                            yield
                            S.ts(A, A, 0.044715, 1.0, op0=ALU.mult, op1=ALU.add, eng="gpsimd")
                            yield
                            S.tt(A, A, g_, ALU.mult, eng="gpsimd")
                            yield
                            S.act(A, A, AF.Sigmoid, scale=1.5957691216057308)
                            yield
                            S.tt(xc, A, g_, ALU.mult, eng="gpsimd")
                            yield
                            S.tt(obg[c][:], Hh, xc, ALU.mult)
                            stq("sync", MIX[256 + c * 128:256 + (c + 1) * 128, tok0:tok0 + 512], obg[c][:])
                            yield
                n = 0
                rg = None
                for t in range(NT):
                    x_ = xb[t % 2]
                    ld("gpsimd", x_[:], XTv[:, :, t * 512:(t + 1) * 512])
                    for (col0, mw) in chunks:
                        pp = pps[n % 6]
                        sg = stg[n % 6]
                        for k in range(8):
                            S.mm(pp[0:mw, :], win[:, k, col0:col0 + mw], x_[:, k, :], start=(k == 0), stop=(k == 7))
                        dst = sg[0:mw, :]
                        store = True
                        if fuse_g and OFF_G <= col0 < OFF_G + 512:
                            ci_ = (col0 - OFF_G) // 128
                            dst = xbh[t % 2][:, ci_, 3:515] if ci_ < 2 else gtt[t % 2][:, ci_ - 2, :]
                            store = False
                        if n % 2 == 0:
                            S.copy(dst, pp[0:mw, :])
                        else:
                            S.act(dst, pp[0:mw, :], AF.Copy)
                        if store:
                            stq("sync", PRJ[col0:col0 + mw, t * 512:(t + 1) * 512], sg[0:mw, :])
                        n += 1
                        if rg is not None:
                            for g__ in rg:
                                next(g__, None)
                    if fuse_g:
                        if rg is not None:
                            for g__ in rg:
                                for _ in g__:
                                    pass
                        rg = [rg_tile_gen(t, 0), rg_tile_gen(t, 1)]
                        for g__ in rg:
                            next(g__, None)
                if rg is not None:
                    for g__ in rg:
                        for _ in g__:
                            pass

        def phase_zero_mix(rows0):
            with Ph() as ph:
                z = ph.sb("zz", [128, 2, 512], BF16)
                S.memset(z[:], 0.0)
                for t in range(NT):
                    stq("sync", MIX[rows0:rows0 + 256, t * 512:(t + 1) * 512].rearrange("(c p) t -> p c t", p=128), z[:])

        def gelu_tanh(ph, out, x, tmp):
            S.tt(tmp, x, x, ALU.mult, eng="gpsimd")
            S.ts(tmp, tmp, 0.044715, 1.0, op0=ALU.mult, op1=ALU.add, eng="gpsimd")
            S.tt(tmp, tmp, x, ALU.mult, eng="gpsimd")
            S.act(tmp, tmp, AF.Sigmoid, scale=1.5957691216057308)
            S.tt(out, tmp, x, ALU.mult, eng="gpsimd")

        def phase_rglru(l, s):
            t0 = s * seq
            with Ph() as ph:
                cw = ph.sb("cw", [128, 2, 4])
                for j in range(4):
                    for c in range(2):
                        ld("sync", cw[:, c, j:j + 1], D["g_conv_w"][l, j, c * 128:(c + 1) * 128].rearrange("(p o) -> p o", o=1))
                cb = ph.sb("cb", [128, 2]); colvec(cb, D["g_conv_b"][l])
                ba = ph.sb("ba", [128, 2]); colvec(ba, D["g_b_a"][l])
                bx = ph.sb("bx", [128, 2]); colvec(bx, D["g_b_x"][l])
                lam = ph.sb("lam", [128, 2]); colvec(lam, D["g_lambda"][l])
                c1 = ph.sb("c1", [128, 2])
                S.act(c1[:], lam[:], AF.Exp, scale=-1.0)
                S.act(c1[:], c1[:], AF.Ln, bias=1.0)
                S.ts(c1[:], c1[:], -8.0, op0=ALU.mult)
                bda = ph.sb("bda", [128, 2, 128]); bdx = ph.sb("bdx", [128, 2, 128])
                S.memset(bda[:], 0.0); S.memset(bdx[:], 0.0)
                for c in range(2):
                    for hh in range(2):
                        ld("sync", bda[hh * 64:(hh + 1) * 64, c, hh * 64:(hh + 1) * 64], D["g_w_a"][l, 2 * c + hh])
                        ld("sync", bdx[hh * 64:(hh + 1) * 64, c, hh * 64:(hh + 1) * 64], D["g_w_x"][l, 2 * c + hh])
                xb = ph.sb("gxb", [128, 2, seq + 3])
                S.memset(xb[:, :, 0:3], 0.0)
                gt = ph.sb("ggt", [128, 2, seq])
                for c in range(2):
                    ld("sync", xb[:, c, 3:3 + seq], PRJ[OFF_G + c * 128:OFF_G + (c + 1) * 128, t0:t0 + seq])
                    ld("sync", gt[:, c, :], PRJ[OFF_G + 256 + c * 128:OFF_G + 256 + (c + 1) * 128, t0:t0 + seq])
                xc = ph.sb("gxc", [128, 2, seq])
                A = ph.sb("gA", [128, 2, seq]); U = ph.sb("gU", [128, 2, seq]); Hh = ph.sb("gH", [128, 2, seq])
                ob = ph.sb("gob", [128, 2, seq], BF16)
                pps = [ph.ps(f"gp{i}", [128, 512]) for i in range(4)]
                for c in range(2):
                    S.ts(xc[:, c, :], xb[:, c, 3:3 + seq], cw[:, c, 3:4], cb[:, c:c + 1], op0=ALU.mult, op1=ALU.add)
                    for j in range(3):
                        S.stt(xc[:, c, :], xb[:, c, j:j + seq], cw[:, c, j:j + 1], xc[:, c, :], ALU.mult, ALU.add)
                n = 0
                for c in range(2):
                    for t in range(NTS):
                        sl = slice(t * 512, (t + 1) * 512)
                        pa = pps[n % 4]; px = pps[(n + 1) % 4]; n += 2
                        S.mm(pa[:], bda[:, c, :], xc[:, c, sl])
                        S.mm(px[:], bdx[:, c, :], xc[:, c, sl])
                        S.act(A[:, c, sl], pa[:], AF.Sigmoid, bias=ba[:, c:c + 1])
                        S.act(U[:, c, sl], px[:], AF.Sigmoid, bias=bx[:, c:c + 1])
                    S.act(A[:, c, :], A[:, c, :], AF.Exp, scale=c1[:, c:c + 1])
                    S.tt(U[:, c, :], U[:, c, :], xc[:, c, :], ALU.mult)
                    S.tt(Hh[:, c, :], A[:, c, :], A[:, c, :], ALU.mult)
                    S.ts(Hh[:, c, :], Hh[:, c, :], -1.0, 1.0, op0=ALU.mult, op1=ALU.add)
                    S.ts(Hh[:, c, :], Hh[:, c, :], 1e-30, None, op0=ALU.max)
                    S.act(Hh[:, c, :], Hh[:, c, :], AF.Sqrt)
                    S.tt(U[:, c, :], U[:, c, :], Hh[:, c, :], ALU.mult)
                    S.scan(Hh[:, c, :], A[:, c, :], U[:, c, :], 0.0)
                    gelu_tanh(ph, xc[:, c, :], gt[:, c, :], A[:, c, :])
                    S.tt(ob[:, c, :], Hh[:, c, :], xc[:, c, :], ALU.mult)
                    stq("sync", MIX[256 + c * 128:256 + (c + 1) * 128, t0:t0 + seq], ob[:, c, :])

        def phase_out(l):
            with Ph() as ph:
                wo = ph.sb("wo", [128, 8, DM], BF16)
                ld("gpsimd", wo[:], D["w_out"][l].rearrange("(k p) n -> p k n", p=128))
                g1 = ph.sb("g1", [128, 8]); colvec(g1, D["ln1_g"][l])
                b1 = ph.sb("b1", [128, 8]); colvec(b1, D["ln1_b"][l])
                ones = ph.sb("ones", [128, 128]); S.memset(ones[:], 1.0 / DM)
                mb = [ph.sb(f"mb{i}", [128, 8, 512], BF16) for i in range(2)]
                xf = [ph.sb(f"xf{i}", [128, 8, 512]) for i in range(2)]
                zz = [ph.sb(f"z{i}", [128, 8, 512]) for i in range(2)]
                for tz in zz + xf + mb:
                    S.split(tz, 512)
                sq = ph.sb("sq", [128, 8, 512]); mean = ph.sb("mean", [128, 512]); rstd = ph.sb("rstd", [128, 512])
                pps = [ph.ps(f"pp{i}", [128, 512]) for i in range(6)]
                pm = ph.ps("pm", [128, 512]); pq = ph.ps("pq", [128, 512])
                for t in range(NT):
                    sl = slice(t * 512, (t + 1) * 512)
                    m_ = mb[t % 2]; x_ = xf[t % 2]; z = zz[t % 2]
                    ld("sync", m_[:], MIXv[:, :, sl])
                    ld("sync", x_[:], XTv[:, :, sl])
                    for m in range(8):
                        pp = pps[(t * 8 + m) % 6]
                        for k in range(8):
                            S.mm(pp[:], wo[:, k, m * 128:(m + 1) * 128], m_[:, k, :], start=(k == 0), stop=(k == 7))
                        S.stt(z[:, m, :], x_[:, m, :], ALPHA, pp[:], ALU.mult, ALU.add)
                    layer_norm(ph, z, ones, g1, b1, pm, pq, sq, mean, rstd)
                    stq("sync", X1Tv[:, :, sl], z[:])

        def phase_ffn(l, last):
            BL = min(seq, 1024)
            NTB = BL // 512
            moe = (l % 2 == 1)
            li = l // 2
            with Ph() as ph:
                K = ph.consts(["c_ident"] + (["c_sel"] if moe else []))
                ident = K["c_ident"]
                x1b = ph.sb("x1b", [128, 8, BL], BF16)
                yacc = ph.sb("yacc", [128, 8, BL])
                S.split(x1b, 512); S.split(yacc, 512)
                wpe = ph.sb("wpe", [128, 2, DM], BF16)
                ld("gpsimd", wpe[:], D["pe_proj"][l].rearrange("(k p) n -> p k n", p=128))
                wg = ph.sb("wg", [128, 8, DM], BF16)
                ld("gpsimd", wg[:], D["pe_gate_w"][l].rearrange("(k p) n -> p k n", p=128))
                gb = ph.sb("gb", [128, 8]); colvec(gb, D["pe_gate_b"][l])
                g2 = ph.sb("g2", [128, 8]); colvec(g2, D["ln2_g"][l])
                b2 = ph.sb("b2", [128, 8]); colvec(b2, D["ln2_b"][l])
                ones = ph.sb("ones", [128, 128]); S.memset(ones[:], 1.0 / DM)
                if moe:
                    wr = ph.sb("wr", [128, 8, 8])
                    ld("sync", wr[:], D["e_router"][li].rearrange("(k p) e -> p k e", p=128))
                    gfm = ph.sb("gfm", [8, BL])
                    S.split(gfm, 512)
                    Ge = ph.sb("Ge", [128, BL])
                    S.split(Ge, 512)
                x1f = [ph.sb(f"x1f{i}", [128, 8, 512]) for i in range(1)] * 2
                pin = [ph.sb(f"pin{i}", [128, 4, 256]) for i in range(2)]
                pT = [ph.sb(f"pT{i}", [128, 2, 512], BF16) for i in range(2)]
                sgs = [ph.sb(f"sg{i}", [128, 512]) for i in range(2)]
                tmps = [ph.sb(f"tmp{i}", [128, 512]) for i in range(2)]
                pps = [ph.ps(f"pp{i}", [128, 512]) for i in range(6)]
                pm = ph.ps("pm", [128, 512]); pq = ph.ps("pq", [128, 512])
                for tz in x1f:
                    S.split(tz, 512)
                np_ = 0
                NWB = 3
                w1b = [ph.sb(f"w1b{i}", [128, 8, 256], BF16) for i in range(NWB)]
                w3b = [ph.sb(f"w3b{i}", [128, 8, 256], BF16) for i in range(NWB)]
                w2b = [ph.sb(f"w2b{i}", [128, 2, DM], BF16) for i in range(NWB)]
                acts = [ph.sb(f"act{i}", [128, 2, 512], BF16) for i in range(2)]
                s1s = [ph.sb(f"s1{i}", [128, 512]) for i in range(2)]
                t3s = [ph.sb(f"t3{i}", [128, 512]) for i in range(2)]
                sq = x1f[0]; mean = ph.sb("mean", [128, 512]); rstd = ph.sb("rstd", [128, 512])
                zt = [ph.sb(f"zt{i}", [128, 8, 512]) for i in range(1)] * 2
                ot = [ph.sb(f"ot{i}", [128, DM]) for i in range(2)]
                for s in range(T // BL):
                    t0 = s * BL
                    for t in range(NTB):
                        sl = slice(t * 512, (t + 1) * 512)
                        gsl = slice(t0 + t * 512, t0 + (t + 1) * 512)
                        xf_ = x1f[t % 2]; pi_ = pin[t % 2]; pT_ = pT[t % 2]
                        ld("sync", xf_[:], X1Tv[:, :, gsl])
                        ld("gpsimd", x1b[:, :, sl], X1Tv[:, :, gsl])
                        ld("sync", pi_[:], D["p"][l, gsl, :].rearrange("(j p) f -> p j f", p=128))
                        for kk in range(2):
                            pt = pps[np_ % 6]; np_ += 1
                            for j in range(4):
                                S.transpose(pt[:, j * 128:(j + 1) * 128], pi_[:, j, kk * 128:(kk + 1) * 128], ident[:])
                            S.copy(pT_[:, kk, :], pt[:])
                        for m in range(8):
                            pg = pps[np_ % 6]; np_ += 1
                            pp = pps[np_ % 6]; np_ += 1
                            sg = sgs[m % 2]; tmp = tmps[m % 2]
                            for k in range(8):
                                S.mm(pg[:], wg[:, k, m * 128:(m + 1) * 128], x1b[:, k, sl], start=(k == 0), stop=(k == 7))
                            S.act(sg[:], pg[:], AF.Sigmoid, bias=gb[:, m:m + 1])
                            for kk in range(2):
                                S.mm(pp[:], wpe[:, kk, m * 128:(m + 1) * 128], pT_[:, kk, :], start=(kk == 0), stop=(kk == 1))
                            S.tt(tmp[:], pp[:], sg[:], ALU.mult)
                            S.stt(yacc[:, m, sl], xf_[:, m, :], ALPHA, tmp[:], ALU.mult, ALU.add)
                        if moe:
                            for j in range(4):
                                pl = pps[np_ % 6]; np_ += 1
                                for k in range(8):
                                    S.mm(pl[:, 0:8], xf_[:, k, j * 128:(j + 1) * 128], wr[:, k, :], start=(k == 0), stop=(k == 7))
                                lgs = ph.sb(f"lgs{s}_{t}_{j}", [128, 8]); mx = ph.sb(f"mx{s}_{t}_{j}", [128, 8])
                                dd = ph.sb(f"dd{s}_{t}_{j}", [128, 4]); ga = ph.sb(f"ga{s}_{t}_{j}", [128, 8]); gb_ = ph.sb(f"gbb{s}_{t}_{j}", [128, 8])
                                S.copy(lgs[:], pl[:, 0:8])
                                S.op("vector", lambda e, a=mx, b=lgs: e.max(a[:], b[:]), reads=[lgs[:]], writes=[mx[:]])
                                S.tt(dd[:, 0:1], mx[:, 0:1], mx[:, 1:2], ALU.subtract)
                                S.act(dd[:, 1:2], dd[:, 0:1], AF.Sigmoid)
                                S.act(dd[:, 2:3], dd[:, 0:1], AF.Sigmoid, scale=-1.0)
                                S.ts(ga[:], lgs[:], mx[:, 0:1], dd[:, 1:2], op0=ALU.is_equal, op1=ALU.mult)
                                S.ts(gb_[:], lgs[:], mx[:, 1:2], dd[:, 2:3], op0=ALU.is_equal, op1=ALU.mult)
                                S.tt(ga[:], ga[:], gb_[:], ALU.add)
                                pt = pps[np_ % 6]; np_ += 1
                                S.transpose(pt[0:8, 0:128], ga[:], ident[:])
                                S.copy(gfm[:, t * 512 + j * 128:t * 512 + (j + 1) * 128], pt[0:8, 0:128])
                    NE = 8 if moe else 1
                    gi = 0
                    hsets = [(pps[0], pps[1]), (pps[2], pps[3])]
                    ybanks = [pps[4], pps[5], pm, pq]
                    cnt = {"h": 0, "y": 0, "u": 0}
                    pend = [None]

                    def emit_H(w1, w3, sl, ac):
                        for c in range(2):
                            p1, p3 = hsets[cnt["h"] % 2]; cnt["h"] += 1
                            for k in range(8):
                                S.mm(p1[:], w1[:, k, c * 128:(c + 1) * 128], x1b[:, k, sl], start=(k == 0), stop=(k == 7))
                            for k in range(8):
                                S.mm(p3[:], w3[:, k, c * 128:(c + 1) * 128], x1b[:, k, sl], start=(k == 0), stop=(k == 7))
                            s1 = s1s[c]
                            S.act(s1[:], p1[:], AF.Silu)
                            if moe:
                                t3 = t3s[c]
                                S.tt(t3[:], p3[:], s1[:], ALU.mult)
                                S.tt(ac[:, c, :], t3[:], Ge[:, sl], ALU.mult)
                            else:
                                S.tt(ac[:, c, :], p3[:], s1[:], ALU.mult)

                    def emit_Y(w2, sl, ac):
                        for m in range(8):
                            py = ybanks[cnt["y"] % 4]; cnt["y"] += 1
                            for c in range(2):
                                S.mm(py[:], w2[:, c, m * 128:(m + 1) * 128], ac[:, c, :], start=(c == 0), stop=(c == 1))
                            S.tt(yacc[:, m, sl], yacc[:, m, sl], py[:], ALU.add)

                    for e in range(NE):
                        if moe:
                            W1 = D["e_w1"][li, e]; W3 = D["e_w3"][li, e]; W2 = D["e_w2"][li, e]
                            for t in range(NTB):
                                sl = slice(t * 512, (t + 1) * 512)
                                pb = ybanks[cnt["y"] % 4]; cnt["y"] += 1
                                S.mm(pb[:], K["c_sel"][:, e, :], gfm[:, sl])
                                S.act(Ge[:, sl], pb[:], AF.Copy)
                        else:
                            W1 = D["f_w1"][li]; W3 = D["f_w3"][li]; W2 = D["f_w2"][li]
                        W1v = W1.rearrange("(k p) f -> p k f", p=128)
                        W3v = W3.rearrange("(k p) f -> p k f", p=128)
                        W2v = W2.rearrange("(k p) n -> p k n", p=128)
                        for g in range(11):
                            w1 = w1b[gi % NWB]; w3 = w3b[gi % NWB]; w2 = w2b[gi % NWB]; gi += 1
                            ld("gpsimd", w1[:], W1v[:, :, g * 256:(g + 1) * 256])
                            ld("gpsimd", w3[:], W3v[:, :, g * 256:(g + 1) * 256])
                            ld("gpsimd", w2[:], W2v[:, 2 * g:2 * g + 2, :])
                            for t in range(NTB):
                                sl = slice(t * 512, (t + 1) * 512)
                                ac = acts[cnt["u"] % 2]; cnt["u"] += 1
                                emit_H(w1, w3, sl, ac)
                                if pend[0] is not None:
                                    emit_Y(*pend[0])
                                pend[0] = (w2, sl, ac)
                    if pend[0] is not None:
                        emit_Y(*pend[0]); pend[0] = None
                    for t in range(NTB):
                        sl = slice(t * 512, (t + 1) * 512)
                        gsl = slice(t0 + t * 512, t0 + (t + 1) * 512)
                        z = zt[t % 2]
                        S.copy(z[:], yacc[:, :, sl], eng="gpsimd")
                        layer_norm(ph, z, ones, g2, b2, pm, pq, sq, mean, rstd)
                        if not last:
                            stq("sync", XTv[:, :, gsl], z[:])
                        else:
                            for j in range(4):
                                o_ = ot[j % 2]
                                for hh in range(2):
                                    pt = pps[np_ % 6]; np_ += 1
                                    for c4 in range(4):
                                        c = hh * 4 + c4
                                        S.transpose(pt[:, c4 * 128:(c4 + 1) * 128], z[:, c, j * 128:(j + 1) * 128], ident[:])
                                    S.copy(o_[:, hh * 512:(hh + 1) * 512], pt[:])
                                stq("sync", out[t0 + t * 512 + j * 128:t0 + t * 512 + (j + 1) * 128, :], o_[:])


        RMS_EPS = 1e-6

        def bcast_row(ph, name, row_ap, n):
            t = ph.sb(name, [128, n])
            ld("sync", t[:], row_ap.broadcast_to([128, n]))
            return t

        def chunk_consts(ph):
            K = ph.consts(["c_ident", "c_triu", "c_strl"])
            ones = ph.sb("ones1", [128, 128])
            S.memset(ones[:], 1.0)
            return K["c_ident"], K["c_triu"], K["c_strl"], ones

        def phase_ssd(l, s):
            t0 = s * seq
            NCH = seq // 128
            with Ph() as ph:
                ident, triu, strl, ones = chunk_consts(ph)
                cw = ph.sb("cw", [128, 4, 4])
                for j in range(4):
                    for c in range(4):
                        ld("sync", cw[:, c, j:j + 1], D["s_conv_w"][l, j, c * 128:(c + 1) * 128].rearrange("(p o) -> p o", o=1))
                cb = ph.sb("cb", [128, 4]); colvec(cb, D["s_conv_b"][l])
                nw = ph.sb("nw", [128, 2]); colvec(nw, D["s_norm_w"][l])
                dtb = ph.sb("dtb", [4, 1]); ld("sync", dtb[:], D["s_dt_bias"][l].rearrange("(p o) -> p o", o=1))
                nA = bcast_row(ph, "nA", D["s_a_log"][l:l + 1, :], 4)
                S.act(nA[:], nA[:], AF.Exp)
                S.ts(nA[:], nA[:], -1.0, None, op0=ALU.mult)
                dsk = bcast_row(ph, "dsk", D["s_d"][l:l + 1, :], 4)
                z = ph.sb("z", [128, 2, seq])
                xbc = ph.sb("xbc", [128, 4, seq + 3])
                S.memset(xbc[:, :, 0:3], 0.0)
                dtf = ph.sb("dtf", [4, seq])
                S.split(dtf, 128)
                for c in range(2):
                    ld("sync", z[:, c, :], PRJ[OFF_S + c * 128:OFF_S + (c + 1) * 128, t0:t0 + seq])
                for c in range(4):
                    ld("sync", xbc[:, c, 3:3 + seq], PRJ[OFF_S + 256 + c * 128:OFF_S + 256 + (c + 1) * 128, t0:t0 + seq])
                ld("sync", dtf[:], PRJ[OFF_S + 768:OFF_S + 772, t0:t0 + seq])
                xs = ph.sb("xs", [128, 4, seq])
                S.split(xs, 128)
                for c in range(4):
                    S.ts(xs[:, c, :], xbc[:, c, 3:3 + seq], cw[:, c, 3:4], cb[:, c:c + 1], op0=ALU.mult, op1=ALU.add)
                    for j in range(3):
                        S.stt(xs[:, c, :], xbc[:, c, j:j + seq], cw[:, c, j:j + 1], xs[:, c, :], ALU.mult, ALU.add)
                    S.act(xs[:, c, :], xs[:, c, :], AF.Silu)
                S.act(dtf[:], dtf[:], AF.Exp, bias=dtb[:, 0:1])
                S.act(dtf[:], dtf[:], AF.Ln, bias=1.0)
                ysb = ph.sb("ysb", [128, 2, seq])
                S.split(ysb, 128)
                Sst = ph.sb("Sst", [128, 2, 64])
                S.split(Sst, 64)
                S.memset(Sst[:], 0.0)
                pA = ph.ps("pA", [128, 512]); pB = ph.ps("pB", [128, 512])
                pC = [ph.ps(f"pC{i}", [128, 512]) for i in range(2)]
                pD = [ph.ps(f"pD{i}", [128, 512]) for i in range(2)]
                pE = ph.ps("pE", [128, 512])
                def T2(name, shape, dt=F32, gran=None):
                    r = [ph.sb(f"{name}{i}", shape, dt) for i in range(2)]
                    if gran:
                        for t_ in r:
                            S.split(t_, gran)
                    return r
                dtT = T2("dtT", [128, 4]); adt = T2("adt", [128, 4]); acs = T2("acs", [128, 8])
                eacs = T2("eacs", [128, 4]); dec = T2("dec", [128, 4]); eat = T2("eat", [128, 4])
                xtok = T2("xtok", [128, 4, 64], gran=64); btok = T2("btok", [128, 128])
                xdt = T2("xdt", [128, 4, 64], gran=64); xdd = T2("xdd", [128, 4, 64], gran=64)
                Sm = T2("Sm", [128, 2, 128], gran=128); Lh = T2("Lh", [128, 4, 128], gran=128)
                E = T2("E", [128, 4, 128], gran=128); P = T2("P", [128, 4, 128], gran=128)
                ytok = T2("ytok", [128, 4, 64], gran=64)
                for ci in range(NCH):
                    b = ci % 2
                    cs = slice(ci * 128, (ci + 1) * 128)
                    S.transpose(pB[:, 256:260], dtf[0:4, cs], ident[0:4, 0:4])
                    S.copy(dtT[b][:], pB[:, 256:260])
                    S.tt(adt[b][:], dtT[b][:], nA[:], ALU.mult)
                    S.mm(pB[:, 264:268], triu[:], adt[b][:])
                    S.mm(pB[:, 268:272], ones[:], adt[b][:])
                    S.copy(acs[b][:], pB[:, 264:272])
                    S.act(eacs[b][:], acs[b][:, 0:4], AF.Exp)
                    S.act(eat[b][:], acs[b][:, 4:8], AF.Exp)
                    S.tt(dec[b][:], acs[b][:, 4:8], acs[b][:, 0:4], ALU.subtract)
                    S.act(dec[b][:], dec[b][:], AF.Exp)
                    for cx in range(2):
                        S.transpose(pA[:, cx * 128:(cx + 1) * 128], xs[:, cx, cs], ident[:])
                    S.transpose(pA[:, 256:384], xs[:, 2, cs], ident[:])
                    S.copy(xtok[b][:].rearrange("p h d -> p (h d)"), pA[:, 0:256])
                    S.act(btok[b][:], pA[:, 256:384], AF.Copy)
                    S.tt(xdt[b][:], xtok[b][:], dtT[b][:].unsqueeze(2).broadcast_to([128, 4, 64]), ALU.mult)
                    S.tt(xdd[b][:], xdt[b][:], dec[b][:].unsqueeze(2).broadcast_to([128, 4, 64]), ALU.mult, eng="gpsimd")
                    for g in range(2):
                        gs = slice(g * 64, (g + 1) * 64)
                        S.mm(pB[:, g * 128:(g + 1) * 128], xs[gs, 2, cs], xs[gs, 3, cs], sync_self=True)
                        S.tt(Sm[b][:, g, :], pB[:, g * 128:(g + 1) * 128], triu[:], ALU.mult)
                    for h in range(4):
                        S.ts(Lh[b][:, h, :], strl[:], adt[b][:, h:h + 1], None, op0=ALU.mult, eng="gpsimd")
                        S.mm(pC[b][:, h * 128:(h + 1) * 128], Lh[b][:, h, :], triu[:])
                    S.act(E[b][:].rearrange("p h l -> p (h l)"), pC[b][:], AF.Exp)
                    for h in range(4):
                        g = h // 2; j = h % 2
                        gs = slice(g * 64, (g + 1) * 64)
                        S.tt(P[b][:, h, :], E[b][:, h, :], Sm[b][:, g, :], ALU.mult, eng=("gpsimd" if h % 2 else "vector"))
                        S.mm(pD[b][:, h * 64:(h + 1) * 64], P[b][:, h, :], xdt[b][:, h, :])
                        S.mm(pD[b][:, 256 + h * 64:256 + (h + 1) * 64], xs[gs, 3, cs], Sst[gs, j, :], sync_self=True)
                        S.mm(pE[gs, j * 64:(j + 1) * 64], btok[b][:, gs], xdd[b][:, h, :])
                        S.ts(ytok[b][:, h, :], pD[b][:, 256 + h * 64:256 + (h + 1) * 64], eacs[b][:, h:h + 1], None, op0=ALU.mult)
                        S.tt(ytok[b][:, h, :], ytok[b][:, h, :], pD[b][:, h * 64:(h + 1) * 64], ALU.add)
                        S.stt(ytok[b][:, h, :], xtok[b][:, h, :], dsk[:, h:h + 1], ytok[b][:, h, :], ALU.mult, ALU.add)
                        S.stt(Sst[gs, j, :], Sst[gs, j, :], eat[b][gs, h:h + 1], pE[gs, j * 64:(j + 1) * 64], ALU.mult, ALU.add)
                    for cx in range(2):
                        S.transpose(pE[:, 128 + cx * 128:128 + (cx + 1) * 128],
                                    ytok[b][:, 2 * cx:2 * cx + 2, :].rearrange("p h d -> p (h d)"), ident[:])
                        S.act(ysb[:, cx, cs], pE[:, 128 + cx * 128:128 + (cx + 1) * 128], AF.Copy)
                ob = ph.sb("ob", [128, 2, seq], BF16)
                rs = [ph.sb(f"rs{i}", [128, 512]) for i in range(2)]
                for c in range(2):
                    S.act(z[:, c, :], z[:, c, :], AF.Silu)
                    S.tt(ysb[:, c, :], ysb[:, c, :], z[:, c, :], ALU.mult)
                    S.tt(z[:, c, :], ysb[:, c, :], ysb[:, c, :], ALU.mult, eng="gpsimd")
                    for t in range(NTS):
                        sl = slice(t * 512, (t + 1) * 512)
                        pp = pC[t % 2]
                        S.mm(pp[:], ones[:], z[:, c, sl])
                        r_ = rs[t % 2]
                        S.act(r_[:], pp[:], AF.Ln, scale=1.0 / 128.0, bias=RMS_EPS)
                        S.act(r_[:], r_[:], AF.Exp, scale=-0.5)
                        S.tt(r_[:], r_[:], ysb[:, c, sl], ALU.mult)
                        S.ts(ob[:, c, sl], r_[:], nw[:, c:c + 1], None, op0=ALU.mult)
                    stq("sync", MIX[768 + c * 128:768 + (c + 1) * 128, t0:t0 + seq], ob[:, c, :])


        def phase_mlstm(l, s):
            t0 = s * seq
            NCH = seq // 128
            with Ph() as ph:
                ident, triu, strl, ones = chunk_consts(ph)
                m125 = ph.sb("m125", [128, 128])
                S.ts(m125[:], triu[:], 0.125, None, op0=ALU.mult)
                ib = ph.sb("ib", [4, 1]); ld("sync", ib[:], D["m_i_bias"][l].rearrange("(p o) -> p o", o=1))
                nfb = ph.sb("nfb", [4, 1]); ld("sync", nfb[:], D["m_f_bias"][l].rearrange("(p o) -> p o", o=1))
                S.ts(nfb[:], nfb[:], -1.0, None, op0=ALU.mult)
                nw = ph.sb("nw", [128, 2]); colvec(nw, D["m_norm_w"][l])
                q = ph.sb("q", [128, 2, seq]); k = ph.sb("k", [128, 2, seq]); v = ph.sb("v", [128, 2, seq])
                o = ph.sb("o", [128, 2, seq])
                i_fm = ph.sb("i_fm", [4, seq]); f_fm = ph.sb("f_fm", [4, seq])
                hsb = ph.sb("hsb", [128, 2, seq])
                for t_ in (q, k, v, i_fm, f_fm, hsb):
                    S.split(t_, 128)
                for c in range(2):
                    for j_, tt_ in enumerate((q, k, v, o)):
                        ld("sync", tt_[:, c, :], PRJ[j_ * 256 + c * 128:j_ * 256 + (c + 1) * 128, t0:t0 + seq])
                ld("sync", i_fm[:], PRJ[1024:1028, t0:t0 + seq])
                ld("sync", f_fm[:], PRJ[1028:1032, t0:t0 + seq])
                S.ts(i_fm[:], i_fm[:], ib[:, 0:1], None, op0=ALU.add)
                S.act(f_fm[:], f_fm[:], AF.Exp, scale=-1.0, bias=nfb[:, 0:1])
                S.act(f_fm[:], f_fm[:], AF.Ln, bias=1.0)
                S.ts(f_fm[:], f_fm[:], -1.0, None, op0=ALU.mult)
                Cst = ph.sb("Cst", [128, 2, 66])
                S.split(Cst, 66)
                S.memset(Cst[:], 0.0)
                pA = ph.ps("pA", [128, 512]); pB = ph.ps("pB", [128, 512]); pS = ph.ps("pS", [128, 512])
                pC = ph.ps("pC", [128, 512]); pD = ph.ps("pD", [128, 512]); pD2 = ph.ps("pD2", [128, 512])
                pE = ph.ps("pE", [128, 512])

                def T2(name, shape, dt=F32, gran=None):
                    r = [ph.sb(f"{name}{i}", shape, dt) for i in range(2)]
                    if gran:
                        for t_ in r:
                            S.split(t_, gran)
                    return r
                lifT = T2("lifT", [128, 8]); acs = T2("acs", [128, 8]); eb = T2("eb", [128, 4]); eg = T2("eg", [128, 4])
                wd = T2("wd", [128, 4]); ktok = T2("ktok", [128, 4, 64], gran=64)
                vt1 = T2("vt1", [128, 4, 66], gran=66); vw = T2("vw", [128, 4, 66], gran=66)
                for b in range(2):
                    S.memset(vt1[b][:], 0.0)
                    S.memset(vt1[b][:, :, 64:65], 1.0)
                Lh = T2("Lh", [128, 4, 128], gran=128); E = T2("E", [128, 4, 128], gran=128)
                Sm = T2("Sm", [128, 4, 128], gran=128); P = T2("P", [128, 4, 128], gran=128)
                tot = T2("tot", [128, 4, 66], gran=66); dd = T2("dd", [128, 8]); hn = T2("hn", [128, 4, 64], gran=64)
                sq = T2("sq", [128, 4, 64]); ss = T2("ss", [128, 4])
                for ci in range(NCH):
                    b = ci % 2
                    cs = slice(ci * 128, (ci + 1) * 128)
                    S.transpose(pB[:, 256:260], i_fm[0:4, cs], ident[0:4, 0:4])
                    S.transpose(pB[:, 260:264], f_fm[0:4, cs], ident[0:4, 0:4])
                    S.copy(lifT[b][:], pB[:, 256:264])
                    S.mm(pB[:, 264:268], triu[:], lifT[b][:, 4:8])
                    S.mm(pB[:, 268:272], ones[:], lifT[b][:, 4:8])
                    S.copy(acs[b][:], pB[:, 264:272])
                    S.act(eb[b][:], acs[b][:, 0:4], AF.Exp)
                    S.act(eg[b][:], acs[b][:, 4:8], AF.Exp)
                    S.tt(wd[b][:], acs[b][:, 4:8], acs[b][:, 0:4], ALU.subtract)
                    S.tt(wd[b][:], wd[b][:], lifT[b][:, 0:4], ALU.add)
                    S.act(wd[b][:], wd[b][:], AF.Exp)
                    S.ts(wd[b][:], wd[b][:], 0.125, None, op0=ALU.mult)
                    for cx in range(2):
                        S.transpose(pA[:, cx * 128:(cx + 1) * 128], k[:, cx, cs], ident[:])
                        S.transpose(pA[:, 256 + cx * 128:256 + (cx + 1) * 128], v[:, cx, cs], ident[:])
                    S.copy(ktok[b][:].rearrange("p h d -> p (h d)"), pA[:, 0:256])
                    S.act(vt1[b][:, :, 0:64], pA[:, 256:512].rearrange("p (h d) -> p h d", h=4), AF.Copy)
                    S.tt(vw[b][:], vt1[b][:], wd[b][:].unsqueeze(2).broadcast_to([128, 4, 66]), ALU.mult, eng="gpsimd")
                    for h in range(4):
                        hc = h // 2
                        hs = slice((h % 2) * 64, (h % 2) * 64 + 64)
                        S.mm(pS[:, h * 128:(h + 1) * 128], k[hs, hc, cs], q[hs, hc, cs], sync_self=True)
                        S.ts(Lh[b][:, h, :], strl[:], lifT[b][:, 4 + h:5 + h], None, op0=ALU.mult, eng="gpsimd")
                        S.mm(pC[:, h * 128:(h + 1) * 128], Lh[b][:, h, :], triu[:])
                    for h in range(4):
                        S.act(E[b][:, h, :], pC[:, h * 128:(h + 1) * 128], AF.Exp, bias=lifT[b][:, h:h + 1])
                        S.tt(Sm[b][:, h, :], pS[:, h * 128:(h + 1) * 128], m125[:], ALU.mult)
                        S.tt(P[b][:, h, :], E[b][:, h, :], Sm[b][:, h, :], ALU.mult, eng="gpsimd")
                    for h in range(4):
                        hc = h // 2
                        hs = slice((h % 2) * 64, (h % 2) * 64 + 64)
                        S.mm(pD[:, h * 66:(h + 1) * 66], P[b][:, h, :], vt1[b][:, h, :])
                        S.mm(pD2[:, h * 66:(h + 1) * 66], q[hs, hc, cs], Cst[hs, hc, :], sync_self=True)
                        S.mm(pE[hs, hc * 66:(hc + 1) * 66], ktok[b][:, h, :], vw[b][:, h, :])
                    for h in range(4):
                        hc = h // 2
                        hs = slice((h % 2) * 64, (h % 2) * 64 + 64)
                        S.ts(tot[b][:, h, :], pD2[:, h * 66:(h + 1) * 66], eb[b][:, h:h + 1], None, op0=ALU.mult)
                        S.tt(tot[b][:, h, :], tot[b][:, h, :], pD[:, h * 66:(h + 1) * 66], ALU.add)
                        S.stt(Cst[hs, hc, :], Cst[hs, hc, :], eg[b][hs, h:h + 1], pE[hs, hc * 66:(hc + 1) * 66], ALU.mult, ALU.add)
                    den = tot[b][:, :, 64:65].rearrange("p h o -> p (h o)")
                    S.stt(dd[b][:, 0:4], den, -1.0, den, ALU.mult, ALU.max)
                    S.ts(dd[b][:, 0:4], dd[b][:, 0:4], 1.0, None, op0=ALU.max)
                    S.recip(dd[b][:, 4:8], dd[b][:, 0:4])
                    S.tt(hn[b][:], tot[b][:, :, 0:64], dd[b][:, 4:8].unsqueeze(2).broadcast_to([128, 4, 64]), ALU.mult)
                    S.tt(sq[b][:], hn[b][:], hn[b][:], ALU.mult, eng="gpsimd")
                    S.reduce(ss[b][:], sq[b][:], ALU.add, AX.X)
                    S.act(ss[b][:], ss[b][:], AF.Ln, scale=1.0 / 64.0, bias=RMS_EPS)
                    S.act(ss[b][:], ss[b][:], AF.Exp, scale=-0.5)
                    S.tt(hn[b][:], hn[b][:], ss[b][:].unsqueeze(2).broadcast_to([128, 4, 64]), ALU.mult)
                    for cx in range(2):
                        S.transpose(pA[:, cx * 128:(cx + 1) * 128],
                                    hn[b][:, 2 * cx:2 * cx + 2, :].rearrange("p h d -> p (h d)"), ident[:])
                        S.act(hsb[:, cx, cs], pA[:, cx * 128:(cx + 1) * 128], AF.Copy)
                ob = ph.sb("ob", [128, 2, seq], BF16)
                for c in range(2):
                    S.act(o[:, c, :], o[:, c, :], AF.Sigmoid)
                    S.tt(hsb[:, c, :], hsb[:, c, :], o[:, c, :], ALU.mult)
                    S.ts(ob[:, c, :], hsb[:, c, :], nw[:, c:c + 1], None, op0=ALU.mult)
                    stq("sync", MIX[c * 128:(c + 1) * 128, t0:t0 + seq], ob[:, c, :])


        def phase_rwkv(l, s):
            t0 = s * seq
            CH = 64
            NCH = seq // CH
            with Ph() as ph:
                ident, triu, strl, ones = chunk_consts(ph)
                bdo = ph.sb("bdo", [128, 128])
                S.memset(bdo[:], 0.0); S.memset(bdo[0:64, 0:64], 1.0); S.memset(bdo[64:128, 64:128], 1.0)
                mk1 = ph.sb("mk1", [64, 128]); mk2 = ph.sb("mk2", [64, 128])
                S.copy(mk1[:, 64:128], triu[0:64, 0:64])
                S.tt(mk1[:, 0:64], triu[0:64, 0:64], ident[0:64, 0:64], ALU.subtract)
                S.copy(mk2[:, 0:64], mk1[:, 0:64])
                S.ts(mk2[:, 64:128], mk1[:, 64:128], -1.0, None, op0=ALU.mult)
                def cv(name, vec, C=2):
                    t_ = ph.sb(name, [128, C]); colvec(t_, vec); return t_
                mu = cv("mu", D["r_mu"][l], 7)
                omm = ph.sb("omm", [128, 7]); S.ts(omm[:], mu[:], -1.0, 1.0, op0=ALU.mult, op1=ALU.add)
                w0c = cv("w0c", D["r_w0"][l]); a0c = cv("a0c", D["r_a0"][l]); kkc = cv("kkc", D["r_k_k"][l])
                kac = cv("kac", D["r_k_a"][l]); rkc = cv("rkc", D["r_r_k"][l].rearrange("h d -> (h d)"))
                lnw = cv("lnw", D["r_ln_w"][l]); lnb = cv("lnb", D["r_ln_b"][l])
                omka = ph.sb("omka", [128, 2]); S.ts(omka[:], kac[:], -1.0, 1.0, op0=ALU.mult, op1=ALU.add)
                W2p = ph.sb("W2p", [128, 2, 128]); A2p = ph.sb("A2p", [128, 2, 128]); G2p = ph.sb("G2p", [128, 2, 128])
                for t_ in (W2p, A2p, G2p):
                    S.memset(t_[:], 0.0)
                for c in range(2):
                    ld("sync", W2p[0:32, c, :], D["r_w2"][l][:, c * 128:(c + 1) * 128])
                    ld("sync", A2p[32:64, c, :], D["r_a2"][l][:, c * 128:(c + 1) * 128])
                    ld("sync", G2p[64:128, c, :], D["r_g2"][l][:, c * 128:(c + 1) * 128])
                RC = ph.sb("RC", [128, 7, seq + 1])
                S.gran[RC.name] = 64
                S.memset(RC[:, :, 0:1], 0.0)
                for c in range(7):
                    ld("sync", RC[:, c, 1:seq + 1], PRJ[OFF_R + c * 128:OFF_R + (c + 1) * 128, t0:t0 + seq])
                tmp = ph.sb("tmp", [128, seq])
                for c in range(7):
                    S.ts(tmp[:], RC[:, c, 0:seq], mu[:, c:c + 1], None, op0=ALU.mult, eng=("gpsimd" if c % 2 else "vector"))
                    S.stt(RC[:, c, 1:seq + 1], RC[:, c, 1:seq + 1], omm[:, c:c + 1], tmp[:], ALU.mult, ALU.add)
                R_ = lambda c: RC[:, c, 1:seq + 1]
                T6 = R_(6)
                S.act(RC[0:32, 6, 1:seq + 1], RC[0:32, 6, 1:seq + 1], AF.Tanh)
                S.act(RC[64:128, 6, 1:seq + 1], RC[64:128, 6, 1:seq + 1], AF.Sigmoid)
                KK = ph.sb("KK", [128, 2, seq]); Bb = ph.sb("Bb", [128, 2, seq]); LD = ph.sb("LD", [128, 2, seq])
                Gg = ph.sb("Gg", [128, 2, seq]); ysb = ph.sb("ysb", [128, 2, seq])
                for t_ in (KK, Bb, LD, ysb):
                    S.split(t_, 64)
                pb = [ph.ps(f"b{i}", [128, 512]) for i in range(8)]
                rn = [ph.sb(f"rn{i}", [128, 512]) for i in range(2)]
                n = 0
                for c in range(2):
                    for t in range(NTS):
                        sl = slice(t * 512, (t + 1) * 512)
                        sl1 = slice(1 + t * 512, 1 + (t + 1) * 512)
                        pw = pb[n % 8]; pa = pb[(n + 1) % 8]; pg = pb[(n + 2) % 8]; n += 3
                        S.mm(pw[:], W2p[:, c, :], RC[:, 6, sl1])
                        S.mm(pa[:], A2p[:, c, :], RC[:, 6, sl1])
                        S.mm(pg[:], G2p[:, c, :], RC[:, 6, sl1])
                        S.act(LD[:, c, sl], pw[:], AF.Sigmoid, bias=w0c[:, c:c + 1])
                        S.act(Bb[:, c, sl], pa[:], AF.Sigmoid, bias=a0c[:, c:c + 1])
                        S.copy(Gg[:, c, sl], pg[:])
                    S.ts(LD[:, c, :], LD[:, c, :], -0.6065306597126334, None, op0=ALU.mult)
                    S.ts(KK[:, c, :], R_(2 + c), kkc[:, c:c + 1], None, op0=ALU.mult)
                    S.tt(tmp[:], KK[:, c, :], KK[:, c, :], ALU.mult, eng="gpsimd")
                    for t in range(NTS):
                        sl = slice(t * 512, (t + 1) * 512)
                        pp = pb[n % 8]; n += 1
                        S.mm(pp[:], bdo[:], tmp[:, sl])
                        r_ = rn[t % 2]
                        S.ts(r_[:], pp[:], 1e-24, None, op0=ALU.max)
                        S.act(r_[:], r_[:], AF.Ln)
                        S.act(r_[:], r_[:], AF.Exp, scale=-0.5)
                        S.tt(KK[:, c, sl], KK[:, c, sl], r_[:], ALU.mult)
                    S.ts(tmp[:], Bb[:, c, :], kac[:, c:c + 1], omka[:, c:c + 1], op0=ALU.mult, op1=ALU.add)
                    S.tt(R_(2 + c), R_(2 + c), tmp[:], ALU.mult)
                    S.tt(Bb[:, c, :], Bb[:, c, :], KK[:, c, :], ALU.mult, eng="gpsimd")
                Z = ph.sb("Z", [64, 4, 64]); S.split(Z, 64); S.memset(Z[:], 0.0)
                TM = ph.sb("TM", [64, 6, 256]); S.split(TM, 256)
                G4 = ph.sb("G4", [64, 4, 256]); S.split(G4, 256)
                TT = ph.sb("TT", [64, 6, 256]); S.split(TT, 256)
                RH = ph.sb("RH", [64, 4, 128]); LH = ph.sb("LH", [64, 4, 128])
                gC = ph.sb("gC", [64, 8])
                A12 = ph.sb("A12", [64, 4, 128]); MA3 = ph.sb("MA3", [64, 4, 128])
                MN = [ph.sb(f"MN{i}", [64, 4, 128]) for i in range(2)]
                PQ = ph.sb("PQ", [64, 4, 128])
                RHS = ph.sb("RHS", [64, 256]); Us = ph.sb("Us", [64, 256])
                yt = ph.sb("yt", [64, 4, 64]); ysq = ph.sb("ysq", [64, 4, 64]); st4 = ph.sb("st4", [64, 8])
                i64 = ident[0:64, 0:64]
                srcs = [(0, 0), (0, 1), (1, 2), (1, 3), (2, 4), (2, 5)]
                for ci in range(NCH):
                    cs = slice(ci * CH, (ci + 1) * CH)
                    cs1 = slice(1 + ci * CH, 1 + (ci + 1) * CH)
                    for cx in range(2):
                        S.transpose(pb[0][0:64, cx * 128:(cx + 1) * 128], RC[:, 0 + cx, cs1], ident[:])
                        S.transpose(pb[0][0:64, 256 + cx * 128:256 + (cx + 1) * 128], RC[:, 2 + cx, cs1], ident[:])
                        S.transpose(pb[1][0:64, cx * 128:(cx + 1) * 128], RC[:, 4 + cx, cs1], ident[:])
                        S.transpose(pb[1][0:64, 256 + cx * 128:256 + (cx + 1) * 128], KK[:, cx, cs], ident[:])
                        S.transpose(pb[2][0:64, cx * 128:(cx + 1) * 128], Bb[:, cx, cs], ident[:])
                        S.transpose(pb[2][0:64, 256 + cx * 128:256 + (cx + 1) * 128], LD[:, cx, cs], ident[:])
                    S.copy(TM[:, 0:2, :].rearrange("p a f -> p (a f)"), pb[0][0:64, :])
                    S.act(TM[:, 2:4, :].rearrange("p a f -> p (a f)"), pb[1][0:64, :], AF.Copy)
                    S.copy(TM[:, 4:6, :].rearrange("p a f -> p (a f)"), pb[2][0:64, :])
                    S.mm(pb[3][0:64, 0:256], triu[0:64, 0:64], TM[:, 5, :])
                    S.mm(pb[3][0:64, 256:512], ones[0:64, 0:64], TM[:, 5, :])
                    S.act(G4[:, 0, :], pb[3][0:64, 0:256], AF.Exp)
                    S.act(G4[:, 1, :], pb[3][0:64, 0:256], AF.Exp, scale=-1.0)
                    S.tt(G4[:, 2, :], pb[3][0:64, 0:256], TM[:, 5, :], ALU.subtract)
                    S.act(G4[:, 2, :], G4[:, 2, :], AF.Exp)
                    S.act(G4[:, 3, :], pb[3][0:64, 0:256], AF.Copy)
                    S.tt(G4[:, 3, :], pb[3][0:64, 256:512], G4[:, 3, :], ALU.subtract)
                    S.act(G4[:, 3, :], G4[:, 3, :], AF.Exp)
                    S.tt(TT[:, 0, :], TM[:, 1, :], G4[:, 1, :], ALU.mult)
                    S.tt(TT[:, 1, :], TM[:, 4, :], G4[:, 1, :], ALU.mult, eng="gpsimd")
                    S.tt(TT[:, 2, :], TM[:, 3, :], G4[:, 2, :], ALU.mult)
                    S.tt(TT[:, 3, :], TM[:, 0, :], G4[:, 0, :], ALU.mult, eng="gpsimd")
                    S.tt(TT[:, 4, :], TM[:, 1, :], G4[:, 3, :], ALU.mult)
                    S.stt(TT[:, 5, :], TM[:, 4, :], -1.0, G4[:, 3, :], ALU.mult, ALU.mult)
                    for h in range(4):
                        hs = slice(h * 64, (h + 1) * 64)
                        S.transpose(pb[4][0:64, h * 128:h * 128 + 64], TT[:, 2, hs], i64)
                        S.transpose(pb[4][0:64, h * 128 + 64:(h + 1) * 128], TT[:, 3, hs], i64)
                        S.transpose(pb[5][0:64, h * 128:h * 128 + 64], TT[:, 0, hs], i64)
                        S.transpose(pb[5][0:64, h * 128 + 64:(h + 1) * 128], TT[:, 1, hs], i64)
                        S.mm(pb[6][0:64, 2 * h:2 * h + 2], TM[:, 5, hs], ones[0:64, 0:2])
                    S.copy(RH[:].rearrange("p h f -> p (h f)"), pb[4][0:64, :])
                    S.act(LH[:].rearrange("p h f -> p (h f)"), pb[5][0:64, :], AF.Copy)
                    S.act(gC[:], pb[6][0:64, 0:8], AF.Exp)
                    for h in range(4):
                        S.mm(pb[0][0:64, h * 128:(h + 1) * 128], LH[:, h, 0:64], RH[:, h, :])
                        S.mm(pb[1][0:64, h * 128:(h + 1) * 128], LH[:, h, 64:128], RH[:, h, :])
                        S.mm(pb[2][0:64, h * 64:(h + 1) * 64], RH[:, h, 0:64], LH[:, h, 64:128])
                    S.tt(A12[:], pb[0][0:64, :].rearrange("p (h f) -> p h f", h=4), mk1[:].unsqueeze(1).broadcast_to([64, 4, 128]), ALU.mult)
                    S.tt(MA3[:], pb[1][0:64, :].rearrange("p (h f) -> p h f", h=4), mk2[:].unsqueeze(1).broadcast_to([64, 4, 128]), ALU.mult)
                    S.copy(MN[0][:, :, 0:64], MA3[:, :, 0:64], eng="gpsimd")
                    S.tt(MN[0][:, :, 64:128], pb[2][0:64, 0:256].rearrange("p (h f) -> p h f", h=4),
                         strl[0:64, 0:64].unsqueeze(1).broadcast_to([64, 4, 64]), ALU.mult)
                    S.tt(PQ[:, :, 0:64], i64.unsqueeze(1).broadcast_to([64, 4, 64]), MN[0][:, :, 0:64], ALU.subtract)
                    S.tt(PQ[:, :, 64:128], i64.unsqueeze(1).broadcast_to([64, 4, 64]), MN[0][:, :, 64:128], ALU.subtract)
                    cur = 0
                    for lev in range(5):
                        a_, b_ = MN[cur], MN[1 - cur]
                        for h in range(4):
                            S.mm(pb[7][0:64, h * 128:h * 128 + 64], a_[:, h, 64:128], a_[:, h, 0:64])
                            S.mm(pb[7][0:64, h * 128 + 64:(h + 1) * 128], a_[:, h, 0:64], a_[:, h, 64:128])
                        S.copy(b_[:].rearrange("p h f -> p (h f)"), pb[7][0:64, :])
                        for h in range(4):
                            S.mm(pb[6][0:64, h * 128:h * 128 + 64], PQ[:, h, 64:128], b_[:, h, 0:64])
                            S.mm(pb[6][0:64, h * 128 + 64:(h + 1) * 128], b_[:, h, 0:64], PQ[:, h, 64:128])
                        S.tt(PQ[:].rearrange("p h f -> p (h f)"), PQ[:].rearrange("p h f -> p (h f)"), pb[6][0:64, :], ALU.add)
                        cur = 1 - cur
                    for h in range(4):
                        hs = slice(h * 64, (h + 1) * 64)
                        S.mm(pb[3][0:64, hs], RH[:, h, 0:64], Z[:, h, :], start=True, stop=False)
                        S.mm(pb[3][0:64, hs], A12[:, h, 0:64], TM[:, 2, hs], start=False, stop=True)
                    S.copy(RHS[:], pb[3][0:64, 0:256])
                    for h in range(4):
                        hs = slice(h * 64, (h + 1) * 64)
                        S.mm(pb[4][0:64, hs], PQ[:, h, 0:64], RHS[:, hs])
                    S.copy(Us[:], pb[4][0:64, 0:256])
                    for h in range(4):
                        hs = slice(h * 64, (h + 1) * 64)
                        S.mm(pb[5][0:64, hs], RH[:, h, 64:128], Z[:, h, :], start=True, stop=False)
                        S.mm(pb[5][0:64, hs], A12[:, h, 64:128], TM[:, 2, hs], start=False, stop=False)
                        S.mm(pb[5][0:64, hs], MA3[:, h, 64:128], Us[:, hs], start=False, stop=True)
                    for h in range(4):
                        hs = slice(h * 64, (h + 1) * 64)
                        S.mm(pb[0][0:64, hs], TT[:, 4, hs], TM[:, 2, hs], start=True, stop=False)
                        S.mm(pb[0][0:64, hs], TT[:, 5, hs], Us[:, hs], start=False, stop=True)
                    for h in range(4):
                        hs = slice(h * 64, (h + 1) * 64)
                        S.stt(Z[:, h, :], Z[:, h, :], gC[:, 2 * h:2 * h + 1], pb[0][0:64, hs], ALU.mult, ALU.add)
                    S.copy(yt[:].rearrange("p h d -> p (h d)"), pb[5][0:64, 0:256])
                    S.reduce(st4[:, 0:4], yt[:], ALU.add, AX.X)
                    S.ts(st4[:, 0:4], st4[:, 0:4], 1.0 / 64.0, None, op0=ALU.mult)
                    S.tt(yt[:], yt[:], st4[:, 0:4].unsqueeze(2).broadcast_to([64, 4, 64]), ALU.subtract)
                    S.tt(ysq[:], yt[:], yt[:], ALU.mult, eng="gpsimd")
                    S.reduce(st4[:, 4:8], ysq[:], ALU.add, AX.X)
                    S.act(st4[:, 4:8], st4[:, 4:8], AF.Ln, scale=1.0 / 64.0, bias=64e-5)
                    S.act(st4[:, 4:8], st4[:, 4:8], AF.Exp, scale=-0.5)
                    S.tt(yt[:], yt[:], st4[:, 4:8].unsqueeze(2).broadcast_to([64, 4, 64]), ALU.mult)
                    for cx in range(2):
                        S.transpose(pb[1][:, cx * 64:(cx + 1) * 64], yt[:, 2 * cx:2 * cx + 2, :].rearrange("p h d -> p (h d)"), i64)
                        S.act(ysb[:, cx, cs], pb[1][:, cx * 64:(cx + 1) * 64], AF.Copy)
                ob = ph.sb("ob", [128, 2, seq], BF16)
                for c in range(2):
                    S.ts(ysb[:, c, :], ysb[:, c, :], lnw[:, c:c + 1], lnb[:, c:c + 1], op0=ALU.mult, op1=ALU.add)
                    S.tt(tmp[:], R_(0 + c), R_(2 + c), ALU.mult)
                    S.ts(tmp[:], tmp[:], rkc[:, c:c + 1], None, op0=ALU.mult)
                    for t in range(NTS):
                        sl = slice(t * 512, (t + 1) * 512)
                        sl1 = slice(1 + t * 512, 1 + (t + 1) * 512)
                        pp = pb[t % 8]
                        S.mm(pp[:], bdo[:], tmp[:, sl])
                        r_ = rn[t % 2]
                        S.tt(r_[:], pp[:], RC[:, 4 + c, sl1], ALU.mult)
                        S.tt(ysb[:, c, sl], ysb[:, c, sl], r_[:], ALU.add)
                    S.tt(ob[:, c, :], ysb[:, c, :], Gg[:, c, :], ALU.mult)
                    stq("sync", MIX[512 + c * 128:512 + (c + 1) * 128, t0:t0 + seq], ob[:, c, :])


        def rr(gens):
            gens = list(gens)
            while gens:
                for g_ in list(gens):
                    try:
                        next(g_)
                    except StopIteration:
                        gens.remove(g_)

        def phase_rwkv2(l):
            CH = 64
            BLK = 512
            NB = seq // BLK
            NCB = BLK // CH
            with Ph() as ph:
                ident, triu, strl, ones = chunk_consts(ph)
                bdo = ph.sb("bdo", [128, 128])
                S.memset(bdo[:], 0.0); S.memset(bdo[0:64, 0:64], 1.0); S.memset(bdo[64:128, 64:128], 1.0)
                mk1 = ph.sb("mk1", [64, 128]); mk2 = ph.sb("mk2", [64, 128])
                S.copy(mk1[:, 64:128], triu[0:64, 0:64])
                S.tt(mk1[:, 0:64], triu[0:64, 0:64], ident[0:64, 0:64], ALU.subtract)
                S.copy(mk2[:, 0:64], mk1[:, 0:64])
                S.ts(mk2[:, 64:128], mk1[:, 64:128], -1.0, None, op0=ALU.mult)
                def cv(name, vec, C=2):
                    t_ = ph.sb(name, [128, C]); colvec(t_, vec); return t_
                mu = cv("mu", D["r_mu"][l], 7)
                omm = ph.sb("omm", [128, 7]); S.ts(omm[:], mu[:], -1.0, 1.0, op0=ALU.mult, op1=ALU.add)
                w0c = cv("w0c", D["r_w0"][l]); a0c = cv("a0c", D["r_a0"][l]); kkc = cv("kkc", D["r_k_k"][l])
                kac = cv("kac", D["r_k_a"][l]); rkc = cv("rkc", D["r_r_k"][l].rearrange("h d -> (h d)"))
                lnw = cv("lnw", D["r_ln_w"][l]); lnb = cv("lnb", D["r_ln_b"][l])
                omka = ph.sb("omka", [128, 2]); S.ts(omka[:], kac[:], -1.0, 1.0, op0=ALU.mult, op1=ALU.add)
                W2p = ph.sb("W2p", [128, 2, 128]); A2p = ph.sb("A2p", [128, 2, 128]); G2p = ph.sb("G2p", [128, 2, 128])
                for t_ in (W2p, A2p, G2p):
                    S.memset(t_[:], 0.0)
                for c in range(2):
                    ld("sync", W2p[0:32, c, :], D["r_w2"][l][:, c * 128:(c + 1) * 128])
                    ld("sync", A2p[32:64, c, :], D["r_a2"][l][:, c * 128:(c + 1) * 128])
                    ld("sync", G2p[64:128, c, :], D["r_g2"][l][:, c * 128:(c + 1) * 128])
                i64 = ident[0:64, 0:64]
                F32R = mybir.dt.float32r
                Rr = lambda ap: ap.bitcast(F32R)
                triuR = ph.sb("triuR", [64, 64], F32R); S.copy(triuR[:], triu[0:64, 0:64])
                onesR = ph.sb("onesR", [64, 64], F32R); S.copy(onesR[:], ones[0:64, 0:64])

                def mkchain(q):
                    C = Ctx()
                    n_ = lambda nm: f"{nm}q{q}"
                    C.pb = [ph.ps(n_(f"b{i}"), [128, 512]) for i in range(4)]
                    C.RC = ph.sb(n_("RC"), [128, 7, BLK + 1]); S.gran[C.RC.name] = 64
                    C.tmp = ph.sb(n_("tmp"), [128, BLK])
                    C.KK = ph.sb(n_("KK"), [128, 2, BLK]); C.Bb = ph.sb(n_("Bb"), [128, 2, BLK])
                    C.LD = ph.sb(n_("LD"), [128, 2, BLK]); C.Gg = ph.sb(n_("Gg"), [128, 2, BLK])
                    C.ysb = ph.sb(n_("ysb"), [128, 2, BLK]); C.ob = ph.sb(n_("ob"), [128, 2, BLK], BF16)
                    for t_ in (C.KK, C.Bb, C.LD, C.ysb):
                        S.split(t_, 64)
                    C.rn = ph.sb(n_("rn"), [128, 512])
                    C.Z = ph.sb(n_("Z"), [64, 4, 64]); S.split(C.Z, 64)
                    for hh_ in range(2):
                        S.ts(C.Z[:, 2 * hh_:2 * hh_ + 2, :].rearrange("p h d -> p (h d)").bitcast(mybir.dt.float32r), ones[0:64, 0:128], 0.0, None, op0=ALU.mult)
                    C.TM = [ph.sb(n_(f"TM{i}"), [64, 6, 256]) for i in range(2)]; [S.split(t_, 256) for t_ in C.TM]
                    C.G4 = ph.sb(n_("G4"), [64, 4, 256]); S.split(C.G4, 256)
                    C.TT = [ph.sb(n_(f"TT{i}"), [64, 6, 256]) for i in range(2)]; [S.split(t_, 256) for t_ in C.TT]
                    C.RH = [ph.sb(n_(f"RH{i}"), [64, 4, 128]) for i in range(2)]; C.LH = ph.sb(n_("LH"), [64, 4, 128])
                    C.gC = [ph.sb(n_(f"gC{i}"), [64, 8]) for i in range(2)]
                    C.A12 = [ph.sb(n_(f"A12{i}"), [64, 4, 128]) for i in range(2)]; C.MA3 = [ph.sb(n_(f"MA3{i}"), [64, 4, 128]) for i in range(2)]
                    C.MN = [ph.sb(n_(f"MN{i}"), [64, 4, 128]) for i in range(2)]
                    C.PQ = [ph.sb(n_(f"PQ{i}"), [64, 4, 128]) for i in range(2)]
                    C.RHS = ph.sb(n_("RHS"), [64, 256]); C.Us = ph.sb(n_("Us"), [64, 256])
                    C.yt = ph.sb(n_("yt"), [64, 4, 64]); C.ysq = ph.sb(n_("ysq"), [64, 4, 64]); C.st4 = ph.sb(n_("st4"), [64, 8])
                    return C

                def zip_rr(subs):
                    subs = list(subs)
                    while subs:
                        for g_ in list(subs):
                            try:
                                next(g_)
                            except StopIteration:
                                subs.remove(g_)
                        yield

                def prep_gen(C, ci, p):
                    cs = slice(ci * CH, (ci + 1) * CH)
                    cs1 = slice(1 + ci * CH, 1 + (ci + 1) * CH)
                    RC, KK, Bb, LD, ysb = C.RC, C.KK, C.Bb, C.LD, C.ysb
                    pA, pB, pC, pD = C.pb
                    Z, G4, LH, MN = C.Z, C.G4, C.LH, C.MN
                    TM, TT, RH, gC, A12, MA3, PQ = C.TM[p], C.TT[p], C.RH[p], C.gC[p], C.A12[p], C.MA3[p], C.PQ[p]
                    RHS, Us, yt, ysq, st4 = C.RHS, C.Us, C.yt, C.ysq, C.st4
                    for cx in range(2):
                        S.transpose(pA[0:64, cx * 128:(cx + 1) * 128], RC[:, 0 + cx, cs1], ident[:])
                        S.transpose(pA[0:64, 256 + cx * 128:256 + (cx + 1) * 128], RC[:, 2 + cx, cs1], ident[:])
                        S.transpose(pB[0:64, cx * 128:(cx + 1) * 128], RC[:, 4 + cx, cs1], ident[:])
                        S.transpose(pB[0:64, 256 + cx * 128:256 + (cx + 1) * 128], KK[:, cx, cs], ident[:])
                        S.transpose(pC[0:64, cx * 128:(cx + 1) * 128], Bb[:, cx, cs], ident[:])
                        S.transpose(pC[0:64, 256 + cx * 128:256 + (cx + 1) * 128], LD[:, cx, cs], ident[:])
                    yield
                    S.copy(Rr(TM[:, 0:2, :].rearrange("p a f -> p (a f)")), pA[0:64, :])
                    S.act(Rr(TM[:, 2:4, :].rearrange("p a f -> p (a f)")), pB[0:64, :], AF.Copy)
                    S.act(Rr(TM[:, 4:6, :].rearrange("p a f -> p (a f)")), pC[0:64, :], AF.Copy)
                    yield
                    S.mm(pA[0:64, 0:256], triuR[:], Rr(TM[:, 5, :]))
                    S.mm(pA[0:64, 256:512], onesR[:], Rr(TM[:, 5, :]))
                    yield
                    S.act(G4[:, 0, :], pA[0:64, 0:256], AF.Exp)
                    S.act(G4[:, 1, :], pA[0:64, 0:256], AF.Exp, scale=-1.0)
                    S.tt(G4[:, 2, :], pA[0:64, 0:256], TM[:, 5, :], ALU.subtract)
                    yield
                    S.act(G4[:, 3, :], pA[0:64, 0:256], AF.Copy)
                    S.act(G4[:, 2, :], G4[:, 2, :], AF.Exp)
                    S.tt(G4[:, 3, :], pA[0:64, 256:512], G4[:, 3, :], ALU.subtract)
                    yield
                    S.act(G4[:, 3, :], G4[:, 3, :], AF.Exp)
                    S.tt(Rr(TT[:, 0, :]), TM[:, 1, :], G4[:, 1, :], ALU.mult, eng="gpsimd")
                    S.tt(Rr(TT[:, 1, :]), TM[:, 4, :], G4[:, 1, :], ALU.mult, eng="gpsimd")
                    yield
                    S.tt(Rr(TT[:, 2, :]), TM[:, 3, :], G4[:, 2, :], ALU.mult)
                    S.tt(Rr(TT[:, 3, :]), TM[:, 0, :], G4[:, 0, :], ALU.mult, eng="gpsimd")
                    yield
                    S.tt(Rr(TT[:, 4, :]), TM[:, 1, :], G4[:, 3, :], ALU.mult, eng="gpsimd")
                    S.stt(Rr(TT[:, 5, :]), TM[:, 4, :], -1.0, G4[:, 3, :], ALU.mult, ALU.mult)
                    yield
                    for h in range(4):
                        hs = slice(h * 64, (h + 1) * 64)
                        S.transpose(pB[0:64, h * 128:h * 128 + 64], TT[:, 2, hs], i64)
                        S.transpose(pB[0:64, h * 128 + 64:(h + 1) * 128], TT[:, 3, hs], i64)
                        S.transpose(pC[0:64, h * 128:h * 128 + 64], TT[:, 0, hs], i64)
                        S.transpose(pC[0:64, h * 128 + 64:(h + 1) * 128], TT[:, 1, hs], i64)
                        S.mm(pA[0:64, 2 * h:2 * h + 2], Rr(TM[:, 5, hs]), onesR[:, 0:2])
                    yield
                    S.act(Rr(RH[:].rearrange("p h f -> p (h f)")), pB[0:64, :], AF.Copy)
                    S.act(Rr(LH[:].rearrange("p h f -> p (h f)")), pC[0:64, :], AF.Copy)
                    S.act(gC[:], pA[0:64, 0:8], AF.Exp)
                    yield
                    for h in range(4):
                        S.mm(pA[0:64, h * 128:(h + 1) * 128], Rr(LH[:, h, 0:64]), Rr(RH[:, h, :]))
                        S.mm(pB[0:64, h * 128:(h + 1) * 128], Rr(LH[:, h, 64:128]), Rr(RH[:, h, :]))
                        S.mm(pC[0:64, h * 64:(h + 1) * 64], Rr(RH[:, h, 0:64]), Rr(LH[:, h, 64:128]))
                    yield
                    S.tt(Rr(A12[:]), pA[0:64, :].rearrange("p (h f) -> p h f", h=4), mk1[:].unsqueeze(1).broadcast_to([64, 4, 128]), ALU.mult)
                    S.tt(Rr(MA3[:]), pB[0:64, :].rearrange("p (h f) -> p h f", h=4), mk2[:].unsqueeze(1).broadcast_to([64, 4, 128]), ALU.mult)
                    yield
                    S.copy(Rr(MN[0][:, :, 0:64]), MA3[:, :, 0:64], eng="gpsimd")
                    S.tt(Rr(MN[0][:, :, 64:128]), pC[0:64, 0:256].rearrange("p (h f) -> p h f", h=4),
                         strl[0:64, 0:64].unsqueeze(1).broadcast_to([64, 4, 64]), ALU.mult)
                    yield
                    S.tt(Rr(PQ[:, :, 0:64]), i64.unsqueeze(1).broadcast_to([64, 4, 64]), MN[0][:, :, 0:64], ALU.subtract, eng="gpsimd")
                    S.tt(Rr(PQ[:, :, 64:128]), i64.unsqueeze(1).broadcast_to([64, 4, 64]), MN[0][:, :, 64:128], ALU.subtract)
                    yield
                    cur = 0
                    for lev in range(5):
                        a_, b_ = MN[cur], MN[1 - cur]
                        for h in range(4):
                            S.mm(pA[0:64, h * 128:h * 128 + 64], Rr(a_[:, h, 64:128]), Rr(a_[:, h, 0:64]))
                            S.mm(pA[0:64, h * 128 + 64:(h + 1) * 128], Rr(a_[:, h, 0:64]), Rr(a_[:, h, 64:128]))
                        yield
                        S.act(Rr(b_[:].rearrange("p h f -> p (h f)")), pA[0:64, :], AF.Copy)
                        yield
                        for h in range(4):
                            S.mm(pB[0:64, h * 128:h * 128 + 64], Rr(PQ[:, h, 64:128]), Rr(b_[:, h, 0:64]))
                            S.mm(pB[0:64, h * 128 + 64:(h + 1) * 128], Rr(b_[:, h, 0:64]), Rr(PQ[:, h, 64:128]))
                        yield
                        S.tt(Rr(PQ[:].rearrange("p h f -> p (h f)")), PQ[:].rearrange("p h f -> p (h f)"), pB[0:64, :], ALU.add)
                        yield
                        cur = 1 - cur

                def state_gen(C, ci, p):
                    cs = slice(ci * CH, (ci + 1) * CH)
                    cs1 = slice(1 + ci * CH, 1 + (ci + 1) * CH)
                    RC, KK, Bb, LD, ysb = C.RC, C.KK, C.Bb, C.LD, C.ysb
                    pA, pB, pC, pD = C.pb
                    Z, G4, LH, MN = C.Z, C.G4, C.LH, C.MN
                    TM, TT, RH, gC, A12, MA3, PQ = C.TM[p], C.TT[p], C.RH[p], C.gC[p], C.A12[p], C.MA3[p], C.PQ[p]
                    RHS, Us, yt, ysq, st4 = C.RHS, C.Us, C.yt, C.ysq, C.st4
                    for h in range(4):
                        hs = slice(h * 64, (h + 1) * 64)
                        S.mm(pD[0:64, hs], Rr(RH[:, h, 0:64]), Rr(Z[:, h, :]), start=True, stop=False)
                        S.mm(pD[0:64, hs], Rr(A12[:, h, 0:64]), Rr(TM[:, 2, hs]), start=False, stop=True)
                    yield
                    S.act(Rr(RHS[:]), pD[0:64, 0:256], AF.Copy)
                    yield
                    for h in range(4):
                        hs = slice(h * 64, (h + 1) * 64)
                        S.mm(pD[0:64, 256 + h * 64:256 + (h + 1) * 64], Rr(PQ[:, h, 0:64]), Rr(RHS[:, hs]))
                    yield
                    S.act(Rr(Us[:]), pD[0:64, 256:512], AF.Copy)
                    yield
                    for h in range(4):
                        hs = slice(h * 64, (h + 1) * 64)
                        S.mm(pD[0:64, hs], Rr(RH[:, h, 64:128]), Rr(Z[:, h, :]), start=True, stop=False)
                        S.mm(pD[0:64, hs], Rr(A12[:, h, 64:128]), Rr(TM[:, 2, hs]), start=False, stop=False)
                        S.mm(pD[0:64, hs], Rr(MA3[:, h, 64:128]), Rr(Us[:, hs]), start=False, stop=True)
                    for h in range(4):
                        hs = slice(h * 64, (h + 1) * 64)
                        S.mm(pD[0:64, 256 + h * 64:256 + (h + 1) * 64], Rr(TT[:, 4, hs]), Rr(TM[:, 2, hs]), start=True, stop=False)
                        S.mm(pD[0:64, 256 + h * 64:256 + (h + 1) * 64], Rr(TT[:, 5, hs]), Rr(Us[:, hs]), start=False, stop=True)
                    yield
                    for h in range(4):
                        hs = slice(h * 64, (h + 1) * 64)
                        S.stt(Rr(Z[:, h, :]), Z[:, h, :], gC[:, 2 * h:2 * h + 1], pD[0:64, 256 + h * 64:256 + (h + 1) * 64], ALU.mult, ALU.add)
                    S.act(yt[:].rearrange("p h d -> p (h d)"), pD[0:64, 0:256], AF.Copy)
                    yield
                    S.reduce(st4[:, 0:4], yt[:], ALU.add, AX.X)
                    yield
                    S.stt(yt[:], st4[:, 0:4].unsqueeze(2).broadcast_to([64, 4, 64]), -1.0 / 64.0, yt[:], ALU.mult, ALU.add)
                    yield
                    S.tt(ysq[:], yt[:], yt[:], ALU.mult, eng="gpsimd")
                    yield
                    S.reduce(st4[:, 4:8], ysq[:], ALU.add, AX.X)
                    yield
                    S.act(st4[:, 4:8], st4[:, 4:8], AF.Ln, scale=1.0 / 64.0, bias=64e-5)
                    S.act(st4[:, 4:8], st4[:, 4:8], AF.Exp, scale=-0.5)
                    yield
                    S.tt(yt[:], yt[:], st4[:, 4:8].unsqueeze(2).broadcast_to([64, 4, 64]), ALU.mult, eng="gpsimd")
                    yield
                    for cx in range(2):
                        S.transpose(pD[:, cx * 64:(cx + 1) * 64], yt[:, 2 * cx:2 * cx + 2, :].rearrange("p h d -> p (h d)"), i64)
                    yield
                    for cx in range(2):
                        S.act(ysb[:, cx, cs], pD[:, cx * 64:(cx + 1) * 64], AF.Copy)
                    yield

                def block_gen(C, q, blk):
                    g0 = q * seq + blk * BLK
                    RC, tmp, KK, Bb, LD, Gg, ysb, ob, rn_ = C.RC, C.tmp, C.KK, C.Bb, C.LD, C.Gg, C.ysb, C.ob, C.rn
                    pA, pB, pC, pD = C.pb
                    RHS, Us, yt, ysq, st4 = C.RHS, C.Us, C.yt, C.ysq, C.st4
                    R_ = lambda c: RC[:, c, 1:BLK + 1]
                    if blk == 0:
                        S.memset(RC[:, :, 0:1], 0.0)
                        for c in range(7):
                            ld("sync", RC[:, c, 1:BLK + 1], PRJ[OFF_R + c * 128:OFF_R + (c + 1) * 128, g0:g0 + BLK])
                    else:
                        for c in range(7):
                            ld("sync", RC[:, c, 0:BLK + 1], PRJ[OFF_R + c * 128:OFF_R + (c + 1) * 128, g0 - 1:g0 + BLK])
                    yield
                    for c in range(7):
                        S.ts(tmp[:], RC[:, c, 0:BLK], mu[:, c:c + 1], None, op0=ALU.mult, eng="gpsimd")
                        S.stt(RC[:, c, 1:BLK + 1], RC[:, c, 1:BLK + 1], omm[:, c:c + 1], tmp[:], ALU.mult, ALU.add)
                        yield
                    S.act(RC[0:32, 6, 1:BLK + 1], RC[0:32, 6, 1:BLK + 1], AF.Tanh)
                    S.act(RC[64:128, 6, 1:BLK + 1], RC[64:128, 6, 1:BLK + 1], AF.Sigmoid)
                    yield
                    sl = slice(0, BLK)
                    sl1 = slice(1, BLK + 1)
                    for c in range(2):
                        S.mm(pA[:], W2p[:, c, :], RC[:, 6, sl1])
                        S.mm(pB[:], A2p[:, c, :], RC[:, 6, sl1])
                        S.mm(pC[:], G2p[:, c, :], RC[:, 6, sl1])
                        S.act(LD[:, c, sl], pA[:], AF.Sigmoid, bias=w0c[:, c:c + 1])
                        S.act(Bb[:, c, sl], pB[:], AF.Sigmoid, bias=a0c[:, c:c + 1])
                        S.copy(Gg[:, c, sl], pC[:])
                        yield
                        S.ts(LD[:, c, :], LD[:, c, :], -0.6065306597126334, None, op0=ALU.mult, eng="gpsimd")
                        S.ts(KK[:, c, :], R_(2 + c), kkc[:, c:c + 1], None, op0=ALU.mult)
                        S.tt(tmp[:], KK[:, c, :], KK[:, c, :], ALU.mult, eng="gpsimd")
                        yield
                        S.mm(pD[:], bdo[:], tmp[:, sl])
                        S.ts(rn_[:], pD[:], 1e-24, None, op0=ALU.max)
                        S.act(rn_[:], rn_[:], AF.Ln)
                        S.act(rn_[:], rn_[:], AF.Exp, scale=-0.5)
                        yield
                        S.tt(KK[:, c, sl], KK[:, c, sl], rn_[:], ALU.mult)
                        S.ts(tmp[:], Bb[:, c, :], kac[:, c:c + 1], omka[:, c:c + 1], op0=ALU.mult, op1=ALU.add)
                        S.tt(R_(2 + c), R_(2 + c), tmp[:], ALU.mult)
                        S.tt(Bb[:, c, :], Bb[:, c, :], KK[:, c, :], ALU.mult, eng="gpsimd")
                        yield
                    yield from prep_gen(C, 0, 0)
                    for ci in range(NCB):
                        subs = [state_gen(C, ci, ci % 2)]
                        if ci + 1 < NCB:
                            subs.append(prep_gen(C, ci + 1, (ci + 1) % 2))
                        yield from zip_rr(subs)
                    for c in range(2):
                        S.ts(ysb[:, c, :], ysb[:, c, :], lnw[:, c:c + 1], lnb[:, c:c + 1], op0=ALU.mult, op1=ALU.add)
                        S.tt(tmp[:], R_(0 + c), R_(2 + c), ALU.mult, eng="gpsimd")
                        S.ts(tmp[:], tmp[:], rkc[:, c:c + 1], None, op0=ALU.mult, eng="gpsimd")
                        yield
                        S.mm(pD[:], bdo[:], tmp[:, sl])
                        S.tt(rn_[:], pD[:], RC[:, 4 + c, sl1], ALU.mult)
                        S.tt(ysb[:, c, sl], ysb[:, c, sl], rn_[:], ALU.add)
                        yield
                        S.tt(ob[:, c, :], ysb[:, c, :], Gg[:, c, :], ALU.mult)
                        stq("sync", MIX[512 + c * 128:512 + (c + 1) * 128, g0:g0 + BLK], ob[:, c, :])
                        yield

                chains = [mkchain(q) for q in range(2)]
                for blk in range(NB):
                    rr([block_gen(chains[q], q, blk) for q in range(2)])


        def phase_ssd2(l):
            NCH = seq // 128
            with Ph() as ph:
                ident, triu, strl, ones = chunk_consts(ph)
                cw = ph.sb("cw", [128, 4, 4])
                for j in range(4):
                    for c in range(4):
                        ld("sync", cw[:, c, j:j + 1], D["s_conv_w"][l, j, c * 128:(c + 1) * 128].rearrange("(p o) -> p o", o=1))
                cb = ph.sb("cb", [128, 4]); colvec(cb, D["s_conv_b"][l])
                nw = ph.sb("nw", [128, 2]); colvec(nw, D["s_norm_w"][l])
                dtb = ph.sb("dtb", [4, 1]); ld("sync", dtb[:], D["s_dt_bias"][l].rearrange("(p o) -> p o", o=1))
                nA = bcast_row(ph, "nA", D["s_a_log"][l:l + 1, :], 4)
                S.act(nA[:], nA[:], AF.Exp)
                S.ts(nA[:], nA[:], -1.0, None, op0=ALU.mult)
                dsk = bcast_row(ph, "dsk", D["s_d"][l:l + 1, :], 4)

                def mkchain(q):
                    C = Ctx()
                    n_ = lambda nm: f"{nm}q{q}"
                    C.pb = [ph.ps(n_(f"P{i}"), [128, 512]) for i in range(4)]
                    C.xs = ph.sb(n_("xs"), [128, 4, seq]); S.split(C.xs, 128)
                    C.ysb = ph.sb(n_("ysb"), [128, 2, seq]); S.split(C.ysb, 128)
                    C.dtf = ph.sb(n_("dtf"), [4, seq]); S.split(C.dtf, 128)
                    C.tmpc = ph.sb(n_("tmpc"), [128, seq + 3])
                    C.ob = ph.sb(n_("ob"), [128, seq], BF16)
                    C.rs = ph.sb(n_("rs"), [128, 512])
                    C.Sst = ph.sb(n_("Sst"), [128, 2, 64]); S.split(C.Sst, 64); S.memset(C.Sst[:], 0.0)
                    for nm, shp, gr in (("dtT", [128, 4], None), ("adt", [128, 4], None), ("acs", [128, 8], None),
                                        ("eacs", [128, 4], None), ("dec", [128, 4], None), ("eat", [128, 4], None),
                                        ("xtok", [128, 4, 64], 64), ("btok", [128, 128], None), ("xdt", [128, 4, 64], 64),
                                        ("xdd", [128, 4, 64], 64), ("Sm", [128, 2, 128], 128), ("Lh", [128, 4, 128], 128),
                                        ("E", [128, 4, 128], 128), ("P", [128, 4, 128], 128), ("ytok", [128, 4, 64], 64)):
                        t_ = ph.sb(n_(nm), shp)
                        if gr:
                            S.split(t_, gr)
                        setattr(C, nm, t_)
                    return C

                def chain_gen(C, q):
                    t0 = q * seq
                    P0, P1, P2, P3 = C.pb
                    xs, ysb, dtf, tmpc, ob, rs, Sst = C.xs, C.ysb, C.dtf, C.tmpc, C.ob, C.rs, C.Sst
                    dtT, adt, acs, eacs, dec, eat = C.dtT, C.adt, C.acs, C.eacs, C.dec, C.eat
                    xtok, btok, xdt, xdd, Sm, Lh, E, P, ytok = C.xtok, C.btok, C.xdt, C.xdd, C.Sm, C.Lh, C.E, C.P, C.ytok
                    S.memset(tmpc[:, 0:3], 0.0)
                    ld("sync", dtf[:], PRJ[OFF_S + 768:OFF_S + 772, t0:t0 + seq])
                    for c in range(4):
                        ld("sync", tmpc[:, 3:3 + seq], PRJ[OFF_S + 256 + c * 128:OFF_S + 256 + (c + 1) * 128, t0:t0 + seq])
                        yield
                        S.ts(xs[:, c, :], tmpc[:, 3:3 + seq], cw[:, c, 3:4], cb[:, c:c + 1], op0=ALU.mult, op1=ALU.add)
                        yield
                        for j in range(3):
                            S.stt(xs[:, c, :], tmpc[:, j:j + seq], cw[:, c, j:j + 1], xs[:, c, :], ALU.mult, ALU.add)
                            yield
                        S.act(xs[:, c, :], xs[:, c, :], AF.Silu)
                        yield
                    S.act(dtf[:], dtf[:], AF.Exp, bias=dtb[:, 0:1])
                    S.act(dtf[:], dtf[:], AF.Ln, bias=1.0)
                    yield
                    for ci in range(NCH):
                        cs = slice(ci * 128, (ci + 1) * 128)
                        S.transpose(P1[:, 256:260], dtf[0:4, cs], ident[0:4, 0:4])
                        for cx in range(2):
                            S.transpose(P0[:, cx * 128:(cx + 1) * 128], xs[:, cx, cs], ident[:])
                        S.transpose(P0[:, 256:384], xs[:, 2, cs], ident[:])
                        yield
                        S.copy(dtT[:], P1[:, 256:260])
                        yield
                        S.tt(adt[:], dtT[:], nA[:], ALU.mult)
                        S.act(btok[:], P0[:, 256:384], AF.Copy)
                        yield
                        S.mm(P1[:, 264:268], triu[:], adt[:])
                        S.mm(P1[:, 268:272], ones[:], adt[:])
                        S.copy(xtok[:].rearrange("p h d -> p (h d)"), P0[:, 0:256])
                        yield
                        S.copy(acs[:], P1[:, 264:272])
                        yield
                        S.act(eacs[:], acs[:, 0:4], AF.Exp)
                        S.act(eat[:], acs[:, 4:8], AF.Exp)
                        S.tt(dec[:], acs[:, 4:8], acs[:, 0:4], ALU.subtract)
                        yield
                        S.act(dec[:], dec[:], AF.Exp)
                        S.tt(xdt[:], xtok[:], dtT[:].unsqueeze(2).broadcast_to([128, 4, 64]), ALU.mult)
                        yield
                        S.tt(xdd[:], xdt[:], dec[:].unsqueeze(2).broadcast_to([128, 4, 64]), ALU.mult, eng="gpsimd")
                        for g in range(2):
                            gs = slice(g * 64, (g + 1) * 64)
                            S.mm(P1[:, g * 128:(g + 1) * 128], xs[gs, 2, cs], xs[gs, 3, cs], sync_self=True)
                        yield
                        for h in range(4):
                            S.ts(Lh[:, h, :], strl[:], adt[:, h:h + 1], None, op0=ALU.mult, eng="gpsimd")
                            S.mm(P2[:, h * 128:(h + 1) * 128], Lh[:, h, :], triu[:])
                            yield
                        for g in range(2):
                            S.tt(Sm[:, g, :], P1[:, g * 128:(g + 1) * 128], triu[:], ALU.mult)
                        S.act(E[:].rearrange("p h l -> p (h l)"), P2[:], AF.Exp)
                        yield
                        for h in range(4):
                            g = h // 2; j = h % 2
                            gs = slice(g * 64, (g + 1) * 64)
                            S.tt(P[:, h, :], E[:, h, :], Sm[:, g, :], ALU.mult, eng=("gpsimd" if h % 2 else "vector"))
                            S.mm(P3[:, h * 64:(h + 1) * 64], P[:, h, :], xdt[:, h, :])
                            S.mm(P3[:, 256 + h * 64:256 + (h + 1) * 64], xs[gs, 3, cs], Sst[gs, j, :], sync_self=True)
                            S.mm(P0[gs, 384 + j * 64:384 + (j + 1) * 64], btok[:, gs], xdd[:, h, :])
                            yield
                        for h in range(4):
                            g = h // 2; j = h % 2
                            gs = slice(g * 64, (g + 1) * 64)
                            S.ts(ytok[:, h, :], P3[:, 256 + h * 64:256 + (h + 1) * 64], eacs[:, h:h + 1], None, op0=ALU.mult)
                            S.stt(Sst[gs, j, :], Sst[gs, j, :], eat[gs, h:h + 1], P0[gs, 384 + j * 64:384 + (j + 1) * 64], ALU.mult, ALU.add)
                            yield
                            S.tt(ytok[:, h, :], ytok[:, h, :], P3[:, h * 64:(h + 1) * 64], ALU.add)
                            yield
                            S.stt(ytok[:, h, :], xtok[:, h, :], dsk[:, h:h + 1], ytok[:, h, :], ALU.mult, ALU.add)
                            yield
                        for cx in range(2):
                            S.transpose(P1[:, cx * 128:(cx + 1) * 128],
                                        ytok[:, 2 * cx:2 * cx + 2, :].rearrange("p h d -> p (h d)"), ident[:])
                        yield
                        for cx in range(2):
                            S.act(ysb[:, cx, cs], P1[:, cx * 128:(cx + 1) * 128], AF.Copy)
                        yield
                    for c in range(2):
                        ld("sync", tmpc[:, 0:seq], PRJ[OFF_S + c * 128:OFF_S + (c + 1) * 128, t0:t0 + seq])
                        yield
                        S.act(tmpc[:, 0:seq], tmpc[:, 0:seq], AF.Silu)
                        yield
                        S.tt(ysb[:, c, :], ysb[:, c, :], tmpc[:, 0:seq], ALU.mult)
                        yield
                        S.tt(tmpc[:, 0:seq], ysb[:, c, :], ysb[:, c, :], ALU.mult, eng="gpsimd")
                        yield
                        for t in range(NTS):
                            sl = slice(t * 512, (t + 1) * 512)
                            pp = (P2, P3)[t % 2]
                            S.mm(pp[:], ones[:], tmpc[:, sl])
                            S.act(rs[:], pp[:], AF.Ln, scale=1.0 / 128.0, bias=RMS_EPS)
                            yield
                            S.act(rs[:], rs[:], AF.Exp, scale=-0.5)
                            S.tt(rs[:], rs[:], ysb[:, c, sl], ALU.mult)
                            yield
                            S.ts(ob[:, sl], rs[:], nw[:, c:c + 1], None, op0=ALU.mult)
                            yield
                        stq("sync", MIX[768 + c * 128:768 + (c + 1) * 128, t0:t0 + seq], ob[:])
                        yield

                chains = [mkchain(q) for q in range(2)]
                rr([chain_gen(chains[q], q) for q in range(2)])


        def phase_mlstm2(l):
            NCH = seq // 128
            with Ph() as ph:
                ident, triu, strl, ones = chunk_consts(ph)
                m125 = ph.sb("m125", [128, 128])
                S.ts(m125[:], triu[:], 0.125, None, op0=ALU.mult)
                ib = ph.sb("ib", [4, 1]); ld("sync", ib[:], D["m_i_bias"][l].rearrange("(p o) -> p o", o=1))
                nfb = ph.sb("nfb", [4, 1]); ld("sync", nfb[:], D["m_f_bias"][l].rearrange("(p o) -> p o", o=1))
                S.ts(nfb[:], nfb[:], -1.0, None, op0=ALU.mult)
                nw = ph.sb("nw", [128, 2]); colvec(nw, D["m_norm_w"][l])

                def mkchain(q):
                    C = Ctx()
                    n_ = lambda nm: f"{nm}q{q}"
                    C.pb = [ph.ps(n_(f"B{i}"), [128, 512]) for i in range(4)]
                    C.qkv = [[ph.sb(n_(f"{nm}{i}"), [128, 2, 512]) for nm in ("q", "k", "v")] for i in range(2)]
                    for set_ in C.qkv:
                        for t_ in set_:
                            S.split(t_, 128)
                    C.ifm = ph.sb(n_("ifm"), [4, seq]); S.split(C.ifm, 128)
                    C.ffm = ph.sb(n_("ffm"), [4, seq]); S.split(C.ffm, 128)
                    C.hsb = ph.sb(n_("hsb"), [128, 2, seq]); S.split(C.hsb, 128)
                    C.tmpo = ph.sb(n_("tmpo"), [128, seq])
                    C.ob = ph.sb(n_("ob"), [128, seq], BF16)
                    C.Cst = ph.sb(n_("Cst"), [128, 2, 66]); S.split(C.Cst, 66); S.memset(C.Cst[:], 0.0)
                    for nm, shp, gr in (("lifT", [128, 8], None), ("acs", [128, 8], None), ("eb", [128, 4], None),
                                        ("eg", [128, 4], None), ("wd", [128, 4], None), ("ktok", [128, 4, 64], 64),
                                        ("vt1", [128, 4, 66], 66), ("vw", [128, 4, 66], 66), ("Lh", [128, 4, 128], 128),
                                        ("E", [128, 4, 128], 128), ("Sm", [128, 4, 128], 128), ("P", [128, 4, 128], 128),
                                        ("tot", [128, 4, 66], 66), ("dd", [128, 8], None), ("hn", [128, 4, 64], 64),
                                        ("sq", [128, 4, 64], None), ("ss", [128, 4], None)):
                        t_ = ph.sb(n_(nm), shp)
                        if gr:
                            S.split(t_, gr)
                        setattr(C, nm, t_)
                    S.memset(C.vt1[:], 0.0)
                    S.memset(C.vt1[:, :, 64:65], 1.0)
                    return C

                def chain_gen(C, q):
                    t0 = q * seq
                    B0, B1, B2, B3 = C.pb
                    ifm, ffm, hsb, tmpo, ob, Cst = C.ifm, C.ffm, C.hsb, C.tmpo, C.ob, C.Cst
                    lifT, acs, eb, eg, wd, ktok, vt1, vw = C.lifT, C.acs, C.eb, C.eg, C.wd, C.ktok, C.vt1, C.vw
                    Lh, E, Sm, P, tot, dd, hn, sq, ss = C.Lh, C.E, C.Sm, C.P, C.tot, C.dd, C.hn, C.sq, C.ss

                    def load_block(nb):
                        qb, kb, vb = C.qkv[nb % 2]
                        g0 = t0 + nb * 512
                        for c in range(2):
                            for j_, tt_ in enumerate((qb, kb, vb)):
                                ld("sync", tt_[:, c, :], PRJ[j_ * 256 + c * 128:j_ * 256 + (c + 1) * 128, g0:g0 + 512])

                    ld("sync", ifm[0:4, :], PRJ[1024:1028, t0:t0 + seq])
                    ld("sync", ffm[:], PRJ[1028:1032, t0:t0 + seq])
                    load_block(0)
                    yield
                    S.ts(ifm[0:4, :], ifm[0:4, :], ib[:, 0:1], None, op0=ALU.add)
                    S.act(ffm[:], ffm[:], AF.Exp, scale=-1.0, bias=nfb[:, 0:1])
                    yield
                    S.act(ffm[:], ffm[:], AF.Ln, bias=1.0)
                    yield
                    S.ts(ffm[:], ffm[:], -1.0, None, op0=ALU.mult)
                    yield
                    for ci in range(NCH):
                        nb = ci // 4
                        if ci % 4 == 0 and (nb + 1) * 512 < seq:
                            load_block(nb + 1)
                        qb, kb, vb = C.qkv[nb % 2]
                        cl = slice((ci % 4) * 128, (ci % 4 + 1) * 128)
                        cs = slice(ci * 128, (ci + 1) * 128)
                        S.transpose(B3[:, 272:276], ifm[0:4, cs], ident[0:4, 0:4])
                        S.transpose(B3[:, 276:280], ffm[0:4, cs], ident[0:4, 0:4])
                        for cx in range(2):
                            S.transpose(B0[:, cx * 128:(cx + 1) * 128], kb[:, cx, cl], ident[:])
                            S.transpose(B0[:, 256 + cx * 128:256 + (cx + 1) * 128], vb[:, cx, cl], ident[:])
                        yield
                        S.copy(lifT[:], B3[:, 272:280])
                        S.act(vt1[:, :, 0:64], B0[:, 256:512].rearrange("p (h d) -> p h d", h=4), AF.Copy)
                        yield
                        S.mm(B3[:, 280:284], triu[:], lifT[:, 4:8])
                        S.mm(B3[:, 284:288], ones[:], lifT[:, 4:8])
                        S.copy(ktok[:].rearrange("p h d -> p (h d)"), B0[:, 0:256])
                        yield
                        S.copy(acs[:], B3[:, 280:288])
                        yield
                        S.act(eb[:], acs[:, 0:4], AF.Exp)
                        S.act(eg[:], acs[:, 4:8], AF.Exp)
                        S.tt(wd[:], acs[:, 4:8], acs[:, 0:4], ALU.subtract)
                        yield
                        S.tt(wd[:], wd[:], lifT[:, 0:4], ALU.add)
                        yield
                        S.act(wd[:], wd[:], AF.Exp)
                        yield
                        S.ts(wd[:], wd[:], 0.125, None, op0=ALU.mult)
                        yield
                        S.tt(vw[:], vt1[:], wd[:].unsqueeze(2).broadcast_to([128, 4, 66]), ALU.mult, eng="gpsimd")
                        for h in range(4):
                            hc = h // 2
                            hs = slice((h % 2) * 64, (h % 2) * 64 + 64)
                            S.mm(B1[:, h * 128:(h + 1) * 128], kb[hs, hc, cl], qb[hs, hc, cl], sync_self=True)
                            S.ts(Lh[:, h, :], strl[:], lifT[:, 4 + h:5 + h], None, op0=ALU.mult, eng="gpsimd")
                            S.mm(B2[:, h * 128:(h + 1) * 128], Lh[:, h, :], triu[:])
                            yield
                        for h in range(4):
                            S.act(E[:, h, :], B2[:, h * 128:(h + 1) * 128], AF.Exp, bias=lifT[:, h:h + 1])
                            S.tt(Sm[:, h, :], B1[:, h * 128:(h + 1) * 128], m125[:], ALU.mult)
                            yield
                            S.tt(P[:, h, :], E[:, h, :], Sm[:, h, :], ALU.mult, eng="gpsimd")
                            yield
                        for h in range(4):
                            hc = h // 2
                            hs = slice((h % 2) * 64, (h % 2) * 64 + 64)
                            S.mm(B3[:, h * 66:(h + 1) * 66], P[:, h, :], vt1[:, h, :])
                            S.mm(B0[:, h * 66:(h + 1) * 66], qb[hs, hc, cl], Cst[hs, hc, :], sync_self=True)
                            S.mm(B1[hs, hc * 66:(hc + 1) * 66], ktok[:, h, :], vw[:, h, :])
                            yield
                        for h in range(4):
                            hc = h // 2
                            hs = slice((h % 2) * 64, (h % 2) * 64 + 64)
                            S.ts(tot[:, h, :], B0[:, h * 66:(h + 1) * 66], eb[:, h:h + 1], None, op0=ALU.mult)
                            S.stt(Cst[hs, hc, :], Cst[hs, hc, :], eg[hs, h:h + 1], B1[hs, hc * 66:(hc + 1) * 66], ALU.mult, ALU.add)
                            yield
                            S.tt(tot[:, h, :], tot[:, h, :], B3[:, h * 66:(h + 1) * 66], ALU.add)
                            yield
                        den = tot[:, :, 64:65].rearrange("p h o -> p (h o)")
                        S.stt(dd[:, 0:4], den, -1.0, den, ALU.mult, ALU.max)
                        yield
                        S.ts(dd[:, 0:4], dd[:, 0:4], 1.0, None, op0=ALU.max)
                        yield
                        S.recip(dd[:, 4:8], dd[:, 0:4])
                        yield
                        S.tt(hn[:], tot[:, :, 0:64], dd[:, 4:8].unsqueeze(2).broadcast_to([128, 4, 64]), ALU.mult)
                        yield
                        S.tt(sq[:], hn[:], hn[:], ALU.mult, eng="gpsimd")
                        yield
                        S.reduce(ss[:], sq[:], ALU.add, AX.X)
                        yield
                        S.act(ss[:], ss[:], AF.Ln, scale=1.0 / 64.0, bias=RMS_EPS)
                        S.act(ss[:], ss[:], AF.Exp, scale=-0.5)
                        yield
                        S.tt(hn[:], hn[:], ss[:].unsqueeze(2).broadcast_to([128, 4, 64]), ALU.mult)
                        yield
                        for cx in range(2):
                            S.transpose(B2[:, cx * 128:(cx + 1) * 128],
                                        hn[:, 2 * cx:2 * cx + 2, :].rearrange("p h d -> p (h d)"), ident[:])
                        yield
                        for cx in range(2):
                            S.act(hsb[:, cx, cs], B2[:, cx * 128:(cx + 1) * 128], AF.Copy)
                        yield
                    for c in range(2):
                        ld("sync", tmpo[:], PRJ[768 + c * 128:768 + (c + 1) * 128, t0:t0 + seq])
                        yield
                        S.act(tmpo[:], tmpo[:], AF.Sigmoid)
                        yield
                        S.tt(hsb[:, c, :], hsb[:, c, :], tmpo[:], ALU.mult)
                        yield
                        S.ts(ob[:], hsb[:, c, :], nw[:, c:c + 1], None, op0=ALU.mult)
                        stq("sync", MIX[c * 128:(c + 1) * 128, t0:t0 + seq], ob[:])
                        yield

                chains = [mkchain(q) for q in range(2)]
                rr([chain_gen(chains[q], q) for q in range(2)])

        MIXERS = {}
        phase_in()
        for l in range(depth):
            phase_proj(l)
            if "r" in mixers:
                phase_rwkv2(l)
            if "s" in mixers:
                phase_ssd2(l)
            if "m" in mixers:
                phase_mlstm2(l)
                for nm in ("m", "r", "s"):
                    if nm in mixers and nm in MIXERS:
                        MIXERS[nm](l, s)
            for nm, r0 in (("m", 0), ("g", 256), ("r", 512), ("s", 768)):
                if nm not in mixers:
                    phase_zero_mix(r0)
            phase_out(l)
            phase_ffn(l, last=(l == depth - 1))
        S.wait_all("sync")
        S.emit()
    return nc


def kernel(**inputs):
    n = 8
    x = np.ascontiguousarray(inputs["x"], dtype=np.float32).reshape(n, 2 * 2048, DM)
    p = np.ascontiguousarray(inputs["p"], dtype=np.float32).reshape(4, n, 2 * 2048, 256)
    nc = build(4, 2048)
    consts = host_consts()
    in_maps = []
    for c in range(n):
        m = {"x": x[c], "p": np.ascontiguousarray(p[:, c])}
        for nm, _ in WNAMES:
            m[nm] = np.ascontiguousarray(inputs[nm], dtype=np.float32)
        m.update(consts)
        in_maps.append(m)
    res = run_bass_kernel_spmd(nc, in_maps, core_ids=list(range(n)))
    o = np.stack([r["out"] for r in res.results], 0)
    return o.reshape(16, 2048, DM).astype(np.float32)
```

```python
import numpy as np
from contextlib import ExitStack
import concourse.bass as bass
import concourse.mybir as mybir
from concourse.bass_utils import run_bass_kernel_spmd

F32 = mybir.dt.float32
BF16 = mybir.dt.bfloat16
AF = mybir.ActivationFunctionType
ALU = mybir.AluOpType
AX = mybir.AxisListType

ENGS = ["tensor", "vector", "scalar", "gpsimd", "sync"]
NPOOL = 12


class Sched:
    def __init__(self, nc, stack):
        self.nc = nc
        self.stack = stack
        self.prog = {e: [] for e in ENGS}
        self.sem = {}
        for e in ["tensor", "vector", "scalar", "gpsimd"]:
            self.sem[("c", e)] = stack.enter_context(nc.semaphore("c_" + e))
        for q in ["sync", "gpsimd"]:
            for i in range(NPOOL):
                self.sem[("d", q, i)] = stack.enter_context(nc.semaphore(f"d_{q}_{i}"))
        self.cnt = {e: 0 for e in ENGS}
        self.dman = {"sync": 0, "gpsimd": 0}
        self.known = {e: {} for e in ENGS}
        self.lastw = {}
        self.readers = {}
        self.nins = 0
        self.gran = {}
        self.allev = {}

    def keys(self, a):
        if isinstance(a, (str, tuple)):
            return [a]
        name = a.tensor.name
        g = self.gran.get(name)
        if g is None:
            return [name]
        ps = 1
        for d in list(a.tensor.shape)[1:]:
            ps *= int(d)
        off = int(a.offset) % ps
        ext = 1
        for (stp, cnt) in list(a.ap)[1:]:
            ext += (int(cnt) - 1) * abs(int(stp))
        return [(name, i) for i in range(off // g, (off + ext - 1) // g + 1)]

    def split(self, t, gran):
        self.gran[t.name] = gran

    def _ks(self, lst):
        out = []
        for a in lst:
            out.extend(self.keys(a))
        return out

    def _deps(self, eng, reads, writes, skip_self=False):
        need = {}
        for k in reads:
            ev = self.lastw.get(k)
            if ev is not None:
                need[ev[0]] = max(need.get(ev[0], 0), ev[1])
        for k in writes:
            ev = self.lastw.get(k)
            if ev is not None:
                need[ev[0]] = max(need.get(ev[0], 0), ev[1])
            for s, v in self.readers.get(k, {}).items():
                need[s] = max(need.get(s, 0), v)
        for s, v in need.items():
            if skip_self and s == ("c", eng):
                continue
            if self.known[eng].get(s, 0) < v:
                self.prog[eng].append(("wait", s, v))
                self.known[eng][s] = v

    def _commit(self, ev, reads, writes):
        self.allev[ev[0]] = max(self.allev.get(ev[0], 0), ev[1])
        for k in writes:
            self.lastw[k] = ev
            self.readers[k] = {}
        for k in reads:
            d = self.readers.setdefault(k, {})
            d[ev[0]] = max(d.get(ev[0], 0), ev[1])

    def op(self, eng, fn, reads=(), writes=(), skip_self=False):
        writes = list(writes) + [a for a in reads if not isinstance(a, (str, tuple)) and "PSum" in type(a.tensor).__name__]
        reads = self._ks(reads)
        writes = self._ks(writes)
        self._deps(eng, reads, writes, skip_self)
        self.cnt[eng] += 1
        ev = (("c", eng), self.cnt[eng])
        self.prog[eng].append(("op", fn, ev))
        self._commit(ev, reads, writes)
        self.nins += 1

    def dma(self, q, out, in_, reads=None, writes=None, **kw):
        reads = self._ks(reads if reads is not None else [in_])
        writes = self._ks(writes if writes is not None else [out])
        n = self.dman[q]
        slot = n % NPOOL
        tgt = 16 * (n // NPOOL + 1)
        s = ("d", q, slot)
        if n >= NPOOL and self.known[q].get(s, 0) < tgt - 16:
            self.prog[q].append(("wait", s, tgt - 16))
            self.known[q][s] = tgt - 16
        self._deps(q, reads, writes)
        self.dman[q] += 1
        ev = (s, tgt)
        self.prog[q].append(("dma", (out, in_, kw), ev))
        self._commit(ev, reads, writes)
        self.nins += 1
        return ev

    def wait_all(self, eng="sync"):
        for s, v in self.allev.items():
            if self.known[eng].get(s, 0) < v:
                self.prog[eng].append(("wait", s, v))
                self.known[eng][s] = v

    def barrier(self):
        for e in ENGS:
            self.wait_all(e)
        self.lastw = {}
        self.readers = {}

    def emit(self):
        nc = self.nc
        sem = self.sem
        prog = self.prog
        with nc.Block() as block:
            def run(engname):
                def body(eng):
                    for it in prog[engname]:
                        if it[0] == "wait":
                            eng.wait_ge(sem[it[1]], it[2])
                        elif it[0] == "op":
                            ins = it[1](eng)
                            ins.then_inc(sem[it[2][0]], 1)
                        else:
                            out, in_, kw = it[1]
                            eng.dma_start(out=out, in_=in_, **kw).then_inc(sem[it[2][0]], 16)
                return body
            block.tensor(run("tensor"))
            block.vector(run("vector"))
            block.scalar(run("scalar"))
            block.gpsimd(run("gpsimd"))
            block.sync(run("sync"))
        self.prog = {e: [] for e in ENGS}

    def mm(self, out, lhsT, rhs, start=True, stop=True, extra_reads=(), sync_self=False):
        self.op("tensor", lambda e: e.matmul(out, lhsT, rhs, start=start, stop=stop),
                reads=[lhsT, rhs, *extra_reads], writes=[out], skip_self=not sync_self)

    def transpose(self, out, in_, ident):
        self.op("tensor", lambda e: e.transpose(out, in_, ident),
                reads=[in_, ident], writes=[out], skip_self=True)

    def act(self, out, in_, func, bias=0.0, scale=1.0, accum_out=None, eng="scalar"):
        reads = [in_]
        if not isinstance(bias, (int, float)):
            reads.append(bias)
        if not isinstance(scale, (int, float)):
            reads.append(scale)
        writes = [out]
        kw = {}
        if accum_out is not None:
            writes.append(accum_out)
            kw["accum_out"] = accum_out
        self.op("scalar", lambda e: e.activation(out, in_, func, bias=bias, scale=scale, **kw),
                reads=reads, writes=writes)

    def tt(self, out, in0, in1, op, eng="vector"):
        self.op(eng, lambda e: e.tensor_tensor(out, in0, in1, op), reads=[in0, in1], writes=[out])

    def ts(self, out, in0, s1, s2=None, op0=ALU.mult, op1=None, eng="vector", accum_out=None):
        reads = [in0]
        if not isinstance(s1, (int, float)):
            reads.append(s1)
        if s2 is not None and not isinstance(s2, (int, float)):
            reads.append(s2)
        writes = [out]
        kw = {}
        if op1 is not None:
            kw["op1"] = op1
        if accum_out is not None:
            kw["accum_out"] = accum_out
            writes.append(accum_out)
        self.op(eng, lambda e: e.tensor_scalar(out, in0, s1, s2, op0, **kw), reads=reads, writes=writes)

    def stt(self, out, in0, scalar, in1, op0, op1, eng="vector"):
        reads = [in0, in1]
        if not isinstance(scalar, (int, float)):
            reads.append(scalar)
        self.op(eng, lambda e: e.scalar_tensor_tensor(out, in0, scalar, in1, op0, op1), reads=reads, writes=[out])

    def copy(self, out, in_, eng="vector"):
        self.op(eng, lambda e: e.tensor_copy(out, in_), reads=[in_], writes=[out])

    def memset(self, ap, val, eng="vector"):
        self.op(eng, lambda e: e.memset(ap, val), reads=[], writes=[ap])

    def scan(self, out, d0, d1, init, op0=ALU.mult, op1=ALU.add):
        reads = [d0, d1]
        if not isinstance(init, (int, float)):
            reads.append(init)
        self.op("vector", lambda e: e.tensor_tensor_scan(out, d0, d1, init, op0, op1), reads=reads, writes=[out])

    def reduce(self, out, in_, op=ALU.add, axis=AX.X, eng="vector"):
        self.op(eng, lambda e: e.tensor_reduce(out, in_, axis, op), reads=[in_], writes=[out])

    def recip(self, out, in_):
        self.op("vector", lambda e: e.reciprocal(out, in_), reads=[in_], writes=[out])


DM = 1024
NIN = 3212
DFF = 2816
ALPHA = 8.0 ** 0.25
LN_EPS = 1e-5
OFF_M, OFF_G, OFF_R, OFF_S = 0, 1032, 1544, 2440

WNAMES = [("w_in", [4, 1024, 3212]), ("m_i_bias", [4, 4]), ("m_f_bias", [4, 4]), ("m_norm_w", [4, 256]),
          ("g_conv_w", [4, 4, 256]), ("g_conv_b", [4, 256]), ("g_w_a", [4, 4, 64, 64]), ("g_b_a", [4, 256]),
          ("g_w_x", [4, 4, 64, 64]), ("g_b_x", [4, 256]), ("g_lambda", [4, 256]), ("r_mu", [4, 896]),
          ("r_w0", [4, 256]), ("r_w2", [4, 32, 256]), ("r_a0", [4, 256]), ("r_a2", [4, 32, 256]),
          ("r_g2", [4, 64, 256]), ("r_k_k", [4, 256]), ("r_k_a", [4, 256]), ("r_r_k", [4, 4, 64]),
          ("r_ln_w", [4, 256]), ("r_ln_b", [4, 256]), ("s_conv_w", [4, 4, 512]), ("s_conv_b", [4, 512]),
          ("s_dt_bias", [4, 4]), ("s_a_log", [4, 4]), ("s_d", [4, 4]), ("s_norm_w", [4, 256]),
          ("w_out", [4, 1024, 1024]), ("ln1_g", [4, 1024]), ("ln1_b", [4, 1024]), ("ln2_g", [4, 1024]),
          ("ln2_b", [4, 1024]), ("f_w1", [2, 1024, 2816]), ("f_w3", [2, 1024, 2816]), ("f_w2", [2, 2816, 1024]),
          ("e_router", [2, 1024, 8]), ("e_w1", [2, 8, 1024, 2816]), ("e_w3", [2, 8, 1024, 2816]),
          ("e_w2", [2, 8, 2816, 1024]), ("pe_proj", [4, 256, 1024]), ("pe_gate_w", [4, 1024, 1024]),
          ("pe_gate_b", [4, 1024])]


def host_consts():
    j = np.arange(128)
    c = {}
    c["c_ident"] = np.eye(128, dtype=np.float32)
    c["c_triu"] = (j[:, None] <= j[None, :]).astype(np.float32)
    c["c_strl"] = (j[:, None] > j[None, :]).astype(np.float32)
    sel = np.zeros((8, 8, 128), np.float32)
    for e in range(8):
        sel[e, e, :] = 1.0
    c["c_sel"] = sel
    return c


class Ctx:
    pass


def build(depth=4, seq=2048, debug=False, mixers=("m", "g", "r", "s")):
    T = 2 * seq
    NT = T // 512
    NTS = seq // 512
    nc = bass.Bass("TRN2", target_bir_lowering=False)
    D = {}
    D["x"] = nc.dram_tensor("x", [T, DM], F32, kind="ExternalInput").ap()
    D["p"] = nc.dram_tensor("p", [4, T, 256], F32, kind="ExternalInput").ap()
    for n, sh in WNAMES:
        D[n] = nc.dram_tensor(n, sh, F32, kind="ExternalInput").ap()
    for n, a in host_consts().items():
        D[n] = nc.dram_tensor(n, list(a.shape), F32, kind="ExternalInput").ap()
    out = nc.dram_tensor("out", [T, DM], F32, kind="ExternalOutput").ap()
    sk = "ExternalOutput" if debug else "Internal"
    XT = nc.dram_tensor("XT", [DM, T], F32, kind=sk).ap()
    PRJ = nc.dram_tensor("PRJ", [3328, T], F32, kind=sk).ap()
    MIX = nc.dram_tensor("MIX", [DM, T], BF16, kind=sk).ap()
    X1T = nc.dram_tensor("X1T", [DM, T], F32, kind=sk).ap()
    XTv = XT.rearrange("(c p) t -> p c t", p=128)
    X1Tv = X1T.rearrange("(c p) t -> p c t", p=128)
    MIXv = MIX.rearrange("(c p) t -> p c t", p=128)

    with ExitStack() as top, nc.allow_non_contiguous_dma(reason="small param / strided tile loads"):
        S = Sched(nc, top)

        def ld(q, dst, src):
            S.dma(q, dst, src, reads=[], writes=[dst])

        def stq(q, dst, src):
            S.dma(q, dst, src, reads=[src], writes=[])

        def colvec(dst, vec, n=128):
            C = dst.shape[1]
            for c in range(C):
                ld("sync", dst[:, c:c + 1], vec[c * n:(c + 1) * n].rearrange("(p o) -> p o", o=1))

        UID = [0]

        class Ph:
            def __init__(self):
                self.st = ExitStack()
                UID[0] += 1
                self.uid = UID[0]

            def __enter__(self):
                self.st.__enter__()
                return self

            def __exit__(self, *a):
                S.barrier()
                S.emit()
                return self.st.__exit__(*a)

            def sb(self, name, shape, dt=F32):
                return self.st.enter_context(nc.sbuf_tensor(f"{name}_{self.uid}", shape, dt))

            def ps(self, name, shape, dt=F32):
                return self.st.enter_context(nc.psum_tensor(f"{name}_{self.uid}", shape, dt))

            def consts(self, names):
                r = {}
                for n in names:
                    sh = list(D[n].shape)
                    t = self.sb("k_" + n, sh if len(sh) == 2 else [sh[0], sh[1], sh[2]])
                    ld("sync", t[:], D[n])
                    r[n] = t
                return r

        def layer_norm(ph, z, ones, g, b, pm, pq, sq, mean, rstd):
            S.act(sq[:], z[:], AF.Square)
            for c in range(8):
                S.mm(pm[:], ones[:], z[:, c, :], start=(c == 0), stop=(c == 7))
            for c in range(8):
                S.mm(pq[:], ones[:], sq[:, c, :], start=(c == 0), stop=(c == 7))
            S.act(mean[:], pm[:], AF.Copy)
            S.tt(rstd[:], mean[:], mean[:], ALU.mult)
            S.tt(rstd[:], pq[:], rstd[:], ALU.subtract)
            S.act(rstd[:], rstd[:], AF.Ln, bias=LN_EPS)
            S.act(rstd[:], rstd[:], AF.Exp, scale=-0.5)
            S.tt(z[:], z[:], mean[:].unsqueeze(1).broadcast_to([128, 8, 512]), ALU.subtract)
            S.tt(z[:], z[:], rstd[:].unsqueeze(1).broadcast_to([128, 8, 512]), ALU.mult)
            for c in range(8):
                S.ts(z[:, c, :], z[:, c, :], g[:, c:c + 1], b[:, c:c + 1], op0=ALU.mult, op1=ALU.add,
                     eng="gpsimd")

        def phase_in():
            with Ph() as ph:
                ident = ph.consts(["c_ident"])["c_ident"]
                xin = [ph.sb(f"xin{i}", [128, 4, DM]) for i in range(2)]
                xo = [ph.sb(f"xo{i}", [128, 8, 512]) for i in range(2)]
                pts = [ph.ps(f"pt{i}", [128, 512]) for i in range(4)]
                for t in range(NT):
                    xi = xin[t % 2]
                    ld("sync", xi[:], D["x"][t * 512:(t + 1) * 512, :].rearrange("(j p) f -> p j f", p=128))
                    o = xo[t % 2]
                    for c in range(8):
                        pt = pts[c % 4]
                        for j in range(4):
                            S.transpose(pt[:, j * 128:(j + 1) * 128], xi[:, j, c * 128:(c + 1) * 128], ident[:])
                        if c % 2 == 0:
                            S.copy(o[:, c, :], pt[:])
                        else:
                            S.act(o[:, c, :], pt[:], AF.Copy)
                    stq("sync", XTv[:, :, t * 512:(t + 1) * 512], o[:])

        def phase_proj(l):
            fuse_g = "g" in mixers
            with Ph() as ph:
                win = ph.sb("win", [128, 8, NIN], BF16)
                wv = D["w_in"][l].rearrange("(k p) n -> p k n", p=128)
                for c0 in range(0, NIN, 1024):
                    c1_ = min(NIN, c0 + 1024)
                    ld("gpsimd", win[:, :, c0:c1_], wv[:, :, c0:c1_])
                xb = [ph.sb(f"xb{i}", [128, 8, 512], BF16) for i in range(2)]
                stg = [ph.sb(f"stg{i}", [128, 512]) for i in range(6)]
                pps = [ph.ps(f"pp{i}", [128, 512]) for i in range(6)]
                chunks = [(i * 128, 128) for i in range(8)] + [(1024, 8)] + [(OFF_G + i * 128, 128) for i in range(4)] \
                    + [(OFF_R + i * 128, 128) for i in range(7)] + [(OFF_S + i * 128, 128) for i in range(6)] + [(3208, 4)]
                if fuse_g:
                    cw = ph.sb("cw", [128, 2, 4])
                    for j in range(4):
                        for c in range(2):
                            ld("sync", cw[:, c, j:j + 1], D["g_conv_w"][l, j, c * 128:(c + 1) * 128].rearrange("(p o) -> p o", o=1))
                    cb = ph.sb("cb", [128, 2]); colvec(cb, D["g_conv_b"][l])
                    ba = ph.sb("ba", [128, 2]); colvec(ba, D["g_b_a"][l])
                    bx = ph.sb("bx", [128, 2]); colvec(bx, D["g_b_x"][l])
                    lam = ph.sb("lam", [128, 2]); colvec(lam, D["g_lambda"][l])
                    c1 = ph.sb("c1", [128, 2])
                    S.act(c1[:], lam[:], AF.Exp, scale=-1.0)
                    S.act(c1[:], c1[:], AF.Ln, bias=1.0)
                    S.ts(c1[:], c1[:], -8.0, op0=ALU.mult)
                    bda = ph.sb("bda", [128, 2, 128]); bdx = ph.sb("bdx", [128, 2, 128])
                    S.memset(bda[:], 0.0); S.memset(bdx[:], 0.0)
                    for c in range(2):
                        for hh in range(2):
                            ld("sync", bda[hh * 64:(hh + 1) * 64, c, hh * 64:(hh + 1) * 64], D["g_w_a"][l, 2 * c + hh])
                            ld("sync", bdx[hh * 64:(hh + 1) * 64, c, hh * 64:(hh + 1) * 64], D["g_w_x"][l, 2 * c + hh])
                    xbh = [ph.sb(f"xbh{i}", [128, 2, 515]) for i in range(2)]
                    gtt = [ph.sb(f"gtt{i}", [128, 2, 512]) for i in range(2)]
                    for t_ in xbh:
                        S.split(t_, 515)
                    for t_ in gtt:
                        S.split(t_, 512)
                    Wk = {nm: [ph.sb(f"rg{nm}{c}", [128, 512]) for c in range(2)] for nm in ("xc", "A", "U", "H")}
                    obg = [ph.sb(f"rgob{c}", [128, 512], BF16) for c in range(2)]
                    hprev = [ph.sb(f"hprev{c}", [128, 1]) for c in range(2)]
                    gps = [ph.ps(f"gp{i}", [128, 512]) for i in range(2)]

                    def rg_tile_gen(t, c):
                        p = t % 2
                        first = (t % NTS == 0)
                        tok0 = t * 512
                        if first:
                            S.memset(xbh[p][:, c, 0:3], 0.0)
                        else:
                            S.copy(xbh[p][:, c, 0:3], xbh[1 - p][:, c, 512:515], eng="gpsimd")
                        yield
                        if True:
                            xh = xbh[p]; g_ = gtt[p][:, c, :]
                            xc, A, U, Hh = Wk["xc"][c][:], Wk["A"][c][:], Wk["U"][c][:], Wk["H"][c][:]
                            S.ts(xc, xh[:, c, 3:515], cw[:, c, 3:4], cb[:, c:c + 1], op0=ALU.mult, op1=ALU.add)
                            yield
                            for j in range(3):
                                S.stt(xc, xh[:, c, j:j + 512], cw[:, c, j:j + 1], xc, ALU.mult, ALU.add)
                                yield
                            S.mm(gps[0][:], bda[:, c, :], xc)
                            S.mm(gps[1][:], bdx[:, c, :], xc)
                            S.act(A, gps[0][:], AF.Sigmoid, bias=ba[:, c:c + 1])
                            S.act(U, gps[1][:], AF.Sigmoid, bias=bx[:, c:c + 1])
                            yield
                            S.act(A, A, AF.Exp, scale=c1[:, c:c + 1])
                            S.tt(U, U, xc, ALU.mult, eng="gpsimd")
                            yield
                            S.tt(Hh, A, A, ALU.mult, eng="gpsimd")
                            yield
                            S.ts(Hh, Hh, -1.0, 1.0, op0=ALU.mult, op1=ALU.add, eng="gpsimd")
                            yield
                            S.ts(Hh, Hh, 1e-30, None, op0=ALU.max, eng="gpsimd")
                            yield
                            S.act(Hh, Hh, AF.Sqrt)
                            yield
                            S.tt(U, U, Hh, ALU.mult, eng="gpsimd")
                            yield
                            S.scan(Hh, A, U, 0.0 if first else hprev[c][:, 0:1])
                            yield
                            S.copy(hprev[c][:, 0:1], Wk["H"][c][:, 511:512], eng="gpsimd")
                            S.tt(A, g_, g_, ALU.mult, eng="gpsimd")
                            yield
                            S.ts(A, A, 0.044715, 1.0, op0=ALU.mult, op1=ALU.add, eng="gpsimd")
                            yield
                            S.tt(A, A, g_, ALU.mult, eng="gpsimd")
                            yield
                            S.act(A, A, AF.Sigmoid, scale=1.5957691216057308)
                            yield
                            S.tt(xc, A, g_, ALU.mult, eng="gpsimd")
                            yield
                            S.tt(obg[c][:], Hh, xc, ALU.mult)
                            stq("sync", MIX[256 + c * 128:256 + (c + 1) * 128, tok0:tok0 + 512], obg[c][:])
                            yield
                n = 0
                rg = None
                for t in range(NT):
                    x_ = xb[t % 2]
                    ld("gpsimd", x_[:], XTv[:, :, t * 512:(t + 1) * 512])
                    for (col0, mw) in chunks:
                        pp = pps[n % 6]
                        sg = stg[n % 6]
                        for k in range(8):
                            S.mm(pp[0:mw, :], win[:, k, col0:col0 + mw], x_[:, k, :], start=(k == 0), stop=(k == 7))
                        dst = sg[0:mw, :]
                        store = True
                        if fuse_g and OFF_G <= col0 < OFF_G + 512:
                            ci_ = (col0 - OFF_G) // 128
                            dst = xbh[t % 2][:, ci_, 3:515] if ci_ < 2 else gtt[t % 2][:, ci_ - 2, :]
                            store = False
                        if n % 2 == 0:
                            S.copy(dst, pp[0:mw, :])
                        else:
                            S.act(dst, pp[0:mw, :], AF.Copy)
                        if store:
                            stq("sync", PRJ[col0:col0 + mw, t * 512:(t + 1) * 512], sg[0:mw, :])
                        n += 1
                        if rg is not None:
                            for g__ in rg:
                                next(g__, None)
                    if fuse_g:
                        if rg is not None:
                            for g__ in rg:
                                for _ in g__:
                                    pass
                        rg = [rg_tile_gen(t, 0), rg_tile_gen(t, 1)]
                        for g__ in rg:
                            next(g__, None)
                if rg is not None:
                    for g__ in rg:
                        for _ in g__:
                            pass

        def phase_zero_mix(rows0):
            with Ph() as ph:
                z = ph.sb("zz", [128, 2, 512], BF16)
                S.memset(z[:], 0.0)
                for t in range(NT):
                    stq("sync", MIX[rows0:rows0 + 256, t * 512:(t + 1) * 512].rearrange("(c p) t -> p c t", p=128), z[:])

        def gelu_tanh(ph, out, x, tmp):
            S.tt(tmp, x, x, ALU.mult, eng="gpsimd")
            S.ts(tmp, tmp, 0.044715, 1.0, op0=ALU.mult, op1=ALU.add, eng="gpsimd")
            S.tt(tmp, tmp, x, ALU.mult, eng="gpsimd")
            S.act(tmp, tmp, AF.Sigmoid, scale=1.5957691216057308)
            S.tt(out, tmp, x, ALU.mult, eng="gpsimd")

        def phase_rglru(l, s):
            t0 = s * seq
            with Ph() as ph:
                cw = ph.sb("cw", [128, 2, 4])
                for j in range(4):
                    for c in range(2):
                        ld("sync", cw[:, c, j:j + 1], D["g_conv_w"][l, j, c * 128:(c + 1) * 128].rearrange("(p o) -> p o", o=1))
                cb = ph.sb("cb", [128, 2]); colvec(cb, D["g_conv_b"][l])
                ba = ph.sb("ba", [128, 2]); colvec(ba, D["g_b_a"][l])
                bx = ph.sb("bx", [128, 2]); colvec(bx, D["g_b_x"][l])
                lam = ph.sb("lam", [128, 2]); colvec(lam, D["g_lambda"][l])
                c1 = ph.sb("c1", [128, 2])
                S.act(c1[:], lam[:], AF.Exp, scale=-1.0)
                S.act(c1[:], c1[:], AF.Ln, bias=1.0)
                S.ts(c1[:], c1[:], -8.0, op0=ALU.mult)
                bda = ph.sb("bda", [128, 2, 128]); bdx = ph.sb("bdx", [128, 2, 128])
                S.memset(bda[:], 0.0); S.memset(bdx[:], 0.0)
                for c in range(2):
                    for hh in range(2):
                        ld("sync", bda[hh * 64:(hh + 1) * 64, c, hh * 64:(hh + 1) * 64], D["g_w_a"][l, 2 * c + hh])
                        ld("sync", bdx[hh * 64:(hh + 1) * 64, c, hh * 64:(hh + 1) * 64], D["g_w_x"][l, 2 * c + hh])
                xb = ph.sb("gxb", [128, 2, seq + 3])
                S.memset(xb[:, :, 0:3], 0.0)
                gt = ph.sb("ggt", [128, 2, seq])
                for c in range(2):
                    ld("sync", xb[:, c, 3:3 + seq], PRJ[OFF_G + c * 128:OFF_G + (c + 1) * 128, t0:t0 + seq])
                    ld("sync", gt[:, c, :], PRJ[OFF_G + 256 + c * 128:OFF_G + 256 + (c + 1) * 128, t0:t0 + seq])
                xc = ph.sb("gxc", [128, 2, seq])
                A = ph.sb("gA", [128, 2, seq]); U = ph.sb("gU", [128, 2, seq]); Hh = ph.sb("gH", [128, 2, seq])
                ob = ph.sb("gob", [128, 2, seq], BF16)
                pps = [ph.ps(f"gp{i}", [128, 512]) for i in range(4)]
                for c in range(2):
                    S.ts(xc[:, c, :], xb[:, c, 3:3 + seq], cw[:, c, 3:4], cb[:, c:c + 1], op0=ALU.mult, op1=ALU.add)
                    for j in range(3):
                        S.stt(xc[:, c, :], xb[:, c, j:j + seq], cw[:, c, j:j + 1], xc[:, c, :], ALU.mult, ALU.add)
                n = 0
                for c in range(2):
                    for t in range(NTS):
                        sl = slice(t * 512, (t + 1) * 512)
                        pa = pps[n % 4]; px = pps[(n + 1) % 4]; n += 2
                        S.mm(pa[:], bda[:, c, :], xc[:, c, sl])
                        S.mm(px[:], bdx[:, c, :], xc[:, c, sl])
                        S.act(A[:, c, sl], pa[:], AF.Sigmoid, bias=ba[:, c:c + 1])
                        S.act(U[:, c, sl], px[:], AF.Sigmoid, bias=bx[:, c:c + 1])
                    S.act(A[:, c, :], A[:, c, :], AF.Exp, scale=c1[:, c:c + 1])
                    S.tt(U[:, c, :], U[:, c, :], xc[:, c, :], ALU.mult)
                    S.tt(Hh[:, c, :], A[:, c, :], A[:, c, :], ALU.mult)
                    S.ts(Hh[:, c, :], Hh[:, c, :], -1.0, 1.0, op0=ALU.mult, op1=ALU.add)
                    S.ts(Hh[:, c, :], Hh[:, c, :], 1e-30, None, op0=ALU.max)
                    S.act(Hh[:, c, :], Hh[:, c, :], AF.Sqrt)
                    S.tt(U[:, c, :], U[:, c, :], Hh[:, c, :], ALU.mult)
                    S.scan(Hh[:, c, :], A[:, c, :], U[:, c, :], 0.0)
                    gelu_tanh(ph, xc[:, c, :], gt[:, c, :], A[:, c, :])
                    S.tt(ob[:, c, :], Hh[:, c, :], xc[:, c, :], ALU.mult)
                    stq("sync", MIX[256 + c * 128:256 + (c + 1) * 128, t0:t0 + seq], ob[:, c, :])

        def phase_out(l):
            with Ph() as ph:
                wo = ph.sb("wo", [128, 8, DM], BF16)
                ld("gpsimd", wo[:], D["w_out"][l].rearrange("(k p) n -> p k n", p=128))
                g1 = ph.sb("g1", [128, 8]); colvec(g1, D["ln1_g"][l])
                b1 = ph.sb("b1", [128, 8]); colvec(b1, D["ln1_b"][l])
                ones = ph.sb("ones", [128, 128]); S.memset(ones[:], 1.0 / DM)
                mb = [ph.sb(f"mb{i}", [128, 8, 512], BF16) for i in range(2)]
                xf = [ph.sb(f"xf{i}", [128, 8, 512]) for i in range(2)]
                zz = [ph.sb(f"z{i}", [128, 8, 512]) for i in range(2)]
                for tz in zz + xf + mb:
                    S.split(tz, 512)
                sq = ph.sb("sq", [128, 8, 512]); mean = ph.sb("mean", [128, 512]); rstd = ph.sb("rstd", [128, 512])
                pps = [ph.ps(f"pp{i}", [128, 512]) for i in range(6)]
                pm = ph.ps("pm", [128, 512]); pq = ph.ps("pq", [128, 512])
                for t in range(NT):
                    sl = slice(t * 512, (t + 1) * 512)
                    m_ = mb[t % 2]; x_ = xf[t % 2]; z = zz[t % 2]
                    ld("sync", m_[:], MIXv[:, :, sl])
                    ld("sync", x_[:], XTv[:, :, sl])
                    for m in range(8):
                        pp = pps[(t * 8 + m) % 6]
                        for k in range(8):
                            S.mm(pp[:], wo[:, k, m * 128:(m + 1) * 128], m_[:, k, :], start=(k == 0), stop=(k == 7))
                        S.stt(z[:, m, :], x_[:, m, :], ALPHA, pp[:], ALU.mult, ALU.add)
                    layer_norm(ph, z, ones, g1, b1, pm, pq, sq, mean, rstd)
                    stq("sync", X1Tv[:, :, sl], z[:])

        def phase_ffn(l, last):
            BL = min(seq, 1024)
            NTB = BL // 512
            moe = (l % 2 == 1)
            li = l // 2
            with Ph() as ph:
                K = ph.consts(["c_ident"] + (["c_sel"] if moe else []))
                ident = K["c_ident"]
                x1b = ph.sb("x1b", [128, 8, BL], BF16)
                yacc = ph.sb("yacc", [128, 8, BL])
                S.split(x1b, 512); S.split(yacc, 512)
                wpe = ph.sb("wpe", [128, 2, DM], BF16)
                ld("gpsimd", wpe[:], D["pe_proj"][l].rearrange("(k p) n -> p k n", p=128))
                wg = ph.sb("wg", [128, 8, DM], BF16)
                ld("gpsimd", wg[:], D["pe_gate_w"][l].rearrange("(k p) n -> p k n", p=128))
                gb = ph.sb("gb", [128, 8]); colvec(gb, D["pe_gate_b"][l])
                g2 = ph.sb("g2", [128, 8]); colvec(g2, D["ln2_g"][l])
                b2 = ph.sb("b2", [128, 8]); colvec(b2, D["ln2_b"][l])
                ones = ph.sb("ones", [128, 128]); S.memset(ones[:], 1.0 / DM)
                if moe:
                    wr = ph.sb("wr", [128, 8, 8])
                    ld("sync", wr[:], D["e_router"][li].rearrange("(k p) e -> p k e", p=128))
                    gfm = ph.sb("gfm", [8, BL])
                    S.split(gfm, 512)
                    Ge = ph.sb("Ge", [128, BL])
                    S.split(Ge, 512)
                x1f = [ph.sb(f"x1f{i}", [128, 8, 512]) for i in range(1)] * 2
                pin = [ph.sb(f"pin{i}", [128, 4, 256]) for i in range(2)]
                pT = [ph.sb(f"pT{i}", [128, 2, 512], BF16) for i in range(2)]
                sgs = [ph.sb(f"sg{i}", [128, 512]) for i in range(2)]
                tmps = [ph.sb(f"tmp{i}", [128, 512]) for i in range(2)]
                pps = [ph.ps(f"pp{i}", [128, 512]) for i in range(6)]
                pm = ph.ps("pm", [128, 512]); pq = ph.ps("pq", [128, 512])
                for tz in x1f:
                    S.split(tz, 512)
                np_ = 0
                NWB = 3
                w1b = [ph.sb(f"w1b{i}", [128, 8, 256], BF16) for i in range(NWB)]
                w3b = [ph.sb(f"w3b{i}", [128, 8, 256], BF16) for i in range(NWB)]
                w2b = [ph.sb(f"w2b{i}", [128, 2, DM], BF16) for i in range(NWB)]
                acts = [ph.sb(f"act{i}", [128, 2, 512], BF16) for i in range(2)]
                s1s = [ph.sb(f"s1{i}", [128, 512]) for i in range(2)]
                t3s = [ph.sb(f"t3{i}", [128, 512]) for i in range(2)]
                sq = x1f[0]; mean = ph.sb("mean", [128, 512]); rstd = ph.sb("rstd", [128, 512])
                zt = [ph.sb(f"zt{i}", [128, 8, 512]) for i in range(1)] * 2
                ot = [ph.sb(f"ot{i}", [128, DM]) for i in range(2)]
                for s in range(T // BL):
                    t0 = s * BL
                    for t in range(NTB):
                        sl = slice(t * 512, (t + 1) * 512)
                        gsl = slice(t0 + t * 512, t0 + (t + 1) * 512)
                        xf_ = x1f[t % 2]; pi_ = pin[t % 2]; pT_ = pT[t % 2]
                        ld("sync", xf_[:], X1Tv[:, :, gsl])
                        ld("gpsimd", x1b[:, :, sl], X1Tv[:, :, gsl])
                        ld("sync", pi_[:], D["p"][l, gsl, :].rearrange("(j p) f -> p j f", p=128))
                        for kk in range(2):
                            pt = pps[np_ % 6]; np_ += 1
                            for j in range(4):
                                S.transpose(pt[:, j * 128:(j + 1) * 128], pi_[:, j, kk * 128:(kk + 1) * 128], ident[:])
                            S.copy(pT_[:, kk, :], pt[:])
                        for m in range(8):
                            pg = pps[np_ % 6]; np_ += 1
                            pp = pps[np_ % 6]; np_ += 1
                            sg = sgs[m % 2]; tmp = tmps[m % 2]
                            for k in range(8):
                                S.mm(pg[:], wg[:, k, m * 128:(m + 1) * 128], x1b[:, k, sl], start=(k == 0), stop=(k == 7))
                            S.act(sg[:], pg[:], AF.Sigmoid, bias=gb[:, m:m + 1])
                            for kk in range(2):
                                S.mm(pp[:], wpe[:, kk, m * 128:(m + 1) * 128], pT_[:, kk, :], start=(kk == 0), stop=(kk == 1))
                            S.tt(tmp[:], pp[:], sg[:], ALU.mult)
                            S.stt(yacc[:, m, sl], xf_[:, m, :], ALPHA, tmp[:], ALU.mult, ALU.add)
                        if moe:
                            for j in range(4):
                                pl = pps[np_ % 6]; np_ += 1
                                for k in range(8):
                                    S.mm(pl[:, 0:8], xf_[:, k, j * 128:(j + 1) * 128], wr[:, k, :], start=(k == 0), stop=(k == 7))
                                lgs = ph.sb(f"lgs{s}_{t}_{j}", [128, 8]); mx = ph.sb(f"mx{s}_{t}_{j}", [128, 8])
                                dd = ph.sb(f"dd{s}_{t}_{j}", [128, 4]); ga = ph.sb(f"ga{s}_{t}_{j}", [128, 8]); gb_ = ph.sb(f"gbb{s}_{t}_{j}", [128, 8])
                                S.copy(lgs[:], pl[:, 0:8])
                                S.op("vector", lambda e, a=mx, b=lgs: e.max(a[:], b[:]), reads=[lgs[:]], writes=[mx[:]])
                                S.tt(dd[:, 0:1], mx[:, 0:1], mx[:, 1:2], ALU.subtract)
                                S.act(dd[:, 1:2], dd[:, 0:1], AF.Sigmoid)
                                S.act(dd[:, 2:3], dd[:, 0:1], AF.Sigmoid, scale=-1.0)
                                S.ts(ga[:], lgs[:], mx[:, 0:1], dd[:, 1:2], op0=ALU.is_equal, op1=ALU.mult)
                                S.ts(gb_[:], lgs[:], mx[:, 1:2], dd[:, 2:3], op0=ALU.is_equal, op1=ALU.mult)
                                S.tt(ga[:], ga[:], gb_[:], ALU.add)
                                pt = pps[np_ % 6]; np_ += 1
                                S.transpose(pt[0:8, 0:128], ga[:], ident[:])
                                S.copy(gfm[:, t * 512 + j * 128:t * 512 + (j + 1) * 128], pt[0:8, 0:128])
                    NE = 8 if moe else 1
                    gi = 0
                    hsets = [(pps[0], pps[1]), (pps[2], pps[3])]
                    ybanks = [pps[4], pps[5], pm, pq]
                    cnt = {"h": 0, "y": 0, "u": 0}
                    pend = [None]

                    def emit_H(w1, w3, sl, ac):
                        for c in range(2):
                            p1, p3 = hsets[cnt["h"] % 2]; cnt["h"] += 1
                            for k in range(8):
                                S.mm(p1[:], w1[:, k, c * 128:(c + 1) * 128], x1b[:, k, sl], start=(k == 0), stop=(k == 7))
                            for k in range(8):
                                S.mm(p3[:], w3[:, k, c * 128:(c + 1) * 128], x1b[:, k, sl], start=(k == 0), stop=(k == 7))
                            s1 = s1s[c]
                            S.act(s1[:], p1[:], AF.Silu)
                            if moe:
                                t3 = t3s[c]
                                S.tt(t3[:], p3[:], s1[:], ALU.mult)
                                S.tt(ac[:, c, :], t3[:], Ge[:, sl], ALU.mult)
                            else:
                                S.tt(ac[:, c, :], p3[:], s1[:], ALU.mult)

                    def emit_Y(w2, sl, ac):
                        for m in range(8):
                            py = ybanks[cnt["y"] % 4]; cnt["y"] += 1
                            for c in range(2):
                                S.mm(py[:], w2[:, c, m * 128:(m + 1) * 128], ac[:, c, :], start=(c == 0), stop=(c == 1))
                            S.tt(yacc[:, m, sl], yacc[:, m, sl], py[:], ALU.add)

                    for e in range(NE):
                        if moe:
                            W1 = D["e_w1"][li, e]; W3 = D["e_w3"][li, e]; W2 = D["e_w2"][li, e]
                            for t in range(NTB):
                                sl = slice(t * 512, (t + 1) * 512)
                                pb = ybanks[cnt["y"] % 4]; cnt["y"] += 1
                                S.mm(pb[:], K["c_sel"][:, e, :], gfm[:, sl])
                                S.act(Ge[:, sl], pb[:], AF.Copy)
                        else:
                            W1 = D["f_w1"][li]; W3 = D["f_w3"][li]; W2 = D["f_w2"][li]
                        W1v = W1.rearrange("(k p) f -> p k f", p=128)
                        W3v = W3.rearrange("(k p) f -> p k f", p=128)
                        W2v = W2.rearrange("(k p) n -> p k n", p=128)
                        for g in range(11):
                            w1 = w1b[gi % NWB]; w3 = w3b[gi % NWB]; w2 = w2b[gi % NWB]; gi += 1
                            ld("gpsimd", w1[:], W1v[:, :, g * 256:(g + 1) * 256])
                            ld("gpsimd", w3[:], W3v[:, :, g * 256:(g + 1) * 256])
                            ld("gpsimd", w2[:], W2v[:, 2 * g:2 * g + 2, :])
                            for t in range(NTB):
                                sl = slice(t * 512, (t + 1) * 512)
                                ac = acts[cnt["u"] % 2]; cnt["u"] += 1
                                emit_H(w1, w3, sl, ac)
                                if pend[0] is not None:
                                    emit_Y(*pend[0])
                                pend[0] = (w2, sl, ac)
                    if pend[0] is not None:
                        emit_Y(*pend[0]); pend[0] = None
                    for t in range(NTB):
                        sl = slice(t * 512, (t + 1) * 512)
                        gsl = slice(t0 + t * 512, t0 + (t + 1) * 512)
                        z = zt[t % 2]
                        S.copy(z[:], yacc[:, :, sl], eng="gpsimd")
                        layer_norm(ph, z, ones, g2, b2, pm, pq, sq, mean, rstd)
                        if not last:
                            stq("sync", XTv[:, :, gsl], z[:])
                        else:
                            for j in range(4):
                                o_ = ot[j % 2]
                                for hh in range(2):
                                    pt = pps[np_ % 6]; np_ += 1
                                    for c4 in range(4):
                                        c = hh * 4 + c4
                                        S.transpose(pt[:, c4 * 128:(c4 + 1) * 128], z[:, c, j * 128:(j + 1) * 128], ident[:])
                                    S.copy(o_[:, hh * 512:(hh + 1) * 512], pt[:])
                                stq("sync", out[t0 + t * 512 + j * 128:t0 + t * 512 + (j + 1) * 128, :], o_[:])


        RMS_EPS = 1e-6

        def bcast_row(ph, name, row_ap, n):
            t = ph.sb(name, [128, n])
            ld("sync", t[:], row_ap.broadcast_to([128, n]))
            return t

        def chunk_consts(ph):
            K = ph.consts(["c_ident", "c_triu", "c_strl"])
            ones = ph.sb("ones1", [128, 128])
            S.memset(ones[:], 1.0)
            return K["c_ident"], K["c_triu"], K["c_strl"], ones

        def phase_ssd(l, s):
            t0 = s * seq
            NCH = seq // 128
            with Ph() as ph:
                ident, triu, strl, ones = chunk_consts(ph)
                cw = ph.sb("cw", [128, 4, 4])
                for j in range(4):
                    for c in range(4):
                        ld("sync", cw[:, c, j:j + 1], D["s_conv_w"][l, j, c * 128:(c + 1) * 128].rearrange("(p o) -> p o", o=1))
                cb = ph.sb("cb", [128, 4]); colvec(cb, D["s_conv_b"][l])
                nw = ph.sb("nw", [128, 2]); colvec(nw, D["s_norm_w"][l])
                dtb = ph.sb("dtb", [4, 1]); ld("sync", dtb[:], D["s_dt_bias"][l].rearrange("(p o) -> p o", o=1))
                nA = bcast_row(ph, "nA", D["s_a_log"][l:l + 1, :], 4)
                S.act(nA[:], nA[:], AF.Exp)
                S.ts(nA[:], nA[:], -1.0, None, op0=ALU.mult)
                dsk = bcast_row(ph, "dsk", D["s_d"][l:l + 1, :], 4)
                z = ph.sb("z", [128, 2, seq])
                xbc = ph.sb("xbc", [128, 4, seq + 3])
                S.memset(xbc[:, :, 0:3], 0.0)
                dtf = ph.sb("dtf", [4, seq])
                S.split(dtf, 128)
                for c in range(2):
                    ld("sync", z[:, c, :], PRJ[OFF_S + c * 128:OFF_S + (c + 1) * 128, t0:t0 + seq])
                for c in range(4):
                    ld("sync", xbc[:, c, 3:3 + seq], PRJ[OFF_S + 256 + c * 128:OFF_S + 256 + (c + 1) * 128, t0:t0 + seq])
                ld("sync", dtf[:], PRJ[OFF_S + 768:OFF_S + 772, t0:t0 + seq])
                xs = ph.sb("xs", [128, 4, seq])
                S.split(xs, 128)
                for c in range(4):
                    S.ts(xs[:, c, :], xbc[:, c, 3:3 + seq], cw[:, c, 3:4], cb[:, c:c + 1], op0=ALU.mult, op1=ALU.add)
                    for j in range(3):
                        S.stt(xs[:, c, :], xbc[:, c, j:j + seq], cw[:, c, j:j + 1], xs[:, c, :], ALU.mult, ALU.add)
                    S.act(xs[:, c, :], xs[:, c, :], AF.Silu)
                S.act(dtf[:], dtf[:], AF.Exp, bias=dtb[:, 0:1])
                S.act(dtf[:], dtf[:], AF.Ln, bias=1.0)
                ysb = ph.sb("ysb", [128, 2, seq])
                S.split(ysb, 128)
                Sst = ph.sb("Sst", [128, 2, 64])
                S.split(Sst, 64)
                S.memset(Sst[:], 0.0)
                pA = ph.ps("pA", [128, 512]); pB = ph.ps("pB", [128, 512])
                pC = [ph.ps(f"pC{i}", [128, 512]) for i in range(2)]
                pD = [ph.ps(f"pD{i}", [128, 512]) for i in range(2)]
                pE = ph.ps("pE", [128, 512])
                def T2(name, shape, dt=F32, gran=None):
                    r = [ph.sb(f"{name}{i}", shape, dt) for i in range(2)]
                    if gran:
                        for t_ in r:
                            S.split(t_, gran)
                    return r
                dtT = T2("dtT", [128, 4]); adt = T2("adt", [128, 4]); acs = T2("acs", [128, 8])
                eacs = T2("eacs", [128, 4]); dec = T2("dec", [128, 4]); eat = T2("eat", [128, 4])
                xtok = T2("xtok", [128, 4, 64], gran=64); btok = T2("btok", [128, 128])
                xdt = T2("xdt", [128, 4, 64], gran=64); xdd = T2("xdd", [128, 4, 64], gran=64)
                Sm = T2("Sm", [128, 2, 128], gran=128); Lh = T2("Lh", [128, 4, 128], gran=128)
                E = T2("E", [128, 4, 128], gran=128); P = T2("P", [128, 4, 128], gran=128)
                ytok = T2("ytok", [128, 4, 64], gran=64)
                for ci in range(NCH):
                    b = ci % 2
                    cs = slice(ci * 128, (ci + 1) * 128)
                    S.transpose(pB[:, 256:260], dtf[0:4, cs], ident[0:4, 0:4])
                    S.copy(dtT[b][:], pB[:, 256:260])
                    S.tt(adt[b][:], dtT[b][:], nA[:], ALU.mult)
                    S.mm(pB[:, 264:268], triu[:], adt[b][:])
                    S.mm(pB[:, 268:272], ones[:], adt[b][:])
                    S.copy(acs[b][:], pB[:, 264:272])
                    S.act(eacs[b][:], acs[b][:, 0:4], AF.Exp)
                    S.act(eat[b][:], acs[b][:, 4:8], AF.Exp)
                    S.tt(dec[b][:], acs[b][:, 4:8], acs[b][:, 0:4], ALU.subtract)
                    S.act(dec[b][:], dec[b][:], AF.Exp)
                    for cx in range(2):
                        S.transpose(pA[:, cx * 128:(cx + 1) * 128], xs[:, cx, cs], ident[:])
                    S.transpose(pA[:, 256:384], xs[:, 2, cs], ident[:])
                    S.copy(xtok[b][:].rearrange("p h d -> p (h d)"), pA[:, 0:256])
                    S.act(btok[b][:], pA[:, 256:384], AF.Copy)
                    S.tt(xdt[b][:], xtok[b][:], dtT[b][:].unsqueeze(2).broadcast_to([128, 4, 64]), ALU.mult)
                    S.tt(xdd[b][:], xdt[b][:], dec[b][:].unsqueeze(2).broadcast_to([128, 4, 64]), ALU.mult, eng="gpsimd")
                    for g in range(2):
                        gs = slice(g * 64, (g + 1) * 64)
                        S.mm(pB[:, g * 128:(g + 1) * 128], xs[gs, 2, cs], xs[gs, 3, cs], sync_self=True)
                        S.tt(Sm[b][:, g, :], pB[:, g * 128:(g + 1) * 128], triu[:], ALU.mult)
                    for h in range(4):
                        S.ts(Lh[b][:, h, :], strl[:], adt[b][:, h:h + 1], None, op0=ALU.mult, eng="gpsimd")
                        S.mm(pC[b][:, h * 128:(h + 1) * 128], Lh[b][:, h, :], triu[:])
                    S.act(E[b][:].rearrange("p h l -> p (h l)"), pC[b][:], AF.Exp)
                    for h in range(4):
                        g = h // 2; j = h % 2
                        gs = slice(g * 64, (g + 1) * 64)
                        S.tt(P[b][:, h, :], E[b][:, h, :], Sm[b][:, g, :], ALU.mult, eng=("gpsimd" if h % 2 else "vector"))
                        S.mm(pD[b][:, h * 64:(h + 1) * 64], P[b][:, h, :], xdt[b][:, h, :])
                        S.mm(pD[b][:, 256 + h * 64:256 + (h + 1) * 64], xs[gs, 3, cs], Sst[gs, j, :], sync_self=True)
                        S.mm(pE[gs, j * 64:(j + 1) * 64], btok[b][:, gs], xdd[b][:, h, :])
                        S.ts(ytok[b][:, h, :], pD[b][:, 256 + h * 64:256 + (h + 1) * 64], eacs[b][:, h:h + 1], None, op0=ALU.mult)
                        S.tt(ytok[b][:, h, :], ytok[b][:, h, :], pD[b][:, h * 64:(h + 1) * 64], ALU.add)
                        S.stt(ytok[b][:, h, :], xtok[b][:, h, :], dsk[:, h:h + 1], ytok[b][:, h, :], ALU.mult, ALU.add)
                        S.stt(Sst[gs, j, :], Sst[gs, j, :], eat[b][gs, h:h + 1], pE[gs, j * 64:(j + 1) * 64], ALU.mult, ALU.add)
                    for cx in range(2):
                        S.transpose(pE[:, 128 + cx * 128:128 + (cx + 1) * 128],
                                    ytok[b][:, 2 * cx:2 * cx + 2, :].rearrange("p h d -> p (h d)"), ident[:])
                        S.act(ysb[:, cx, cs], pE[:, 128 + cx * 128:128 + (cx + 1) * 128], AF.Copy)
                ob = ph.sb("ob", [128, 2, seq], BF16)
                rs = [ph.sb(f"rs{i}", [128, 512]) for i in range(2)]
                for c in range(2):
                    S.act(z[:, c, :], z[:, c, :], AF.Silu)
                    S.tt(ysb[:, c, :], ysb[:, c, :], z[:, c, :], ALU.mult)
                    S.tt(z[:, c, :], ysb[:, c, :], ysb[:, c, :], ALU.mult, eng="gpsimd")
                    for t in range(NTS):
                        sl = slice(t * 512, (t + 1) * 512)
                        pp = pC[t % 2]
                        S.mm(pp[:], ones[:], z[:, c, sl])
                        r_ = rs[t % 2]
                        S.act(r_[:], pp[:], AF.Ln, scale=1.0 / 128.0, bias=RMS_EPS)
                        S.act(r_[:], r_[:], AF.Exp, scale=-0.5)
                        S.tt(r_[:], r_[:], ysb[:, c, sl], ALU.mult)
                        S.ts(ob[:, c, sl], r_[:], nw[:, c:c + 1], None, op0=ALU.mult)
                    stq("sync", MIX[768 + c * 128:768 + (c + 1) * 128, t0:t0 + seq], ob[:, c, :])


        def phase_mlstm(l, s):
            t0 = s * seq
            NCH = seq // 128
            with Ph() as ph:
                ident, triu, strl, ones = chunk_consts(ph)
                m125 = ph.sb("m125", [128, 128])
                S.ts(m125[:], triu[:], 0.125, None, op0=ALU.mult)
                ib = ph.sb("ib", [4, 1]); ld("sync", ib[:], D["m_i_bias"][l].rearrange("(p o) -> p o", o=1))
                nfb = ph.sb("nfb", [4, 1]); ld("sync", nfb[:], D["m_f_bias"][l].rearrange("(p o) -> p o", o=1))
                S.ts(nfb[:], nfb[:], -1.0, None, op0=ALU.mult)
                nw = ph.sb("nw", [128, 2]); colvec(nw, D["m_norm_w"][l])
                q = ph.sb("q", [128, 2, seq]); k = ph.sb("k", [128, 2, seq]); v = ph.sb("v", [128, 2, seq])
                o = ph.sb("o", [128, 2, seq])
                i_fm = ph.sb("i_fm", [4, seq]); f_fm = ph.sb("f_fm", [4, seq])
                hsb = ph.sb("hsb", [128, 2, seq])
                for t_ in (q, k, v, i_fm, f_fm, hsb):
                    S.split(t_, 128)
                for c in range(2):
                    for j_, tt_ in enumerate((q, k, v, o)):
                        ld("sync", tt_[:, c, :], PRJ[j_ * 256 + c * 128:j_ * 256 + (c + 1) * 128, t0:t0 + seq])
                ld("sync", i_fm[:], PRJ[1024:1028, t0:t0 + seq])
                ld("sync", f_fm[:], PRJ[1028:1032, t0:t0 + seq])
                S.ts(i_fm[:], i_fm[:], ib[:, 0:1], None, op0=ALU.add)
                S.act(f_fm[:], f_fm[:], AF.Exp, scale=-1.0, bias=nfb[:, 0:1])
                S.act(f_fm[:], f_fm[:], AF.Ln, bias=1.0)
                S.ts(f_fm[:], f_fm[:], -1.0, None, op0=ALU.mult)
                Cst = ph.sb("Cst", [128, 2, 66])
                S.split(Cst, 66)
                S.memset(Cst[:], 0.0)
                pA = ph.ps("pA", [128, 512]); pB = ph.ps("pB", [128, 512]); pS = ph.ps("pS", [128, 512])
                pC = ph.ps("pC", [128, 512]); pD = ph.ps("pD", [128, 512]); pD2 = ph.ps("pD2", [128, 512])
                pE = ph.ps("pE", [128, 512])

                def T2(name, shape, dt=F32, gran=None):
                    r = [ph.sb(f"{name}{i}", shape, dt) for i in range(2)]
                    if gran:
                        for t_ in r:
                            S.split(t_, gran)
                    return r
                lifT = T2("lifT", [128, 8]); acs = T2("acs", [128, 8]); eb = T2("eb", [128, 4]); eg = T2("eg", [128, 4])
                wd = T2("wd", [128, 4]); ktok = T2("ktok", [128, 4, 64], gran=64)
                vt1 = T2("vt1", [128, 4, 66], gran=66); vw = T2("vw", [128, 4, 66], gran=66)
                for b in range(2):
                    S.memset(vt1[b][:], 0.0)
                    S.memset(vt1[b][:, :, 64:65], 1.0)
                Lh = T2("Lh", [128, 4, 128], gran=128); E = T2("E", [128, 4, 128], gran=128)
                Sm = T2("Sm", [128, 4, 128], gran=128); P = T2("P", [128, 4, 128], gran=128)
                tot = T2("tot", [128, 4, 66], gran=66); dd = T2("dd", [128, 8]); hn = T2("hn", [128, 4, 64], gran=64)
                sq = T2("sq", [128, 4, 64]); ss = T2("ss", [128, 4])
                for ci in range(NCH):
                    b = ci % 2
                    cs = slice(ci * 128, (ci + 1) * 128)
                    S.transpose(pB[:, 256:260], i_fm[0:4, cs], ident[0:4, 0:4])
                    S.transpose(pB[:, 260:264], f_fm[0:4, cs], ident[0:4, 0:4])
                    S.copy(lifT[b][:], pB[:, 256:264])
                    S.mm(pB[:, 264:268], triu[:], lifT[b][:, 4:8])
                    S.mm(pB[:, 268:272], ones[:], lifT[b][:, 4:8])
                    S.copy(acs[b][:], pB[:, 264:272])
                    S.act(eb[b][:], acs[b][:, 0:4], AF.Exp)
                    S.act(eg[b][:], acs[b][:, 4:8], AF.Exp)
                    S.tt(wd[b][:], acs[b][:, 4:8], acs[b][:, 0:4], ALU.subtract)
                    S.tt(wd[b][:], wd[b][:], lifT[b][:, 0:4], ALU.add)
                    S.act(wd[b][:], wd[b][:], AF.Exp)
                    S.ts(wd[b][:], wd[b][:], 0.125, None, op0=ALU.mult)
                    for cx in range(2):
                        S.transpose(pA[:, cx * 128:(cx + 1) * 128], k[:, cx, cs], ident[:])
                        S.transpose(pA[:, 256 + cx * 128:256 + (cx + 1) * 128], v[:, cx, cs], ident[:])
                    S.copy(ktok[b][:].rearrange("p h d -> p (h d)"), pA[:, 0:256])
                    S.act(vt1[b][:, :, 0:64], pA[:, 256:512].rearrange("p (h d) -> p h d", h=4), AF.Copy)
                    S.tt(vw[b][:], vt1[b][:], wd[b][:].unsqueeze(2).broadcast_to([128, 4, 66]), ALU.mult, eng="gpsimd")
                    for h in range(4):
                        hc = h // 2
                        hs = slice((h % 2) * 64, (h % 2) * 64 + 64)
                        S.mm(pS[:, h * 128:(h + 1) * 128], k[hs, hc, cs], q[hs, hc, cs], sync_self=True)
                        S.ts(Lh[b][:, h, :], strl[:], lifT[b][:, 4 + h:5 + h], None, op0=ALU.mult, eng="gpsimd")
                        S.mm(pC[:, h * 128:(h + 1) * 128], Lh[b][:, h, :], triu[:])
                    for h in range(4):
                        S.act(E[b][:, h, :], pC[:, h * 128:(h + 1) * 128], AF.Exp, bias=lifT[b][:, h:h + 1])
                        S.tt(Sm[b][:, h, :], pS[:, h * 128:(h + 1) * 128], m125[:], ALU.mult)
                        S.tt(P[b][:, h, :], E[b][:, h, :], Sm[b][:, h, :], ALU.mult, eng="gpsimd")
                    for h in range(4):
                        hc = h // 2
                        hs = slice((h % 2) * 64, (h % 2) * 64 + 64)
                        S.mm(pD[:, h * 66:(h + 1) * 66], P[b][:, h, :], vt1[b][:, h, :])
                        S.mm(pD2[:, h * 66:(h + 1) * 66], q[hs, hc, cs], Cst[hs, hc, :], sync_self=True)
                        S.mm(pE[hs, hc * 66:(hc + 1) * 66], ktok[b][:, h, :], vw[b][:, h, :])
                    for h in range(4):
                        hc = h // 2
                        hs = slice((h % 2) * 64, (h % 2) * 64 + 64)
                        S.ts(tot[b][:, h, :], pD2[:, h * 66:(h + 1) * 66], eb[b][:, h:h + 1], None, op0=ALU.mult)
                        S.tt(tot[b][:, h, :], tot[b][:, h, :], pD[:, h * 66:(h + 1) * 66], ALU.add)
                        S.stt(Cst[hs, hc, :], Cst[hs, hc, :], eg[b][hs, h:h + 1], pE[hs, hc * 66:(hc + 1) * 66], ALU.mult, ALU.add)
                    den = tot[b][:, :, 64:65].rearrange("p h o -> p (h o)")
                    S.stt(dd[b][:, 0:4], den, -1.0, den, ALU.mult, ALU.max)
                    S.ts(dd[b][:, 0:4], dd[b][:, 0:4], 1.0, None, op0=ALU.max)
                    S.recip(dd[b][:, 4:8], dd[b][:, 0:4])
                    S.tt(hn[b][:], tot[b][:, :, 0:64], dd[b][:, 4:8].unsqueeze(2).broadcast_to([128, 4, 64]), ALU.mult)
                    S.tt(sq[b][:], hn[b][:], hn[b][:], ALU.mult, eng="gpsimd")
                    S.reduce(ss[b][:], sq[b][:], ALU.add, AX.X)
                    S.act(ss[b][:], ss[b][:], AF.Ln, scale=1.0 / 64.0, bias=RMS_EPS)
                    S.act(ss[b][:], ss[b][:], AF.Exp, scale=-0.5)
                    S.tt(hn[b][:], hn[b][:], ss[b][:].unsqueeze(2).broadcast_to([128, 4, 64]), ALU.mult)
                    for cx in range(2):
                        S.transpose(pA[:, cx * 128:(cx + 1) * 128],
                                    hn[b][:, 2 * cx:2 * cx + 2, :].rearrange("p h d -> p (h d)"), ident[:])
                        S.act(hsb[:, cx, cs], pA[:, cx * 128:(cx + 1) * 128], AF.Copy)
                ob = ph.sb("ob", [128, 2, seq], BF16)
                for c in range(2):
                    S.act(o[:, c, :], o[:, c, :], AF.Sigmoid)
                    S.tt(hsb[:, c, :], hsb[:, c, :], o[:, c, :], ALU.mult)
                    S.ts(ob[:, c, :], hsb[:, c, :], nw[:, c:c + 1], None, op0=ALU.mult)
                    stq("sync", MIX[c * 128:(c + 1) * 128, t0:t0 + seq], ob[:, c, :])


        def phase_rwkv(l, s):
            t0 = s * seq
            CH = 64
            NCH = seq // CH
            with Ph() as ph:
                ident, triu, strl, ones = chunk_consts(ph)
                bdo = ph.sb("bdo", [128, 128])
                S.memset(bdo[:], 0.0); S.memset(bdo[0:64, 0:64], 1.0); S.memset(bdo[64:128, 64:128], 1.0)
                mk1 = ph.sb("mk1", [64, 128]); mk2 = ph.sb("mk2", [64, 128])
                S.copy(mk1[:, 64:128], triu[0:64, 0:64])
                S.tt(mk1[:, 0:64], triu[0:64, 0:64], ident[0:64, 0:64], ALU.subtract)
                S.copy(mk2[:, 0:64], mk1[:, 0:64])
                S.ts(mk2[:, 64:128], mk1[:, 64:128], -1.0, None, op0=ALU.mult)
                def cv(name, vec, C=2):
                    t_ = ph.sb(name, [128, C]); colvec(t_, vec); return t_
                mu = cv("mu", D["r_mu"][l], 7)
                omm = ph.sb("omm", [128, 7]); S.ts(omm[:], mu[:], -1.0, 1.0, op0=ALU.mult, op1=ALU.add)
                w0c = cv("w0c", D["r_w0"][l]); a0c = cv("a0c", D["r_a0"][l]); kkc = cv("kkc", D["r_k_k"][l])
                kac = cv("kac", D["r_k_a"][l]); rkc = cv("rkc", D["r_r_k"][l].rearrange("h d -> (h d)"))
                lnw = cv("lnw", D["r_ln_w"][l]); lnb = cv("lnb", D["r_ln_b"][l])
                omka = ph.sb("omka", [128, 2]); S.ts(omka[:], kac[:], -1.0, 1.0, op0=ALU.mult, op1=ALU.add)
                W2p = ph.sb("W2p", [128, 2, 128]); A2p = ph.sb("A2p", [128, 2, 128]); G2p = ph.sb("G2p", [128, 2, 128])
                for t_ in (W2p, A2p, G2p):
                    S.memset(t_[:], 0.0)
                for c in range(2):
                    ld("sync", W2p[0:32, c, :], D["r_w2"][l][:, c * 128:(c + 1) * 128])
                    ld("sync", A2p[32:64, c, :], D["r_a2"][l][:, c * 128:(c + 1) * 128])
                    ld("sync", G2p[64:128, c, :], D["r_g2"][l][:, c * 128:(c + 1) * 128])
                RC = ph.sb("RC", [128, 7, seq + 1])
                S.gran[RC.name] = 64
                S.memset(RC[:, :, 0:1], 0.0)
                for c in range(7):
                    ld("sync", RC[:, c, 1:seq + 1], PRJ[OFF_R + c * 128:OFF_R + (c + 1) * 128, t0:t0 + seq])
                tmp = ph.sb("tmp", [128, seq])
                for c in range(7):
                    S.ts(tmp[:], RC[:, c, 0:seq], mu[:, c:c + 1], None, op0=ALU.mult, eng=("gpsimd" if c % 2 else "vector"))
                    S.stt(RC[:, c, 1:seq + 1], RC[:, c, 1:seq + 1], omm[:, c:c + 1], tmp[:], ALU.mult, ALU.add)
                R_ = lambda c: RC[:, c, 1:seq + 1]
                T6 = R_(6)
                S.act(RC[0:32, 6, 1:seq + 1], RC[0:32, 6, 1:seq + 1], AF.Tanh)
                S.act(RC[64:128, 6, 1:seq + 1], RC[64:128, 6, 1:seq + 1], AF.Sigmoid)
                KK = ph.sb("KK", [128, 2, seq]); Bb = ph.sb("Bb", [128, 2, seq]); LD = ph.sb("LD", [128, 2, seq])
                Gg = ph.sb("Gg", [128, 2, seq]); ysb = ph.sb("ysb", [128, 2, seq])
                for t_ in (KK, Bb, LD, ysb):
                    S.split(t_, 64)
                pb = [ph.ps(f"b{i}", [128, 512]) for i in range(8)]
                rn = [ph.sb(f"rn{i}", [128, 512]) for i in range(2)]
                n = 0
                for c in range(2):
                    for t in range(NTS):
                        sl = slice(t * 512, (t + 1) * 512)
                        sl1 = slice(1 + t * 512, 1 + (t + 1) * 512)
                        pw = pb[n % 8]; pa = pb[(n + 1) % 8]; pg = pb[(n + 2) % 8]; n += 3
                        S.mm(pw[:], W2p[:, c, :], RC[:, 6, sl1])
                        S.mm(pa[:], A2p[:, c, :], RC[:, 6, sl1])
                        S.mm(pg[:], G2p[:, c, :], RC[:, 6, sl1])
                        S.act(LD[:, c, sl], pw[:], AF.Sigmoid, bias=w0c[:, c:c + 1])
                        S.act(Bb[:, c, sl], pa[:], AF.Sigmoid, bias=a0c[:, c:c + 1])
                        S.copy(Gg[:, c, sl], pg[:])
                    S.ts(LD[:, c, :], LD[:, c, :], -0.6065306597126334, None, op0=ALU.mult)
                    S.ts(KK[:, c, :], R_(2 + c), kkc[:, c:c + 1], None, op0=ALU.mult)
                    S.tt(tmp[:], KK[:, c, :], KK[:, c, :], ALU.mult, eng="gpsimd")
                    for t in range(NTS):
                        sl = slice(t * 512, (t + 1) * 512)
                        pp = pb[n % 8]; n += 1
                        S.mm(pp[:], bdo[:], tmp[:, sl])
                        r_ = rn[t % 2]
                        S.ts(r_[:], pp[:], 1e-24, None, op0=ALU.max)
                        S.act(r_[:], r_[:], AF.Ln)
                        S.act(r_[:], r_[:], AF.Exp, scale=-0.5)
                        S.tt(KK[:, c, sl], KK[:, c, sl], r_[:], ALU.mult)
                    S.ts(tmp[:], Bb[:, c, :], kac[:, c:c + 1], omka[:, c:c + 1], op0=ALU.mult, op1=ALU.add)
                    S.tt(R_(2 + c), R_(2 + c), tmp[:], ALU.mult)
                    S.tt(Bb[:, c, :], Bb[:, c, :], KK[:, c, :], ALU.mult, eng="gpsimd")
                Z = ph.sb("Z", [64, 4, 64]); S.split(Z, 64); S.memset(Z[:], 0.0)
                TM = ph.sb("TM", [64, 6, 256]); S.split(TM, 256)
                G4 = ph.sb("G4", [64, 4, 256]); S.split(G4, 256)
                TT = ph.sb("TT", [64, 6, 256]); S.split(TT, 256)
                RH = ph.sb("RH", [64, 4, 128]); LH = ph.sb("LH", [64, 4, 128])
                gC = ph.sb("gC", [64, 8])
                A12 = ph.sb("A12", [64, 4, 128]); MA3 = ph.sb("MA3", [64, 4, 128])
                MN = [ph.sb(f"MN{i}", [64, 4, 128]) for i in range(2)]
                PQ = ph.sb("PQ", [64, 4, 128])
                RHS = ph.sb("RHS", [64, 256]); Us = ph.sb("Us", [64, 256])
                yt = ph.sb("yt", [64, 4, 64]); ysq = ph.sb("ysq", [64, 4, 64]); st4 = ph.sb("st4", [64, 8])
                i64 = ident[0:64, 0:64]
                srcs = [(0, 0), (0, 1), (1, 2), (1, 3), (2, 4), (2, 5)]
                for ci in range(NCH):
                    cs = slice(ci * CH, (ci + 1) * CH)
                    cs1 = slice(1 + ci * CH, 1 + (ci + 1) * CH)
                    for cx in range(2):
                        S.transpose(pb[0][0:64, cx * 128:(cx + 1) * 128], RC[:, 0 + cx, cs1], ident[:])
                        S.transpose(pb[0][0:64, 256 + cx * 128:256 + (cx + 1) * 128], RC[:, 2 + cx, cs1], ident[:])
                        S.transpose(pb[1][0:64, cx * 128:(cx + 1) * 128], RC[:, 4 + cx, cs1], ident[:])
                        S.transpose(pb[1][0:64, 256 + cx * 128:256 + (cx + 1) * 128], KK[:, cx, cs], ident[:])
                        S.transpose(pb[2][0:64, cx * 128:(cx + 1) * 128], Bb[:, cx, cs], ident[:])
                        S.transpose(pb[2][0:64, 256 + cx * 128:256 + (cx + 1) * 128], LD[:, cx, cs], ident[:])
                    S.copy(TM[:, 0:2, :].rearrange("p a f -> p (a f)"), pb[0][0:64, :])
                    S.act(TM[:, 2:4, :].rearrange("p a f -> p (a f)"), pb[1][0:64, :], AF.Copy)
                    S.copy(TM[:, 4:6, :].rearrange("p a f -> p (a f)"), pb[2][0:64, :])
                    S.mm(pb[3][0:64, 0:256], triu[0:64, 0:64], TM[:, 5, :])
                    S.mm(pb[3][0:64, 256:512], ones[0:64, 0:64], TM[:, 5, :])
                    S.act(G4[:, 0, :], pb[3][0:64, 0:256], AF.Exp)
                    S.act(G4[:, 1, :], pb[3][0:64, 0:256], AF.Exp, scale=-1.0)
                    S.tt(G4[:, 2, :], pb[3][0:64, 0:256], TM[:, 5, :], ALU.subtract)
                    S.act(G4[:, 2, :], G4[:, 2, :], AF.Exp)
                    S.act(G4[:, 3, :], pb[3][0:64, 0:256], AF.Copy)
                    S.tt(G4[:, 3, :], pb[3][0:64, 256:512], G4[:, 3, :], ALU.subtract)
                    S.act(G4[:, 3, :], G4[:, 3, :], AF.Exp)
                    S.tt(TT[:, 0, :], TM[:, 1, :], G4[:, 1, :], ALU.mult)
                    S.tt(TT[:, 1, :], TM[:, 4, :], G4[:, 1, :], ALU.mult, eng="gpsimd")
                    S.tt(TT[:, 2, :], TM[:, 3, :], G4[:, 2, :], ALU.mult)
                    S.tt(TT[:, 3, :], TM[:, 0, :], G4[:, 0, :], ALU.mult, eng="gpsimd")
                    S.tt(TT[:, 4, :], TM[:, 1, :], G4[:, 3, :], ALU.mult)
                    S.stt(TT[:, 5, :], TM[:, 4, :], -1.0, G4[:, 3, :], ALU.mult, ALU.mult)
                    for h in range(4):
                        hs = slice(h * 64, (h + 1) * 64)
                        S.transpose(pb[4][0:64, h * 128:h * 128 + 64], TT[:, 2, hs], i64)
                        S.transpose(pb[4][0:64, h * 128 + 64:(h + 1) * 128], TT[:, 3, hs], i64)
                        S.transpose(pb[5][0:64, h * 128:h * 128 + 64], TT[:, 0, hs], i64)
                        S.transpose(pb[5][0:64, h * 128 + 64:(h + 1) * 128], TT[:, 1, hs], i64)
                        S.mm(pb[6][0:64, 2 * h:2 * h + 2], TM[:, 5, hs], ones[0:64, 0:2])
                    S.copy(RH[:].rearrange("p h f -> p (h f)"), pb[4][0:64, :])
                    S.act(LH[:].rearrange("p h f -> p (h f)"), pb[5][0:64, :], AF.Copy)
                    S.act(gC[:], pb[6][0:64, 0:8], AF.Exp)
                    for h in range(4):
                        S.mm(pb[0][0:64, h * 128:(h + 1) * 128], LH[:, h, 0:64], RH[:, h, :])
                        S.mm(pb[1][0:64, h * 128:(h + 1) * 128], LH[:, h, 64:128], RH[:, h, :])
                        S.mm(pb[2][0:64, h * 64:(h + 1) * 64], RH[:, h, 0:64], LH[:, h, 64:128])
                    S.tt(A12[:], pb[0][0:64, :].rearrange("p (h f) -> p h f", h=4), mk1[:].unsqueeze(1).broadcast_to([64, 4, 128]), ALU.mult)
                    S.tt(MA3[:], pb[1][0:64, :].rearrange("p (h f) -> p h f", h=4), mk2[:].unsqueeze(1).broadcast_to([64, 4, 128]), ALU.mult)
                    S.copy(MN[0][:, :, 0:64], MA3[:, :, 0:64], eng="gpsimd")
                    S.tt(MN[0][:, :, 64:128], pb[2][0:64, 0:256].rearrange("p (h f) -> p h f", h=4),
                         strl[0:64, 0:64].unsqueeze(1).broadcast_to([64, 4, 64]), ALU.mult)
                    S.tt(PQ[:, :, 0:64], i64.unsqueeze(1).broadcast_to([64, 4, 64]), MN[0][:, :, 0:64], ALU.subtract)
                    S.tt(PQ[:, :, 64:128], i64.unsqueeze(1).broadcast_to([64, 4, 64]), MN[0][:, :, 64:128], ALU.subtract)
                    cur = 0
                    for lev in range(5):
                        a_, b_ = MN[cur], MN[1 - cur]
                        for h in range(4):
                            S.mm(pb[7][0:64, h * 128:h * 128 + 64], a_[:, h, 64:128], a_[:, h, 0:64])
                            S.mm(pb[7][0:64, h * 128 + 64:(h + 1) * 128], a_[:, h, 0:64], a_[:, h, 64:128])
                        S.copy(b_[:].rearrange("p h f -> p (h f)"), pb[7][0:64, :])
                        for h in range(4):
                            S.mm(pb[6][0:64, h * 128:h * 128 + 64], PQ[:, h, 64:128], b_[:, h, 0:64])
                            S.mm(pb[6][0:64, h * 128 + 64:(h + 1) * 128], b_[:, h, 0:64], PQ[:, h, 64:128])
                        S.tt(PQ[:].rearrange("p h f -> p (h f)"), PQ[:].rearrange("p h f -> p (h f)"), pb[6][0:64, :], ALU.add)
                        cur = 1 - cur
                    for h in range(4):
                        hs = slice(h * 64, (h + 1) * 64)
                        S.mm(pb[3][0:64, hs], RH[:, h, 0:64], Z[:, h, :], start=True, stop=False)
                        S.mm(pb[3][0:64, hs], A12[:, h, 0:64], TM[:, 2, hs], start=False, stop=True)
                    S.copy(RHS[:], pb[3][0:64, 0:256])
                    for h in range(4):
                        hs = slice(h * 64, (h + 1) * 64)
                        S.mm(pb[4][0:64, hs], PQ[:, h, 0:64], RHS[:, hs])
                    S.copy(Us[:], pb[4][0:64, 0:256])
                    for h in range(4):
                        hs = slice(h * 64, (h + 1) * 64)
                        S.mm(pb[5][0:64, hs], RH[:, h, 64:128], Z[:, h, :], start=True, stop=False)
                        S.mm(pb[5][0:64, hs], A12[:, h, 64:128], TM[:, 2, hs], start=False, stop=False)
                        S.mm(pb[5][0:64, hs], MA3[:, h, 64:128], Us[:, hs], start=False, stop=True)
                    for h in range(4):
                        hs = slice(h * 64, (h + 1) * 64)
                        S.mm(pb[0][0:64, hs], TT[:, 4, hs], TM[:, 2, hs], start=True, stop=False)
                        S.mm(pb[0][0:64, hs], TT[:, 5, hs], Us[:, hs], start=False, stop=True)
                    for h in range(4):
                        hs = slice(h * 64, (h + 1) * 64)
                        S.stt(Z[:, h, :], Z[:, h, :], gC[:, 2 * h:2 * h + 1], pb[0][0:64, hs], ALU.mult, ALU.add)
                    S.copy(yt[:].rearrange("p h d -> p (h d)"), pb[5][0:64, 0:256])
                    S.reduce(st4[:, 0:4], yt[:], ALU.add, AX.X)
                    S.ts(st4[:, 0:4], st4[:, 0:4], 1.0 / 64.0, None, op0=ALU.mult)
                    S.tt(yt[:], yt[:], st4[:, 0:4].unsqueeze(2).broadcast_to([64, 4, 64]), ALU.subtract)
                    S.tt(ysq[:], yt[:], yt[:], ALU.mult, eng="gpsimd")
                    S.reduce(st4[:, 4:8], ysq[:], ALU.add, AX.X)
                    S.act(st4[:, 4:8], st4[:, 4:8], AF.Ln, scale=1.0 / 64.0, bias=64e-5)
                    S.act(st4[:, 4:8], st4[:, 4:8], AF.Exp, scale=-0.5)
                    S.tt(yt[:], yt[:], st4[:, 4:8].unsqueeze(2).broadcast_to([64, 4, 64]), ALU.mult)
                    for cx in range(2):
                        S.transpose(pb[1][:, cx * 64:(cx + 1) * 64], yt[:, 2 * cx:2 * cx + 2, :].rearrange("p h d -> p (h d)"), i64)
                        S.act(ysb[:, cx, cs], pb[1][:, cx * 64:(cx + 1) * 64], AF.Copy)
                ob = ph.sb("ob", [128, 2, seq], BF16)
                for c in range(2):
                    S.ts(ysb[:, c, :], ysb[:, c, :], lnw[:, c:c + 1], lnb[:, c:c + 1], op0=ALU.mult, op1=ALU.add)
                    S.tt(tmp[:], R_(0 + c), R_(2 + c), ALU.mult)
                    S.ts(tmp[:], tmp[:], rkc[:, c:c + 1], None, op0=ALU.mult)
                    for t in range(NTS):
                        sl = slice(t * 512, (t + 1) * 512)
                        sl1 = slice(1 + t * 512, 1 + (t + 1) * 512)
                        pp = pb[t % 8]
                        S.mm(pp[:], bdo[:], tmp[:, sl])
                        r_ = rn[t % 2]
                        S.tt(r_[:], pp[:], RC[:, 4 + c, sl1], ALU.mult)
                        S.tt(ysb[:, c, sl], ysb[:, c, sl], r_[:], ALU.add)
                    S.tt(ob[:, c, :], ysb[:, c, :], Gg[:, c, :], ALU.mult)
                    stq("sync", MIX[512 + c * 128:512 + (c + 1) * 128, t0:t0 + seq], ob[:, c, :])


        def rr(gens):
            gens = list(gens)
            while gens:
                for g_ in list(gens):
                    try:
                        next(g_)
                    except StopIteration:
                        gens.remove(g_)

        def phase_rwkv2(l):
            CH = 64
            BLK = 512
            NB = seq // BLK
            NCB = BLK // CH
            with Ph() as ph:
                ident, triu, strl, ones = chunk_consts(ph)
                bdo = ph.sb("bdo", [128, 128])
                S.memset(bdo[:], 0.0); S.memset(bdo[0:64, 0:64], 1.0); S.memset(bdo[64:128, 64:128], 1.0)
                mk1 = ph.sb("mk1", [64, 128]); mk2 = ph.sb("mk2", [64, 128])
                S.copy(mk1[:, 64:128], triu[0:64, 0:64])
                S.tt(mk1[:, 0:64], triu[0:64, 0:64], ident[0:64, 0:64], ALU.subtract)
                S.copy(mk2[:, 0:64], mk1[:, 0:64])
                S.ts(mk2[:, 64:128], mk1[:, 64:128], -1.0, None, op0=ALU.mult)
                def cv(name, vec, C=2):
                    t_ = ph.sb(name, [128, C]); colvec(t_, vec); return t_
                mu = cv("mu", D["r_mu"][l], 7)
                omm = ph.sb("omm", [128, 7]); S.ts(omm[:], mu[:], -1.0, 1.0, op0=ALU.mult, op1=ALU.add)
                w0c = cv("w0c", D["r_w0"][l]); a0c = cv("a0c", D["r_a0"][l]); kkc = cv("kkc", D["r_k_k"][l])
                kac = cv("kac", D["r_k_a"][l]); rkc = cv("rkc", D["r_r_k"][l].rearrange("h d -> (h d)"))
                lnw = cv("lnw", D["r_ln_w"][l]); lnb = cv("lnb", D["r_ln_b"][l])
                omka = ph.sb("omka", [128, 2]); S.ts(omka[:], kac[:], -1.0, 1.0, op0=ALU.mult, op1=ALU.add)
                W2p = ph.sb("W2p", [128, 2, 128]); A2p = ph.sb("A2p", [128, 2, 128]); G2p = ph.sb("G2p", [128, 2, 128])
                for t_ in (W2p, A2p, G2p):
                    S.memset(t_[:], 0.0)
                for c in range(2):
                    ld("sync", W2p[0:32, c, :], D["r_w2"][l][:, c * 128:(c + 1) * 128])
                    ld("sync", A2p[32:64, c, :], D["r_a2"][l][:, c * 128:(c + 1) * 128])
                    ld("sync", G2p[64:128, c, :], D["r_g2"][l][:, c * 128:(c + 1) * 128])
                i64 = ident[0:64, 0:64]
                F32R = mybir.dt.float32r
                Rr = lambda ap: ap.bitcast(F32R)
                triuR = ph.sb("triuR", [64, 64], F32R); S.copy(triuR[:], triu[0:64, 0:64])
                onesR = ph.sb("onesR", [64, 64], F32R); S.copy(onesR[:], ones[0:64, 0:64])

                def mkchain(q):
                    C = Ctx()
                    n_ = lambda nm: f"{nm}q{q}"
                    C.pb = [ph.ps(n_(f"b{i}"), [128, 512]) for i in range(4)]
                    C.RC = ph.sb(n_("RC"), [128, 7, BLK + 1]); S.gran[C.RC.name] = 64
                    C.tmp = ph.sb(n_("tmp"), [128, BLK])
                    C.KK = ph.sb(n_("KK"), [128, 2, BLK]); C.Bb = ph.sb(n_("Bb"), [128, 2, BLK])
                    C.LD = ph.sb(n_("LD"), [128, 2, BLK]); C.Gg = ph.sb(n_("Gg"), [128, 2, BLK])
                    C.ysb = ph.sb(n_("ysb"), [128, 2, BLK]); C.ob = ph.sb(n_("ob"), [128, 2, BLK], BF16)
                    for t_ in (C.KK, C.Bb, C.LD, C.ysb):
                        S.split(t_, 64)
                    C.rn = ph.sb(n_("rn"), [128, 512])
                    C.Z = ph.sb(n_("Z"), [64, 4, 64]); S.split(C.Z, 64)
                    for hh_ in range(2):
                        S.ts(C.Z[:, 2 * hh_:2 * hh_ + 2, :].rearrange("p h d -> p (h d)").bitcast(mybir.dt.float32r), ones[0:64, 0:128], 0.0, None, op0=ALU.mult)
                    C.TM = [ph.sb(n_(f"TM{i}"), [64, 6, 256]) for i in range(2)]; [S.split(t_, 256) for t_ in C.TM]
                    C.G4 = ph.sb(n_("G4"), [64, 4, 256]); S.split(C.G4, 256)
                    C.TT = [ph.sb(n_(f"TT{i}"), [64, 6, 256]) for i in range(2)]; [S.split(t_, 256) for t_ in C.TT]
                    C.RH = [ph.sb(n_(f"RH{i}"), [64, 4, 128]) for i in range(2)]; C.LH = ph.sb(n_("LH"), [64, 4, 128])
                    C.gC = [ph.sb(n_(f"gC{i}"), [64, 8]) for i in range(2)]
                    C.A12 = [ph.sb(n_(f"A12{i}"), [64, 4, 128]) for i in range(2)]; C.MA3 = [ph.sb(n_(f"MA3{i}"), [64, 4, 128]) for i in range(2)]
                    C.MN = [ph.sb(n_(f"MN{i}"), [64, 4, 128]) for i in range(2)]
                    C.PQ = [ph.sb(n_(f"PQ{i}"), [64, 4, 128]) for i in range(2)]
                    C.RHS = ph.sb(n_("RHS"), [64, 256]); C.Us = ph.sb(n_("Us"), [64, 256])
                    C.yt = ph.sb(n_("yt"), [64, 4, 64]); C.ysq = ph.sb(n_("ysq"), [64, 4, 64]); C.st4 = ph.sb(n_("st4"), [64, 8])
                    return C

                def zip_rr(subs):
                    subs = list(subs)
                    while subs:
                        for g_ in list(subs):
                            try:
                                next(g_)
                            except StopIteration:
                                subs.remove(g_)
                        yield

                def prep_gen(C, ci, p):
                    cs = slice(ci * CH, (ci + 1) * CH)
                    cs1 = slice(1 + ci * CH, 1 + (ci + 1) * CH)
                    RC, KK, Bb, LD, ysb = C.RC, C.KK, C.Bb, C.LD, C.ysb
                    pA, pB, pC, pD = C.pb
                    Z, G4, LH, MN = C.Z, C.G4, C.LH, C.MN
                    TM, TT, RH, gC, A12, MA3, PQ = C.TM[p], C.TT[p], C.RH[p], C.gC[p], C.A12[p], C.MA3[p], C.PQ[p]
                    RHS, Us, yt, ysq, st4 = C.RHS, C.Us, C.yt, C.ysq, C.st4
                    for cx in range(2):
                        S.transpose(pA[0:64, cx * 128:(cx + 1) * 128], RC[:, 0 + cx, cs1], ident[:])
                        S.transpose(pA[0:64, 256 + cx * 128:256 + (cx + 1) * 128], RC[:, 2 + cx, cs1], ident[:])
                        S.transpose(pB[0:64, cx * 128:(cx + 1) * 128], RC[:, 4 + cx, cs1], ident[:])
                        S.transpose(pB[0:64, 256 + cx * 128:256 + (cx + 1) * 128], KK[:, cx, cs], ident[:])
                        S.transpose(pC[0:64, cx * 128:(cx + 1) * 128], Bb[:, cx, cs], ident[:])
                        S.transpose(pC[0:64, 256 + cx * 128:256 + (cx + 1) * 128], LD[:, cx, cs], ident[:])
                    yield
                    S.copy(Rr(TM[:, 0:2, :].rearrange("p a f -> p (a f)")), pA[0:64, :])
                    S.act(Rr(TM[:, 2:4, :].rearrange("p a f -> p (a f)")), pB[0:64, :], AF.Copy)
                    S.act(Rr(TM[:, 4:6, :].rearrange("p a f -> p (a f)")), pC[0:64, :], AF.Copy)
                    yield
                    S.mm(pA[0:64, 0:256], triuR[:], Rr(TM[:, 5, :]))
                    S.mm(pA[0:64, 256:512], onesR[:], Rr(TM[:, 5, :]))
                    yield
                    S.act(G4[:, 0, :], pA[0:64, 0:256], AF.Exp)
                    S.act(G4[:, 1, :], pA[0:64, 0:256], AF.Exp, scale=-1.0)
                    S.tt(G4[:, 2, :], pA[0:64, 0:256], TM[:, 5, :], ALU.subtract)
                    yield
                    S.act(G4[:, 3, :], pA[0:64, 0:256], AF.Copy)
                    S.act(G4[:, 2, :], G4[:, 2, :], AF.Exp)
                    S.tt(G4[:, 3, :], pA[0:64, 256:512], G4[:, 3, :], ALU.subtract)
                    yield
                    S.act(G4[:, 3, :], G4[:, 3, :], AF.Exp)
                    S.tt(Rr(TT[:, 0, :]), TM[:, 1, :], G4[:, 1, :], ALU.mult, eng="gpsimd")
                    S.tt(Rr(TT[:, 1, :]), TM[:, 4, :], G4[:, 1, :], ALU.mult, eng="gpsimd")
                    yield
                    S.tt(Rr(TT[:, 2, :]), TM[:, 3, :], G4[:, 2, :], ALU.mult)
                    S.tt(Rr(TT[:, 3, :]), TM[:, 0, :], G4[:, 0, :], ALU.mult, eng="gpsimd")
                    yield
                    S.tt(Rr(TT[:, 4, :]), TM[:, 1, :], G4[:, 3, :], ALU.mult, eng="gpsimd")
                    S.stt(Rr(TT[:, 5, :]), TM[:, 4, :], -1.0, G4[:, 3, :], ALU.mult, ALU.mult)
                    yield
                    for h in range(4):
                        hs = slice(h * 64, (h + 1) * 64)
                        S.transpose(pB[0:64, h * 128:h * 128 + 64], TT[:, 2, hs], i64)
                        S.transpose(pB[0:64, h * 128 + 64:(h + 1) * 128], TT[:, 3, hs], i64)
                        S.transpose(pC[0:64, h * 128:h * 128 + 64], TT[:, 0, hs], i64)
                        S.transpose(pC[0:64, h * 128 + 64:(h + 1) * 128], TT[:, 1, hs], i64)
                        S.mm(pA[0:64, 2 * h:2 * h + 2], Rr(TM[:, 5, hs]), onesR[:, 0:2])
                    yield
                    S.act(Rr(RH[:].rearrange("p h f -> p (h f)")), pB[0:64, :], AF.Copy)
                    S.act(Rr(LH[:].rearrange("p h f -> p (h f)")), pC[0:64, :], AF.Copy)
                    S.act(gC[:], pA[0:64, 0:8], AF.Exp)
                    yield
                    for h in range(4):
                        S.mm(pA[0:64, h * 128:(h + 1) * 128], Rr(LH[:, h, 0:64]), Rr(RH[:, h, :]))
                        S.mm(pB[0:64, h * 128:(h + 1) * 128], Rr(LH[:, h, 64:128]), Rr(RH[:, h, :]))
                        S.mm(pC[0:64, h * 64:(h + 1) * 64], Rr(RH[:, h, 0:64]), Rr(LH[:, h, 64:128]))
                    yield
                    S.tt(Rr(A12[:]), pA[0:64, :].rearrange("p (h f) -> p h f", h=4), mk1[:].unsqueeze(1).broadcast_to([64, 4, 128]), ALU.mult)
                    S.tt(Rr(MA3[:]), pB[0:64, :].rearrange("p (h f) -> p h f", h=4), mk2[:].unsqueeze(1).broadcast_to([64, 4, 128]), ALU.mult)
                    yield
                    S.copy(Rr(MN[0][:, :, 0:64]), MA3[:, :, 0:64], eng="gpsimd")
                    S.tt(Rr(MN[0][:, :, 64:128]), pC[0:64, 0:256].rearrange("p (h f) -> p h f", h=4),
                         strl[0:64, 0:64].unsqueeze(1).broadcast_to([64, 4, 64]), ALU.mult)
                    yield
                    S.tt(Rr(PQ[:, :, 0:64]), i64.unsqueeze(1).broadcast_to([64, 4, 64]), MN[0][:, :, 0:64], ALU.subtract, eng="gpsimd")
                    S.tt(Rr(PQ[:, :, 64:128]), i64.unsqueeze(1).broadcast_to([64, 4, 64]), MN[0][:, :, 64:128], ALU.subtract)
                    yield
                    cur = 0
                    for lev in range(5):
                        a_, b_ = MN[cur], MN[1 - cur]
                        for h in range(4):
                            S.mm(pA[0:64, h * 128:h * 128 + 64], Rr(a_[:, h, 64:128]), Rr(a_[:, h, 0:64]))
                            S.mm(pA[0:64, h * 128 + 64:(h + 1) * 128], Rr(a_[:, h, 0:64]), Rr(a_[:, h, 64:128]))
                        yield
                        S.act(Rr(b_[:].rearrange("p h f -> p (h f)")), pA[0:64, :], AF.Copy)
                        yield
                        for h in range(4):
                            S.mm(pB[0:64, h * 128:h * 128 + 64], Rr(PQ[:, h, 64:128]), Rr(b_[:, h, 0:64]))
                            S.mm(pB[0:64, h * 128 + 64:(h + 1) * 128], Rr(b_[:, h, 0:64]), Rr(PQ[:, h, 64:128]))
                        yield
                        S.tt(Rr(PQ[:].rearrange("p h f -> p (h f)")), PQ[:].rearrange("p h f -> p (h f)"), pB[0:64, :], ALU.add)
                        yield
                        cur = 1 - cur

                def state_gen(C, ci, p):
                    cs = slice(ci * CH, (ci + 1) * CH)
                    cs1 = slice(1 + ci * CH, 1 + (ci + 1) * CH)
                    RC, KK, Bb, LD, ysb = C.RC, C.KK, C.Bb, C.LD, C.ysb
                    pA, pB, pC, pD = C.pb
                    Z, G4, LH, MN = C.Z, C.G4, C.LH, C.MN
                    TM, TT, RH, gC, A12, MA3, PQ = C.TM[p], C.TT[p], C.RH[p], C.gC[p], C.A12[p], C.MA3[p], C.PQ[p]
                    RHS, Us, yt, ysq, st4 = C.RHS, C.Us, C.yt, C.ysq, C.st4
                    for h in range(4):
                        hs = slice(h * 64, (h + 1) * 64)
                        S.mm(pD[0:64, hs], Rr(RH[:, h, 0:64]), Rr(Z[:, h, :]), start=True, stop=False)
                        S.mm(pD[0:64, hs], Rr(A12[:, h, 0:64]), Rr(TM[:, 2, hs]), start=False, stop=True)
                    yield
                    S.act(Rr(RHS[:]), pD[0:64, 0:256], AF.Copy)
                    yield
                    for h in range(4):
                        hs = slice(h * 64, (h + 1) * 64)
                        S.mm(pD[0:64, 256 + h * 64:256 + (h + 1) * 64], Rr(PQ[:, h, 0:64]), Rr(RHS[:, hs]))
                    yield
                    S.act(Rr(Us[:]), pD[0:64, 256:512], AF.Copy)
                    yield
                    for h in range(4):
                        hs = slice(h * 64, (h + 1) * 64)
                        S.mm(pD[0:64, hs], Rr(RH[:, h, 64:128]), Rr(Z[:, h, :]), start=True, stop=False)
                        S.mm(pD[0:64, hs], Rr(A12[:, h, 64:128]), Rr(TM[:, 2, hs]), start=False, stop=False)
                        S.mm(pD[0:64, hs], Rr(MA3[:, h, 64:128]), Rr(Us[:, hs]), start=False, stop=True)
                    for h in range(4):
                        hs = slice(h * 64, (h + 1) * 64)
                        S.mm(pD[0:64, 256 + h * 64:256 + (h + 1) * 64], Rr(TT[:, 4, hs]), Rr(TM[:, 2, hs]), start=True, stop=False)
                        S.mm(pD[0:64, 256 + h * 64:256 + (h + 1) * 64], Rr(TT[:, 5, hs]), Rr(Us[:, hs]), start=False, stop=True)
                    yield
                    for h in range(4):
                        hs = slice(h * 64, (h + 1) * 64)
                        S.stt(Rr(Z[:, h, :]), Z[:, h, :], gC[:, 2 * h:2 * h + 1], pD[0:64, 256 + h * 64:256 + (h + 1) * 64], ALU.mult, ALU.add)
                    S.act(yt[:].rearrange("p h d -> p (h d)"), pD[0:64, 0:256], AF.Copy)
                    yield
                    S.reduce(st4[:, 0:4], yt[:], ALU.add, AX.X)
                    yield
                    S.stt(yt[:], st4[:, 0:4].unsqueeze(2).broadcast_to([64, 4, 64]), -1.0 / 64.0, yt[:], ALU.mult, ALU.add)
                    yield
                    S.tt(ysq[:], yt[:], yt[:], ALU.mult, eng="gpsimd")
                    yield
                    S.reduce(st4[:, 4:8], ysq[:], ALU.add, AX.X)
                    yield
                    S.act(st4[:, 4:8], st4[:, 4:8], AF.Ln, scale=1.0 / 64.0, bias=64e-5)
                    S.act(st4[:, 4:8], st4[:, 4:8], AF.Exp, scale=-0.5)
                    yield
                    S.tt(yt[:], yt[:], st4[:, 4:8].unsqueeze(2).broadcast_to([64, 4, 64]), ALU.mult, eng="gpsimd")
                    yield
                    for cx in range(2):
                        S.transpose(pD[:, cx * 64:(cx + 1) * 64], yt[:, 2 * cx:2 * cx + 2, :].rearrange("p h d -> p (h d)"), i64)
                    yield
                    for cx in range(2):
                        S.act(ysb[:, cx, cs], pD[:, cx * 64:(cx + 1) * 64], AF.Copy)
                    yield

                def block_gen(C, q, blk):
                    g0 = q * seq + blk * BLK
                    RC, tmp, KK, Bb, LD, Gg, ysb, ob, rn_ = C.RC, C.tmp, C.KK, C.Bb, C.LD, C.Gg, C.ysb, C.ob, C.rn
                    pA, pB, pC, pD = C.pb
                    RHS, Us, yt, ysq, st4 = C.RHS, C.Us, C.yt, C.ysq, C.st4
                    R_ = lambda c: RC[:, c, 1:BLK + 1]
                    if blk == 0:
                        S.memset(RC[:, :, 0:1], 0.0)
                        for c in range(7):
                            ld("sync", RC[:, c, 1:BLK + 1], PRJ[OFF_R + c * 128:OFF_R + (c + 1) * 128, g0:g0 + BLK])
                    else:
                        for c in range(7):
                            ld("sync", RC[:, c, 0:BLK + 1], PRJ[OFF_R + c * 128:OFF_R + (c + 1) * 128, g0 - 1:g0 + BLK])
                    yield
                    for c in range(7):
                        S.ts(tmp[:], RC[:, c, 0:BLK], mu[:, c:c + 1], None, op0=ALU.mult, eng="gpsimd")
                        S.stt(RC[:, c, 1:BLK + 1], RC[:, c, 1:BLK + 1], omm[:, c:c + 1], tmp[:], ALU.mult, ALU.add)
                        yield
                    S.act(RC[0:32, 6, 1:BLK + 1], RC[0:32, 6, 1:BLK + 1], AF.Tanh)
                    S.act(RC[64:128, 6, 1:BLK + 1], RC[64:128, 6, 1:BLK + 1], AF.Sigmoid)
                    yield
                    sl = slice(0, BLK)
                    sl1 = slice(1, BLK + 1)
                    for c in range(2):
                        S.mm(pA[:], W2p[:, c, :], RC[:, 6, sl1])
                        S.mm(pB[:], A2p[:, c, :], RC[:, 6, sl1])
                        S.mm(pC[:], G2p[:, c, :], RC[:, 6, sl1])
                        S.act(LD[:, c, sl], pA[:], AF.Sigmoid, bias=w0c[:, c:c + 1])
                        S.act(Bb[:, c, sl], pB[:], AF.Sigmoid, bias=a0c[:, c:c + 1])
                        S.copy(Gg[:, c, sl], pC[:])
                        yield
                        S.ts(LD[:, c, :], LD[:, c, :], -0.6065306597126334, None, op0=ALU.mult, eng="gpsimd")
                        S.ts(KK[:, c, :], R_(2 + c), kkc[:, c:c + 1], None, op0=ALU.mult)
                        S.tt(tmp[:], KK[:, c, :], KK[:, c, :], ALU.mult, eng="gpsimd")
                        yield
                        S.mm(pD[:], bdo[:], tmp[:, sl])
                        S.ts(rn_[:], pD[:], 1e-24, None, op0=ALU.max)
                        S.act(rn_[:], rn_[:], AF.Ln)
                        S.act(rn_[:], rn_[:], AF.Exp, scale=-0.5)
                        yield
                        S.tt(KK[:, c, sl], KK[:, c, sl], rn_[:], ALU.mult)
                        S.ts(tmp[:], Bb[:, c, :], kac[:, c:c + 1], omka[:, c:c + 1], op0=ALU.mult, op1=ALU.add)
                        S.tt(R_(2 + c), R_(2 + c), tmp[:], ALU.mult)
                        S.tt(Bb[:, c, :], Bb[:, c, :], KK[:, c, :], ALU.mult, eng="gpsimd")
                        yield
                    yield from prep_gen(C, 0, 0)
                    for ci in range(NCB):
                        subs = [state_gen(C, ci, ci % 2)]
                        if ci + 1 < NCB:
                            subs.append(prep_gen(C, ci + 1, (ci + 1) % 2))
                        yield from zip_rr(subs)
                    for c in range(2):
                        S.ts(ysb[:, c, :], ysb[:, c, :], lnw[:, c:c + 1], lnb[:, c:c + 1], op0=ALU.mult, op1=ALU.add)
                        S.tt(tmp[:], R_(0 + c), R_(2 + c), ALU.mult, eng="gpsimd")
                        S.ts(tmp[:], tmp[:], rkc[:, c:c + 1], None, op0=ALU.mult, eng="gpsimd")
                        yield
                        S.mm(pD[:], bdo[:], tmp[:, sl])
                        S.tt(rn_[:], pD[:], RC[:, 4 + c, sl1], ALU.mult)
                        S.tt(ysb[:, c, sl], ysb[:, c, sl], rn_[:], ALU.add)
                        yield
                        S.tt(ob[:, c, :], ysb[:, c, :], Gg[:, c, :], ALU.mult)
                        stq("sync", MIX[512 + c * 128:512 + (c + 1) * 128, g0:g0 + BLK], ob[:, c, :])
                        yield

                chains = [mkchain(q) for q in range(2)]
                for blk in range(NB):
                    rr([block_gen(chains[q], q, blk) for q in range(2)])


        def phase_ssd2(l):
            NCH = seq // 128
            with Ph() as ph:
                ident, triu, strl, ones = chunk_consts(ph)
                cw = ph.sb("cw", [128, 4, 4])
                for j in range(4):
                    for c in range(4):
                        ld("sync", cw[:, c, j:j + 1], D["s_conv_w"][l, j, c * 128:(c + 1) * 128].rearrange("(p o) -> p o", o=1))
                cb = ph.sb("cb", [128, 4]); colvec(cb, D["s_conv_b"][l])
                nw = ph.sb("nw", [128, 2]); colvec(nw, D["s_norm_w"][l])
                dtb = ph.sb("dtb", [4, 1]); ld("sync", dtb[:], D["s_dt_bias"][l].rearrange("(p o) -> p o", o=1))
                nA = bcast_row(ph, "nA", D["s_a_log"][l:l + 1, :], 4)
                S.act(nA[:], nA[:], AF.Exp)
                S.ts(nA[:], nA[:], -1.0, None, op0=ALU.mult)
                dsk = bcast_row(ph, "dsk", D["s_d"][l:l + 1, :], 4)

                def mkchain(q):
                    C = Ctx()
                    n_ = lambda nm: f"{nm}q{q}"
                    C.pb = [ph.ps(n_(f"P{i}"), [128, 512]) for i in range(4)]
                    C.xs = ph.sb(n_("xs"), [128, 4, seq]); S.split(C.xs, 128)
                    C.ysb = ph.sb(n_("ysb"), [128, 2, seq]); S.split(C.ysb, 128)
                    C.dtf = ph.sb(n_("dtf"), [4, seq]); S.split(C.dtf, 128)
                    C.tmpc = ph.sb(n_("tmpc"), [128, seq + 3])
                    C.ob = ph.sb(n_("ob"), [128, seq], BF16)
                    C.rs = ph.sb(n_("rs"), [128, 512])
                    C.Sst = ph.sb(n_("Sst"), [128, 2, 64]); S.split(C.Sst, 64); S.memset(C.Sst[:], 0.0)
                    for nm, shp, gr in (("dtT", [128, 4], None), ("adt", [128, 4], None), ("acs", [128, 8], None),
                                        ("eacs", [128, 4], None), ("dec", [128, 4], None), ("eat", [128, 4], None),
                                        ("xtok", [128, 4, 64], 64), ("btok", [128, 128], None), ("xdt", [128, 4, 64], 64),
                                        ("xdd", [128, 4, 64], 64), ("Sm", [128, 2, 128], 128), ("Lh", [128, 4, 128], 128),
                                        ("E", [128, 4, 128], 128), ("P", [128, 4, 128], 128), ("ytok", [128, 4, 64], 64)):
                        t_ = ph.sb(n_(nm), shp)
                        if gr:
                            S.split(t_, gr)
                        setattr(C, nm, t_)
                    return C

                def chain_gen(C, q):
                    t0 = q * seq
                    P0, P1, P2, P3 = C.pb
                    xs, ysb, dtf, tmpc, ob, rs, Sst = C.xs, C.ysb, C.dtf, C.tmpc, C.ob, C.rs, C.Sst
                    dtT, adt, acs, eacs, dec, eat = C.dtT, C.adt, C.acs, C.eacs, C.dec, C.eat
                    xtok, btok, xdt, xdd, Sm, Lh, E, P, ytok = C.xtok, C.btok, C.xdt, C.xdd, C.Sm, C.Lh, C.E, C.P, C.ytok
                    S.memset(tmpc[:, 0:3], 0.0)
                    ld("sync", dtf[:], PRJ[OFF_S + 768:OFF_S + 772, t0:t0 + seq])
                    for c in range(4):
                        ld("sync", tmpc[:, 3:3 + seq], PRJ[OFF_S + 256 + c * 128:OFF_S + 256 + (c + 1) * 128, t0:t0 + seq])
                        yield
                        S.ts(xs[:, c, :], tmpc[:, 3:3 + seq], cw[:, c, 3:4], cb[:, c:c + 1], op0=ALU.mult, op1=ALU.add)
                        yield
                        for j in range(3):
                            S.stt(xs[:, c, :], tmpc[:, j:j + seq], cw[:, c, j:j + 1], xs[:, c, :], ALU.mult, ALU.add)
                            yield
                        S.act(xs[:, c, :], xs[:, c, :], AF.Silu)
                        yield
                    S.act(dtf[:], dtf[:], AF.Exp, bias=dtb[:, 0:1])
                    S.act(dtf[:], dtf[:], AF.Ln, bias=1.0)
                    yield
                    for ci in range(NCH):
                        cs = slice(ci * 128, (ci + 1) * 128)
                        S.transpose(P1[:, 256:260], dtf[0:4, cs], ident[0:4, 0:4])
                        for cx in range(2):
                            S.transpose(P0[:, cx * 128:(cx + 1) * 128], xs[:, cx, cs], ident[:])
                        S.transpose(P0[:, 256:384], xs[:, 2, cs], ident[:])
                        yield
                        S.copy(dtT[:], P1[:, 256:260])
                        yield
                        S.tt(adt[:], dtT[:], nA[:], ALU.mult)
                        S.act(btok[:], P0[:, 256:384], AF.Copy)
                        yield
                        S.mm(P1[:, 264:268], triu[:], adt[:])
                        S.mm(P1[:, 268:272], ones[:], adt[:])
                        S.copy(xtok[:].rearrange("p h d -> p (h d)"), P0[:, 0:256])
                        yield
                        S.copy(acs[:], P1[:, 264:272])
                        yield
                        S.act(eacs[:], acs[:, 0:4], AF.Exp)
                        S.act(eat[:], acs[:, 4:8], AF.Exp)
                        S.tt(dec[:], acs[:, 4:8], acs[:, 0:4], ALU.subtract)
                        yield
                        S.act(dec[:], dec[:], AF.Exp)
                        S.tt(xdt[:], xtok[:], dtT[:].unsqueeze(2).broadcast_to([128, 4, 64]), ALU.mult)
                        yield
                        S.tt(xdd[:], xdt[:], dec[:].unsqueeze(2).broadcast_to([128, 4, 64]), ALU.mult, eng="gpsimd")
                        for g in range(2):
                            gs = slice(g * 64, (g + 1) * 64)
                            S.mm(P1[:, g * 128:(g + 1) * 128], xs[gs, 2, cs], xs[gs, 3, cs], sync_self=True)
                        yield
                        for h in range(4):
                            S.ts(Lh[:, h, :], strl[:], adt[:, h:h + 1], None, op0=ALU.mult, eng="gpsimd")
                            S.mm(P2[:, h * 128:(h + 1) * 128], Lh[:, h, :], triu[:])
                            yield
                        for g in range(2):
                            S.tt(Sm[:, g, :], P1[:, g * 128:(g + 1) * 128], triu[:], ALU.mult)
                        S.act(E[:].rearrange("p h l -> p (h l)"), P2[:], AF.Exp)
                        yield
                        for h in range(4):
                            g = h // 2; j = h % 2
                            gs = slice(g * 64, (g + 1) * 64)
                            S.tt(P[:, h, :], E[:, h, :], Sm[:, g, :], ALU.mult, eng=("gpsimd" if h % 2 else "vector"))
                            S.mm(P3[:, h * 64:(h + 1) * 64], P[:, h, :], xdt[:, h, :])
                            S.mm(P3[:, 256 + h * 64:256 + (h + 1) * 64], xs[gs, 3, cs], Sst[gs, j, :], sync_self=True)
                            S.mm(P0[gs, 384 + j * 64:384 + (j + 1) * 64], btok[:, gs], xdd[:, h, :])
                            yield
                        for h in range(4):
                            g = h // 2; j = h % 2
                            gs = slice(g * 64, (g + 1) * 64)
                            S.ts(ytok[:, h, :], P3[:, 256 + h * 64:256 + (h + 1) * 64], eacs[:, h:h + 1], None, op0=ALU.mult)
                            S.stt(Sst[gs, j, :], Sst[gs, j, :], eat[gs, h:h + 1], P0[gs, 384 + j * 64:384 + (j + 1) * 64], ALU.mult, ALU.add)
                            yield
                            S.tt(ytok[:, h, :], ytok[:, h, :], P3[:, h * 64:(h + 1) * 64], ALU.add)
                            yield
                            S.stt(ytok[:, h, :], xtok[:, h, :], dsk[:, h:h + 1], ytok[:, h, :], ALU.mult, ALU.add)
                            yield
                        for cx in range(2):
                            S.transpose(P1[:, cx * 128:(cx + 1) * 128],
                                        ytok[:, 2 * cx:2 * cx + 2, :].rearrange("p h d -> p (h d)"), ident[:])
                        yield
                        for cx in range(2):
                            S.act(ysb[:, cx, cs], P1[:, cx * 128:(cx + 1) * 128], AF.Copy)
                        yield
                    for c in range(2):
                        ld("sync", tmpc[:, 0:seq], PRJ[OFF_S + c * 128:OFF_S + (c + 1) * 128, t0:t0 + seq])
                        yield
                        S.act(tmpc[:, 0:seq], tmpc[:, 0:seq], AF.Silu)
                        yield
                        S.tt(ysb[:, c, :], ysb[:, c, :], tmpc[:, 0:seq], ALU.mult)
                        yield
                        S.tt(tmpc[:, 0:seq], ysb[:, c, :], ysb[:, c, :], ALU.mult, eng="gpsimd")
                        yield
                        for t in range(NTS):
                            sl = slice(t * 512, (t + 1) * 512)
                            pp = (P2, P3)[t % 2]
                            S.mm(pp[:], ones[:], tmpc[:, sl])
                            S.act(rs[:], pp[:], AF.Ln, scale=1.0 / 128.0, bias=RMS_EPS)
                            yield
                            S.act(rs[:], rs[:], AF.Exp, scale=-0.5)
                            S.tt(rs[:], rs[:], ysb[:, c, sl], ALU.mult)
                            yield
                            S.ts(ob[:, sl], rs[:], nw[:, c:c + 1], None, op0=ALU.mult)
                            yield
                        stq("sync", MIX[768 + c * 128:768 + (c + 1) * 128, t0:t0 + seq], ob[:])
                        yield

                chains = [mkchain(q) for q in range(2)]
                rr([chain_gen(chains[q], q) for q in range(2)])


        def phase_mlstm2(l):
            NCH = seq // 128
            with Ph() as ph:
                ident, triu, strl, ones = chunk_consts(ph)
                m125 = ph.sb("m125", [128, 128])
                S.ts(m125[:], triu[:], 0.125, None, op0=ALU.mult)
                ib = ph.sb("ib", [4, 1]); ld("sync", ib[:], D["m_i_bias"][l].rearrange("(p o) -> p o", o=1))
                nfb = ph.sb("nfb", [4, 1]); ld("sync", nfb[:], D["m_f_bias"][l].rearrange("(p o) -> p o", o=1))
                S.ts(nfb[:], nfb[:], -1.0, None, op0=ALU.mult)
                nw = ph.sb("nw", [128, 2]); colvec(nw, D["m_norm_w"][l])

                def mkchain(q):
                    C = Ctx()
                    n_ = lambda nm: f"{nm}q{q}"
                    C.pb = [ph.ps(n_(f"B{i}"), [128, 512]) for i in range(4)]
                    C.qkv = [[ph.sb(n_(f"{nm}{i}"), [128, 2, 512]) for nm in ("q", "k", "v")] for i in range(2)]
                    for set_ in C.qkv:
                        for t_ in set_:
                            S.split(t_, 128)
                    C.ifm = ph.sb(n_("ifm"), [4, seq]); S.split(C.ifm, 128)
                    C.ffm = ph.sb(n_("ffm"), [4, seq]); S.split(C.ffm, 128)
                    C.hsb = ph.sb(n_("hsb"), [128, 2, seq]); S.split(C.hsb, 128)
                    C.tmpo = ph.sb(n_("tmpo"), [128, seq])
                    C.ob = ph.sb(n_("ob"), [128, seq], BF16)
                    C.Cst = ph.sb(n_("Cst"), [128, 2, 66]); S.split(C.Cst, 66); S.memset(C.Cst[:], 0.0)
                    for nm, shp, gr in (("lifT", [128, 8], None), ("acs", [128, 8], None), ("eb", [128, 4], None),
                                        ("eg", [128, 4], None), ("wd", [128, 4], None), ("ktok", [128, 4, 64], 64),
                                        ("vt1", [128, 4, 66], 66), ("vw", [128, 4, 66], 66), ("Lh", [128, 4, 128], 128),
                                        ("E", [128, 4, 128], 128), ("Sm", [128, 4, 128], 128), ("P", [128, 4, 128], 128),
                                        ("tot", [128, 4, 66], 66), ("dd", [128, 8], None), ("hn", [128, 4, 64], 64),
                                        ("sq", [128, 4, 64], None), ("ss", [128, 4], None)):
                        t_ = ph.sb(n_(nm), shp)
                        if gr:
                            S.split(t_, gr)
                        setattr(C, nm, t_)
                    S.memset(C.vt1[:], 0.0)
                    S.memset(C.vt1[:, :, 64:65], 1.0)
                    return C

                def chain_gen(C, q):
                    t0 = q * seq
                    B0, B1, B2, B3 = C.pb
                    ifm, ffm, hsb, tmpo, ob, Cst = C.ifm, C.ffm, C.hsb, C.tmpo, C.ob, C.Cst
                    lifT, acs, eb, eg, wd, ktok, vt1, vw = C.lifT, C.acs, C.eb, C.eg, C.wd, C.ktok, C.vt1, C.vw
                    Lh, E, Sm, P, tot, dd, hn, sq, ss = C.Lh, C.E, C.Sm, C.P, C.tot, C.dd, C.hn, C.sq, C.ss

                    def load_block(nb):
                        qb, kb, vb = C.qkv[nb % 2]
                        g0 = t0 + nb * 512
                        for c in range(2):
                            for j_, tt_ in enumerate((qb, kb, vb)):
                                ld("sync", tt_[:, c, :], PRJ[j_ * 256 + c * 128:j_ * 256 + (c + 1) * 128, g0:g0 + 512])

                    ld("sync", ifm[0:4, :], PRJ[1024:1028, t0:t0 + seq])
                    ld("sync", ffm[:], PRJ[1028:1032, t0:t0 + seq])
                    load_block(0)
                    yield
                    S.ts(ifm[0:4, :], ifm[0:4, :], ib[:, 0:1], None, op0=ALU.add)
                    S.act(ffm[:], ffm[:], AF.Exp, scale=-1.0, bias=nfb[:, 0:1])
                    yield
                    S.act(ffm[:], ffm[:], AF.Ln, bias=1.0)
                    yield
                    S.ts(ffm[:], ffm[:], -1.0, None, op0=ALU.mult)
                    yield
                    for ci in range(NCH):
                        nb = ci // 4
                        if ci % 4 == 0 and (nb + 1) * 512 < seq:
                            load_block(nb + 1)
                        qb, kb, vb = C.qkv[nb % 2]
                        cl = slice((ci % 4) * 128, (ci % 4 + 1) * 128)
                        cs = slice(ci * 128, (ci + 1) * 128)
                        S.transpose(B3[:, 272:276], ifm[0:4, cs], ident[0:4, 0:4])
                        S.transpose(B3[:, 276:280], ffm[0:4, cs], ident[0:4, 0:4])
                        for cx in range(2):
                            S.transpose(B0[:, cx * 128:(cx + 1) * 128], kb[:, cx, cl], ident[:])
                            S.transpose(B0[:, 256 + cx * 128:256 + (cx + 1) * 128], vb[:, cx, cl], ident[:])
                        yield
                        S.copy(lifT[:], B3[:, 272:280])
                        S.act(vt1[:, :, 0:64], B0[:, 256:512].rearrange("p (h d) -> p h d", h=4), AF.Copy)
                        yield
                        S.mm(B3[:, 280:284], triu[:], lifT[:, 4:8])
                        S.mm(B3[:, 284:288], ones[:], lifT[:, 4:8])
                        S.copy(ktok[:].rearrange("p h d -> p (h d)"), B0[:, 0:256])
                        yield
                        S.copy(acs[:], B3[:, 280:288])
                        yield
                        S.act(eb[:], acs[:, 0:4], AF.Exp)
                        S.act(eg[:], acs[:, 4:8], AF.Exp)
                        S.tt(wd[:], acs[:, 4:8], acs[:, 0:4], ALU.subtract)
                        yield
                        S.tt(wd[:], wd[:], lifT[:, 0:4], ALU.add)
                        yield
                        S.act(wd[:], wd[:], AF.Exp)
                        yield
                        S.ts(wd[:], wd[:], 0.125, None, op0=ALU.mult)
                        yield
                        S.tt(vw[:], vt1[:], wd[:].unsqueeze(2).broadcast_to([128, 4, 66]), ALU.mult, eng="gpsimd")
                        for h in range(4):
                            hc = h // 2
                            hs = slice((h % 2) * 64, (h % 2) * 64 + 64)
                            S.mm(B1[:, h * 128:(h + 1) * 128], kb[hs, hc, cl], qb[hs, hc, cl], sync_self=True)
                            S.ts(Lh[:, h, :], strl[:], lifT[:, 4 + h:5 + h], None, op0=ALU.mult, eng="gpsimd")
                            S.mm(B2[:, h * 128:(h + 1) * 128], Lh[:, h, :], triu[:])
                            yield
                        for h in range(4):
                            S.act(E[:, h, :], B2[:, h * 128:(h + 1) * 128], AF.Exp, bias=lifT[:, h:h + 1])
                            S.tt(Sm[:, h, :], B1[:, h * 128:(h + 1) * 128], m125[:], ALU.mult)
                            yield
                            S.tt(P[:, h, :], E[:, h, :], Sm[:, h, :], ALU.mult, eng="gpsimd")
                            yield
                        for h in range(4):
                            hc = h // 2
                            hs = slice((h % 2) * 64, (h % 2) * 64 + 64)
                            S.mm(B3[:, h * 66:(h + 1) * 66], P[:, h, :], vt1[:, h, :])
                            S.mm(B0[:, h * 66:(h + 1) * 66], qb[hs, hc, cl], Cst[hs, hc, :], sync_self=True)
                            S.mm(B1[hs, hc * 66:(hc + 1) * 66], ktok[:, h, :], vw[:, h, :])
                            yield
                        for h in range(4):
                            hc = h // 2
                            hs = slice((h % 2) * 64, (h % 2) * 64 + 64)
                            S.ts(tot[:, h, :], B0[:, h * 66:(h + 1) * 66], eb[:, h:h + 1], None, op0=ALU.mult)
                            S.stt(Cst[hs, hc, :], Cst[hs, hc, :], eg[hs, h:h + 1], B1[hs, hc * 66:(hc + 1) * 66], ALU.mult, ALU.add)
                            yield
                            S.tt(tot[:, h, :], tot[:, h, :], B3[:, h * 66:(h + 1) * 66], ALU.add)
                            yield
                        den = tot[:, :, 64:65].rearrange("p h o -> p (h o)")
                        S.stt(dd[:, 0:4], den, -1.0, den, ALU.mult, ALU.max)
                        yield
                        S.ts(dd[:, 0:4], dd[:, 0:4], 1.0, None, op0=ALU.max)
                        yield
                        S.recip(dd[:, 4:8], dd[:, 0:4])
                        yield
                        S.tt(hn[:], tot[:, :, 0:64], dd[:, 4:8].unsqueeze(2).broadcast_to([128, 4, 64]), ALU.mult)
                        yield
                        S.tt(sq[:], hn[:], hn[:], ALU.mult, eng="gpsimd")
                        yield
                        S.reduce(ss[:], sq[:], ALU.add, AX.X)
                        yield
                        S.act(ss[:], ss[:], AF.Ln, scale=1.0 / 64.0, bias=RMS_EPS)
                        S.act(ss[:], ss[:], AF.Exp, scale=-0.5)
                        yield
                        S.tt(hn[:], hn[:], ss[:].unsqueeze(2).broadcast_to([128, 4, 64]), ALU.mult)
                        yield
                        for cx in range(2):
                            S.transpose(B2[:, cx * 128:(cx + 1) * 128],
                                        hn[:, 2 * cx:2 * cx + 2, :].rearrange("p h d -> p (h d)"), ident[:])
                        yield
                        for cx in range(2):
                            S.act(hsb[:, cx, cs], B2[:, cx * 128:(cx + 1) * 128], AF.Copy)
                        yield
                    for c in range(2):
                        ld("sync", tmpo[:], PRJ[768 + c * 128:768 + (c + 1) * 128, t0:t0 + seq])
                        yield
                        S.act(tmpo[:], tmpo[:], AF.Sigmoid)
                        yield
                        S.tt(hsb[:, c, :], hsb[:, c, :], tmpo[:], ALU.mult)
                        yield
                        S.ts(ob[:], hsb[:, c, :], nw[:, c:c + 1], None, op0=ALU.mult)
                        stq("sync", MIX[c * 128:(c + 1) * 128, t0:t0 + seq], ob[:])
                        yield

                chains = [mkchain(q) for q in range(2)]
                rr([chain_gen(chains[q], q) for q in range(2)])

        MIXERS = {}
        phase_in()
        for l in range(depth):
            phase_proj(l)
            if "r" in mixers:
                phase_rwkv2(l)
            if "s" in mixers:
                phase_ssd2(l)
            if "m" in mixers:
                phase_mlstm2(l)
                for nm in ("m", "r", "s"):
                    if nm in mixers and nm in MIXERS:
                        MIXERS[nm](l, s)
            for nm, r0 in (("m", 0), ("g", 256), ("r", 512), ("s", 768)):
                if nm not in mixers:
                    phase_zero_mix(r0)
            phase_out(l)
            phase_ffn(l, last=(l == depth - 1))
        S.wait_all("sync")
        S.emit()
    return nc


def kernel(**inputs):
    n = 8
    x = np.ascontiguousarray(inputs["x"], dtype=np.float32).reshape(n, 2 * 2048, DM)
    p = np.ascontiguousarray(inputs["p"], dtype=np.float32).reshape(4, n, 2 * 2048, 256)
    nc = build(4, 2048)
    consts = host_consts()
    in_maps = []
    for c in range(n):
        m = {"x": x[c], "p": np.ascontiguousarray(p[:, c])}
        for nm, _ in WNAMES:
            m[nm] = np.ascontiguousarray(inputs[nm], dtype=np.float32)
        m.update(consts)
        in_maps.append(m)
    res = run_bass_kernel_spmd(nc, in_maps, core_ids=list(range(n)))
    o = np.stack([r["out"] for r in res.results], 0)
    return o.reshape(16, 2048, DM).astype(np.float32)
```
